# Optimizing a Trainium2 kernel written in Bass

```python
import math
import jax
import jax.numpy as jnp
from jax import lax
import numpy as np

D_MODEL = 1024
BATCH = 32
SEQ = 2048
DEPTH = 2

HEAD_DIM = 64
ROT_DIM = HEAD_DIM // 4
ROPE_THETA = 500000.0
QBLOCK = 128
EPS = 1e-6

MLA_HEADS = 8
MLA_Q_RANK = 256
MLA_KV_RANK = 128
MLA_NOPE = 64
MLA_ROPE = 32
MLA_V = 64

DIL_GROUPS = ((128, 1), (512, 4), (2048, 16))
DIL_HEADS = 4

DSA_HEADS = 8
IDX_HEADS = 8
IDX_DIM = 64
TOPK_MAX = 256

IN_SIZES = (MLA_Q_RANK, MLA_KV_RANK, MLA_ROPE,
            3 * DIL_HEADS * HEAD_DIM, 3 * DIL_HEADS * HEAD_DIM, 3 * DIL_HEADS * HEAD_DIM,
            DSA_HEADS * HEAD_DIM, HEAD_DIM, HEAD_DIM, IDX_HEADS * IDX_DIM, IDX_DIM, IDX_HEADS)
C_IN = sum(IN_SIZES)
N_BRANCH = 3

N_GROUPS = 4
EXPERTS_PER_GROUP = 8
N_EXPERTS = N_GROUPS * EXPERTS_PER_GROUP
TOP_K_SUB = 2
D_EXPERT = 256

kernel_name = "hybrid_mla_dilated_dsa_hmoe_deepnorm"

F32 = jnp.float32


def layer_norm(x, g, b):
    xf = x.astype(F32)
    mu = jnp.mean(xf, axis=-1, keepdims=True)
    xc = xf - mu
    var = jnp.mean(xc * xc, axis=-1, keepdims=True)
    return (xc * lax.rsqrt(var + EPS) * g + b).astype(x.dtype)


def rms_norm(x, g):
    xf = x.astype(F32)
    return (xf * lax.rsqrt(jnp.mean(xf * xf, axis=-1, keepdims=True) + EPS) * g).astype(x.dtype)


def rope_tables(seq, rot_dim):
    inv = ROPE_THETA ** (-jnp.arange(0, rot_dim, 2, dtype=F32) / rot_dim)
    ang = jnp.arange(seq, dtype=F32)[:, None] * inv[None, :]
    return jnp.cos(ang), jnp.sin(ang)


def apply_rope(x, cos, sin):
    half = cos.shape[-1]
    rot = 2 * half
    shape = (1, x.shape[1]) + (1,) * (x.ndim - 3) + (half,)
    c = cos.reshape(shape)
    s = sin.reshape(shape)
    x1 = x[..., :half].astype(F32)
    x2 = x[..., half:rot].astype(F32)
    return jnp.concatenate([(x1 * c - x2 * s).astype(x.dtype),
                            (x2 * c + x1 * s).astype(x.dtype),
                            x[..., rot:]], axis=-1)


def causal_mask(lo, hi):
    return (lo + jnp.arange(hi - lo))[:, None] >= jnp.arange(hi)[None, :]


def mla_branch(c_q, c_kv, k_rope, q_norm_g, w_uq, kv_norm_g, w_ukv, cos_m, sin_m):
    B, S, _ = c_q.shape
    q = (rms_norm(c_q, q_norm_g) @ w_uq).reshape(B, S, MLA_HEADS, MLA_NOPE + MLA_ROPE)
    q_nope = q[..., :MLA_NOPE]
    q_rope = apply_rope(q[..., MLA_NOPE:], cos_m, sin_m)
    kv = (rms_norm(c_kv, kv_norm_g) @ w_ukv).reshape(B, S, MLA_HEADS, MLA_NOPE + MLA_V)
    k_nope = kv[..., :MLA_NOPE]
    v = kv[..., MLA_NOPE:]
    k_rope = apply_rope(k_rope, cos_m, sin_m)
    scale = (MLA_NOPE + MLA_ROPE) ** -0.5
    outs = []
    for i in range(S // QBLOCK):
        lo, hi = i * QBLOCK, (i + 1) * QBLOCK
        s = (jnp.einsum('bqhd,bkhd->bhqk', q_nope[:, lo:hi], k_nope[:, :hi])
             + jnp.einsum('bqhr,bkr->bhqk', q_rope[:, lo:hi], k_rope[:, :hi])).astype(F32) * scale
        s = jnp.where(causal_mask(lo, hi), s, -jnp.inf)
        p = jax.nn.softmax(s, axis=-1).astype(v.dtype)
        outs.append(jnp.einsum('bhqk,bkhd->bqhd', p, v[:, :hi]))
    return jnp.concatenate(outs, axis=1).reshape(B, S, MLA_HEADS * MLA_V)


def dilated_group(q, k, v, window, dilation):
    B, S, H, hd = q.shape
    band = window // dilation
    M = S // dilation
    nblk = -(-M // band)
    Mp = nblk * band

    def lattice(t):
        t = t.reshape(B, M, dilation, H, hd).transpose(0, 2, 1, 3, 4)
        t = jnp.pad(t, ((0, 0), (0, 0), (0, Mp - M), (0, 0), (0, 0)))
        return t.reshape(B, dilation, nblk, band, H, hd)

    def with_prev(t):
        prev = jnp.pad(t[:, :, :-1], ((0, 0), (0, 0), (1, 0), (0, 0), (0, 0), (0, 0)))
        return jnp.concatenate([prev, t], axis=3)

    ql = lattice(q)
    kb = with_prev(lattice(k))
    vb = with_prev(lattice(v))
    s = jnp.einsum('brnqhd,brnkhd->brnhqk', ql, kb).astype(F32) * (hd ** -0.5)
    dist = (band + jnp.arange(band))[:, None] - jnp.arange(2 * band)[None, :]
    in_band = (dist >= 0) & (dist <= band)
    has_prev = (jnp.arange(nblk) > 0)[:, None, None] | (jnp.arange(2 * band) >= band)[None, None, :]
    mask = in_band[None] & has_prev
    s = jnp.where(mask[None, None, :, None], s, -jnp.inf)
    lse = jax.nn.logsumexp(s, axis=-1)
    p = jnp.exp(s - lse[..., None]).astype(v.dtype)
    o = jnp.einsum('brnhqk,brnkhd->brnqhd', p, vb)
    o = o.reshape(B, dilation, Mp, H, hd)[:, :, :M].transpose(0, 2, 1, 3, 4).reshape(B, S, H, hd)
    lse = lse.transpose(0, 1, 2, 4, 3).reshape(B, dilation, Mp, H)[:, :, :M]
    lse = lse.transpose(0, 2, 1, 3).reshape(B, S, H)
    return o, lse


def dilated_branch(group_cols, cos_p, sin_p):
    outs, lses = [], []
    for (window, dilation), cols in zip(DIL_GROUPS, group_cols):
        B, S, _ = cols.shape
        qkv = cols.reshape(B, S, 3, DIL_HEADS, HEAD_DIM)
        q = apply_rope(qkv[:, :, 0], cos_p, sin_p)
        k = apply_rope(qkv[:, :, 1], cos_p, sin_p)
        o, lse = dilated_group(q, k, qkv[:, :, 2], window, dilation)
        outs.append(o)
        lses.append(lse)
    w = jax.nn.softmax(jnp.stack(lses, axis=0), axis=0).astype(outs[0].dtype)
    o = jnp.einsum('gbsh,gbshd->bshd', w, jnp.stack(outs, axis=0))
    return o.reshape(B, S, DIL_HEADS * HEAD_DIM)


def dsa_branch(q, k, v, iq, ik, iw, cos_p, sin_p):
    B, S, _ = q.shape
    q = apply_rope(q.reshape(B, S, DSA_HEADS, HEAD_DIM), cos_p, sin_p)
    k = apply_rope(k, cos_p, sin_p)
    iq = apply_rope(iq.reshape(B, S, IDX_HEADS, IDX_DIM), cos_p, sin_p)
    ik = apply_rope(ik, cos_p, sin_p)
    iw = iw.astype(F32) * (IDX_HEADS ** -0.5 * IDX_DIM ** -0.5)
    top = min(TOPK_MAX, S // 4)
    gather = jax.vmap(lambda t, i: t[i])
    outs = []
    for i in range(S // QBLOCK):
        lo, hi = i * QBLOCK, (i + 1) * QBLOCK
        kk = min(top, hi)
        rel = jax.nn.relu(jnp.einsum('bqhd,bsd->bqhs', iq[:, lo:hi], ik[:, :hi]).astype(F32))
        score = jnp.einsum('bqh,bqhs->bqs', iw[:, lo:hi], rel)
        score = jnp.where(causal_mask(lo, hi)[None], score, -jnp.inf)
        _, idx = lax.top_k(score, kk)
        kg = gather(k[:, :hi], idx)
        vg = gather(v[:, :hi], idx)
        s = jnp.einsum('bqhd,bqkd->bqhk', q[:, lo:hi], kg).astype(F32) * (HEAD_DIM ** -0.5)
        ok = idx <= (lo + jnp.arange(QBLOCK))[None, :, None]
        s = jnp.where(ok[:, :, None, :], s, -jnp.inf)
        p = jax.nn.softmax(s, axis=-1).astype(v.dtype)
        outs.append(jnp.einsum('bqhk,bqkd->bqhd', p, vg))
    return jnp.concatenate(outs, axis=1).reshape(B, S, DSA_HEADS * HEAD_DIM)


def token_mixer(x, w_in, q_norm_g, w_uq, kv_norm_g, w_ukv, w_gate, b_gate, w_a, w_b, w_c, w_o,
                cos_m, sin_m, cos_p, sin_p):
    B, S, D = x.shape
    split_points = tuple(int(v) for v in np.cumsum(IN_SIZES)[:-1])
    parts = jnp.split(x @ w_in, split_points, axis=-1)
    c_q, c_kv, k_r = parts[0], parts[1], parts[2]
    dil_cols = parts[3:6]
    cq, ck, cv, iq, ik, iw = parts[6:12]
    o_a = mla_branch(c_q, c_kv, k_r, q_norm_g, w_uq, kv_norm_g, w_ukv, cos_m, sin_m)
    o_b = dilated_branch(dil_cols, cos_p, sin_p)
    o_c = dsa_branch(cq, ck, cv, iq, ik, iw, cos_p, sin_p)
    g = jax.nn.sigmoid((x @ w_gate + b_gate).astype(F32)).astype(x.dtype).reshape(B, S, N_BRANCH, D)
    merged = g[:, :, 0] * (o_a @ w_a) + g[:, :, 1] * (o_b @ w_b) + g[:, :, 2] * (o_c @ w_c)
    return merged @ w_o


def hier_moe(x, w_group, b_group, w_sub, b_sub, w1, w3, w2):
    B, S, D = x.shape
    T = B * S
    t = x.reshape(T, D)
    g_prob = jax.nn.softmax((t @ w_group + b_group).astype(F32), axis=-1)
    g_p, g_idx = lax.top_k(g_prob, 1)
    sub = (t @ w_sub + b_sub).astype(F32).reshape(T, N_GROUPS, EXPERTS_PER_GROUP)
    sub = jnp.take_along_axis(sub, g_idx[:, :, None], axis=1)[:, 0]
    e_val, e_idx = lax.top_k(sub, TOP_K_SUB)
    e_w = jax.nn.softmax(e_val, axis=-1) * g_p
    expert = g_idx * EXPERTS_PER_GROUP + e_idx
    combine = jnp.einsum('tk,tke->te', e_w, jax.nn.one_hot(expert, N_EXPERTS, dtype=F32))
    combine = combine.astype(x.dtype)
    y = jnp.zeros_like(t)
    for e in range(N_EXPERTS):
        h = jax.nn.silu(t @ w1[e]) * (t @ w3[e])
        y = y + combine[:, e:e + 1] * (h @ w2[e])
    return y.reshape(B, S, D)


def setup_inputs(seed: int = 0) -> dict:
    key = jax.random.key(seed)
    ks = jax.random.split(key, 24)
    L, D = DEPTH, D_MODEL
    beta = (8 * DEPTH) ** -0.25

    def nrm(k, shape, scale):
        return jax.random.normal(k, shape, F32) * scale

    return {
        "x": nrm(ks[0], (BATCH, SEQ, D), 1.0),
        "w_in": nrm(ks[1], (L, D, C_IN), D ** -0.5),
        "q_norm_g": 1.0 + nrm(ks[2], (L, MLA_Q_RANK), 0.01),
        "w_uq": nrm(ks[3], (L, MLA_Q_RANK, MLA_HEADS * (MLA_NOPE + MLA_ROPE)), MLA_Q_RANK ** -0.5),
        "kv_norm_g": 1.0 + nrm(ks[4], (L, MLA_KV_RANK), 0.01),
        "w_ukv": nrm(ks[5], (L, MLA_KV_RANK, MLA_HEADS * (MLA_NOPE + MLA_V)), MLA_KV_RANK ** -0.5),
        "w_gate": nrm(ks[6], (L, D, N_BRANCH * D), D ** -0.5),
        "b_gate": nrm(ks[7], (L, N_BRANCH * D), 0.01),
        "w_a": nrm(ks[8], (L, MLA_HEADS * MLA_V, D), beta * (MLA_HEADS * MLA_V) ** -0.5),
        "w_b": nrm(ks[9], (L, DIL_HEADS * HEAD_DIM, D), beta * (DIL_HEADS * HEAD_DIM) ** -0.5),
        "w_c": nrm(ks[10], (L, DSA_HEADS * HEAD_DIM, D), beta * (DSA_HEADS * HEAD_DIM) ** -0.5),
        "w_o": nrm(ks[11], (L, D, D), beta * D ** -0.5),
        "ln1_g": 1.0 + nrm(ks[12], (L, D), 0.01),
        "ln1_b": nrm(ks[13], (L, D), 0.01),
        "w_group": nrm(ks[14], (L, D, N_GROUPS), D ** -0.5),
        "b_group": nrm(ks[15], (L, N_GROUPS), 0.01),
        "w_sub": nrm(ks[16], (L, D, N_EXPERTS), D ** -0.5),
        "b_sub": nrm(ks[17], (L, N_EXPERTS), 0.01),
        "w1": nrm(ks[18], (L, N_EXPERTS, D, D_EXPERT), D ** -0.5),
        "w3": nrm(ks[19], (L, N_EXPERTS, D, D_EXPERT), D ** -0.5),
        "w2": nrm(ks[20], (L, N_EXPERTS, D_EXPERT, D), beta * D_EXPERT ** -0.5),
        "ln2_g": 1.0 + nrm(ks[21], (L, D), 0.01),
        "ln2_b": nrm(ks[22], (L, D), 0.01),
    }


def reference(x, w_in, q_norm_g, w_uq, kv_norm_g, w_ukv, w_gate, b_gate, w_a, w_b, w_c, w_o,
              ln1_g, ln1_b, w_group, b_group, w_sub, b_sub, w1, w3, w2, ln2_g, ln2_b):
    S = x.shape[1]
    alpha = (2 * DEPTH) ** 0.25
    cos_m, sin_m = rope_tables(S, MLA_ROPE)
    cos_p, sin_p = rope_tables(S, ROT_DIM)
    for l in range(DEPTH):
        mix = token_mixer(x, w_in[l], q_norm_g[l], w_uq[l], kv_norm_g[l], w_ukv[l], w_gate[l],
                          b_gate[l], w_a[l], w_b[l], w_c[l], w_o[l], cos_m, sin_m, cos_p, sin_p)
        x = layer_norm(alpha * x + mix, ln1_g[l], ln1_b[l])
        ffn = hier_moe(x, w_group[l], b_group[l], w_sub[l], b_sub[l], w1[l], w3[l], w2[l])
        x = layer_norm(alpha * x + ffn, ln2_g[l], ln2_b[l])
    return x
```

```python
import numpy as np
from contextlib import ExitStack
import concourse.bass as bass
import concourse.mybir as mybir
from concourse.bass_utils import run_bass_kernel_spmd

F32 = mybir.dt.float32
BF16 = mybir.dt.bfloat16
AF = mybir.ActivationFunctionType
ALU = mybir.AluOpType
AX = mybir.AxisListType

S = 2048
NT = 16
D = 1024
KC = 8
DEPTH = 2
NCORES = 8
ALPHA = float((2 * DEPTH) ** 0.25)
EPS = 1e-6
NEG = -1.0e30
ENGS = ("pe", "act", "dve", "pool", "sp")
NBIS = 22
DBG = {"dsa_tiles": NT, "dsa_stage": 4, "att": 9}


class Op:
    __slots__ = ("eng", "fn", "deps", "dma", "need", "sig", "waits")

    def __init__(self, eng, fn, deps, dma):
        self.eng = eng
        self.fn = fn
        self.deps = deps
        self.dma = dma
        self.need = False
        self.sig = None
        self.waits = None


class Prog:
    def __init__(self):
        self.ops = []
        self.gen = {}
        self.fence = {}
        self.last_eng = {}
        self.last_dma = {}

    def add(self, eng, fn, r=(), w=(), dma=None):
        idx = len(self.ops)
        deps = {}
        for k in r:
            if isinstance(k, tuple) and k and k[0] == "ps":
                g = self.gen.get(k)
                if g:
                    for d in g[1]:
                        deps.setdefault(d, False)
        for k in r:
            g = self.gen.get(k)
            if g:
                for d in g[0]:
                    deps[d] = True
        for k in w:
            g = self.gen.get(k)
            if g:
                for d in g[1]:
                    deps.setdefault(d, False)
        for d in self.fence.values():
            deps.setdefault(d, False)
        for k in r:
            self.gen.setdefault(k, [[], []])[1].append(idx)
        for k in w:
            g = self.gen.setdefault(k, [[], []])
            if g[1]:
                g[0] = [idx]
                g[1] = []
            else:
                g[0].append(idx)
        self.ops.append(Op(eng, fn, deps, dma))
        if dma is None:
            self.last_eng[eng] = idx
        else:
            self.last_dma[dma[0]] = idx
        return idx

    def barrier(self):
        f = {}
        for e, i in self.last_eng.items():
            f[("e", e)] = i
        for s, i in self.last_dma.items():
            f[("d", s)] = i
        self.fence = f

    def emit(self, nc, stack):
        ops = self.ops
        for o in ops:
            o.waits = []
            for d, raw in o.deps.items():
                p = ops[d]
                if p.dma is None and p.eng == o.eng and o.dma is None and o.eng == "pe":
                    continue
                o.waits.append(d)
                p.need = True
        dcount = {}
        dround_end = {}
        for o in ops:
            if o.dma is not None:
                s, rd = o.dma
                dcount[s] = dcount.get(s, 0) + 1
                dround_end[(s, rd)] = dcount[s]
        sems = {}

        def getsem(name):
            if name not in sems:
                sems[name] = stack.enter_context(nc.semaphore("s_" + name))
            return sems[name]

        LIM = 24000
        cnt = {e: 0 for e in ENGS}
        for o in ops:
            if o.dma is not None:
                s, rd = o.dma
                o.sig = (getsem("d_" + s), 16 * dround_end[(s, rd)])
            elif o.need:
                c = cnt[o.eng]
                ep = c // LIM
                o.sig = (getsem("%s%d" % (o.eng, ep)), c % LIM + 1)
                cnt[o.eng] = c + 1
        per = {e: [] for e in ENGS}
        for o in ops:
            per[o.eng].append(o)

        def run(e, lst):
            waited = {}
            for o in lst:
                for d in o.waits:
                    sem, val = ops[d].sig
                    key = id(sem)
                    if waited.get(key, 0) >= val:
                        continue
                    waited[key] = val
                    e.wait_ge(sem, val)
                ins = o.fn(e)
                if o.dma is not None:
                    ins.then_inc(o.sig[0], 16)
                elif o.need:
                    ins.then_inc(o.sig[0], 1)

        with nc.Block() as block:
            @block.tensor
            def _(e):
                run(e, per["pe"])

            @block.scalar
            def _(e):
                run(e, per["act"])

            @block.vector
            def _(e):
                run(e, per["dve"])

            @block.gpsimd
            def _(e):
                run(e, per["pool"])

            @block.sync
            def _(e):
                run(e, per["sp"])


class Arena:
    def __init__(self, tens, n):
        self.t = tens
        self.n = n
        self.top = 0
        self.peak = 0

    def alloc(self, dtype, *shape):
        n = 1
        for s in shape:
            n *= s
        size = n * (2 if dtype == F32 else 1)
        size = (size + 31) // 32 * 32
        off = self.top
        self.top += size
        self.peak = max(self.peak, self.top)
        assert self.top <= self.n, ("arena overflow", self.top, self.n)
        ap = self.t[:, off:off + (n * 2 if dtype == F32 else n)]
        if dtype == F32:
            ap = ap.bitcast(F32)
        if len(shape) > 1:
            names = "abcdefg"[: len(shape)]
            pat = "p (%s) -> p %s" % (" ".join(names), " ".join(names))
            kw = {names[i]: shape[i] for i in range(len(shape))}
            ap = ap.rearrange(pat, **kw)
        return ap

    def view(self, off, dtype, *shape):
        save = self.top
        self.top = off
        ap = self.alloc(dtype, *shape)
        end = self.top
        self.top = save
        return ap, end

    def mark(self):
        return self.top

    def release(self, m):
        self.top = m


class K:
    pass


def MM(P, out, lhsT, rhs, start, stop, r, w, skip=False):
    if skip:
        P.add("pe", lambda e: e.matmul(out, lhsT=lhsT, rhs=rhs, start=start, stop=stop, skip_group_check=True), r, w)
    else:
        P.add("pe", lambda e: e.matmul(out, lhsT=lhsT, rhs=rhs, start=start, stop=stop), r, w)


def TR(P, out, in_, ident, r, w):
    P.add("pe", lambda e: e.transpose(out, in_, ident), r, w)


def ACTV(P, out, in_, func, r, w, bias=None, scale=None, accum=None):
    kw = {}
    if bias is not None:
        kw["bias"] = bias
    if scale is not None:
        kw["scale"] = scale
    if accum is not None:
        kw["accum_out"] = accum
    P.add("act", lambda e: e.activation(out=out, in_=in_, func=func, **kw), r, w)


def TT(P, eng, out, in0, in1, op, r, w):
    P.add(eng, lambda e: e.tensor_tensor(out=out, in0=in0, in1=in1, op=op), r, w)


def TS(P, eng, out, in0, s1, s2, op0, op1, r, w, accum=None):
    if op1 is None:
        P.add(eng, lambda e: e.tensor_scalar(out=out, in0=in0, scalar1=s1, scalar2=0.0, op0=op0, op1=ALU.add), r, w)
    elif accum is None:
        P.add(eng, lambda e: e.tensor_scalar(out=out, in0=in0, scalar1=s1, scalar2=s2, op0=op0, op1=op1), r, w)
    else:
        P.add(eng, lambda e: e.tensor_scalar(out=out, in0=in0, scalar1=s1, scalar2=s2, op0=op0, op1=op1,
                                             accum_out=accum), r, w)


def STT(P, eng, out, in0, scalar, in1, op0, op1, r, w):
    P.add(eng, lambda e: e.scalar_tensor_tensor(out=out, in0=in0, scalar=scalar, in1=in1, op0=op0, op1=op1), r, w)


def CP(P, eng, out, in_, r, w):
    if eng == "act":
        P.add("act", lambda e: e.copy(out=out, in_=in_), r, w)
    else:
        P.add(eng, lambda e: e.tensor_copy(out=out, in_=in_), r, w)


def RED(P, out, in_, op, r, w, absv=False):
    if absv:
        P.add("dve", lambda e: e.tensor_reduce(out=out, in_=in_, axis=AX.X, op=op, apply_absolute_value=True), r, w)
    else:
        P.add("dve", lambda e: e.tensor_reduce(out=out, in_=in_, axis=AX.X, op=op), r, w)


def RECIP(P, out, in_, r, w):
    P.add("dve", lambda e: e.reciprocal(out=out, in_=in_), r, w)


def MSET(P, eng, ap, val, r, w):
    P.add(eng, lambda e: e.memset(ap, val), r, w)


def DMA(P, q, out, in_, stream, rnd, r, w):
    P.add(q, lambda e: e.dma_start(out=out, in_=in_), r, w, dma=(q + "_" + stream, rnd))


def build(nseq=4, nlayers=DEPTH, dbg=None, stop_after=None):
    dbg = dbg or set()
    nc = bass.Bass("TRN2", target_bir_lowering=False)
    L = DEPTH

    def din(name, shape):
        return nc.dram_tensor(name, list(shape), F32, kind="ExternalInput").ap()

    x = din("x", (nseq, S, D))
    w_in = din("w_in", (L, D, 3944))
    q_norm_g = din("q_norm_g", (L, 256))
    w_uq = din("w_uq", (L, 256, 768))
    kv_norm_g = din("kv_norm_g", (L, 128))
    w_ukv = din("w_ukv", (L, 128, 1024))
    w_gate = din("w_gate", (L, D, 3072))
    b_gate = din("b_gate", (L, 3072))
    w_a = din("w_a", (L, 512, D))
    w_b = din("w_b", (L, 256, D))
    w_c = din("w_c", (L, 512, D))
    w_o = din("w_o", (L, D, D))
    ln1_g = din("ln1_g", (L, D))
    ln1_b = din("ln1_b", (L, D))
    w_group = din("w_group", (L, D, 4))
    b_group = din("b_group", (L, 4))
    w_sub = din("w_sub", (L, D, 32))
    b_sub = din("b_sub", (L, 32))
    w1 = din("w1", (L, 32, D, 256))
    w3 = din("w3", (L, 32, D, 256))
    w2 = din("w2", (L, 32, 256, D))
    ln2_g = din("ln2_g", (L, D))
    ln2_b = din("ln2_b", (L, D))
    c_ident = din("c_ident", (128, 128))
    c_masks = din("c_masks", (128, 7 * 128))
    c_negm = din("c_negm", (128, 128))
    c_ropep = din("c_ropep", (128, NT * 32))
    c_ropem = din("c_ropem", (128, NT * 64))
    out = nc.dram_tensor("out", [nseq, S, D], F32, kind="ExternalOutput").ap()
    dbg_t = {}
    for name, shape in (("d_oc", (128, 4 * S)), ("d_oa", (128, 4 * S)), ("d_ob", (128, 2 * S)),
                        ("d_x1", (S, D)), ("d_sc", (128, S)), ("d_comb", (128, NT * 32))):
        if name in dbg:
            dbg_t[name] = nc.dram_tensor(name, list(shape), F32, kind="ExternalOutput").ap()

    P = Prog()
    stack = ExitStack()
    with stack:
        ARN = 106000
        arena_t = stack.enter_context(nc.sbuf_tensor("arena", [128, ARN], BF16))
        A = Arena(arena_t, ARN)
        psb = [stack.enter_context(nc.psum_tensor("psb%d" % i, [128, 512], F32)) for i in range(8)]

        def ps(b):
            return psb[b][:, :]

        def psbf(b):
            return psb[b][:, :].bitcast(BF16)

        BIG = A.alloc(F32, NT, D)
        XT = A.alloc(BF16, KC, S)
        identb = A.alloc(BF16, 128)
        identf = A.alloc(F32, 128)
        masks = A.alloc(BF16, 7, 128)
        negm = A.alloc(F32, 128)
        ropep = A.alloc(F32, NT, 32)
        ropem = A.alloc(F32, NT, 64)
        onesf = A.alloc(F32, 128)
        pow2 = A.alloc(F32, NBIS)

        DMA(P, "sp", identf, c_ident, "const", 0, [], ["identf"])
        DMA(P, "pool", identb, c_ident, "constb", 0, [], ["identb"])
        DMA(P, "pool", masks, c_masks.rearrange("p (m t) -> p m t", m=7), "constb", 0, [], ["masks"])
        DMA(P, "sp", negm, c_negm, "const", 0, [], ["negm"])
        DMA(P, "sp", ropep, c_ropep.rearrange("p (i c) -> p i c", i=NT), "const", 0, [], ["ropep"])
        DMA(P, "sp", ropem, c_ropem.rearrange("p (i c) -> p i c", i=NT), "const", 0, [], ["ropem"])
        MSET(P, "dve", onesf, 1.0, [], ["onesf"])
        for k in range(NBIS):
            MSET(P, "dve", pow2[:, k:k + 1], float(2.0 ** -k), [], ["pow2"])

        M_CAUS, M_PREV, M_R4D, M_R4M, M_R4F, M_R16D, M_R16O = range(7)

        rot = {}

        def slot(name, n):
            v = rot.get(name, 0)
            rot[name] = v + 1
            return v % n

        def to_XT(i, xb_tiles):
            sl = slot("xb", 2)
            xb = xb_tiles[sl]
            CP(P, "dve", xb, BIG[:, i, :], [("BIG", i)], [("xb", sl)])
            b = 6 + slot("tp", 2)
            pv = psbf(b).rearrange("p (k t) -> p k t", k=8)
            for kc in range(KC):
                TR(P, pv[:, kc, :], xb[:, kc * 128:(kc + 1) * 128], identb, [("xb", sl), "identb"], [("ps", b)])
            CP(P, "act", XT[:, :, i * 128:(i + 1) * 128], pv, [("ps", b)], [("XT", i)])

        def proj(i, wt, c0, ncols, pout, wkey, b, start=True, stop=True):
            for kc in range(KC):
                MM(P, pout, XT[:, kc, i * 128:(i + 1) * 128], wt[:, kc, c0:c0 + ncols],
                   start and kc == 0, stop and kc == KC - 1, [("XT", i), wkey], [("ps", b)])

        def rope(src, dst, H, hd, half, table, i, rkeys, wkeys, tmp, tkey):
            r2 = 2 * half
            cc = table[:, i, 0:r2].unsqueeze(1).to_broadcast([128, H, r2])
            ns = table[:, i, r2:r2 + half].unsqueeze(1).to_broadcast([128, H, half])
            ps_ = table[:, i, r2 + half:r2 + 2 * half].unsqueeze(1).to_broadcast([128, H, half])
            u = tmp[0][:, 0:H * r2].rearrange("p (h c) -> p h c", h=H)
            v = tmp[1][:, 0:H * r2].rearrange("p (h c) -> p h c", h=H)
            TT(P, "dve", u[:, :, 0:half], src[:, :, half:r2], ns, ALU.mult, rkeys + ["rtu"], ["rtu"])
            TT(P, "dve", u[:, :, half:r2], src[:, :, 0:half], ps_, ALU.mult, rkeys + ["rtu"], ["rtu"])
            TT(P, "dve", v, src[:, :, 0:r2], cc, ALU.mult, rkeys + ["rtv"], ["rtv"])
            TT(P, "dve", dst[:, :, 0:r2], u, v, ALU.add, ["rtu", "rtv"], wkeys)
            if hd > r2:
                CP(P, "dve" if H > 1 else "act", dst[:, :, r2:hd], src[:, :, r2:hd], rkeys, wkeys)

        def layer_norm(i, g_rep, b_rep, gkey, tl):
            xt_ = BIG[:, i, :]
            kB = ("BIG", i)
            st = tl["st"]
            sk = "lnst"
            RED(P, st[:, 0:1], xt_, ALU.add, [kB], [sk + "0"])
            TS(P, "dve", st[:, 1:2], st[:, 0:1], -1.0 / D, None, ALU.mult, None, [sk + "0"], [sk + "1"])
            ACTV(P, xt_, xt_, AF.Identity, [kB, sk + "1"], [kB], bias=st[:, 1:2])
            ACTV(P, tl["junk"], xt_, AF.Square, [kB], ["lnjunk", sk + "2"], accum=st[:, 2:3])
            ACTV(P, st[:, 3:4], st[:, 2:3], AF.Sqrt, [sk + "2"], [sk + "3"], bias=EPS, scale=1.0 / D)
            RECIP(P, st[:, 4:5], st[:, 3:4], [sk + "3"], [sk + "4"])
            STT(P, "dve", xt_, xt_, st[:, 4:5], g_rep, ALU.mult, ALU.mult, [kB, sk + "4", gkey], [kB])
            TT(P, "dve", xt_, xt_, b_rep, ALU.add, [kB, gkey], [kB])

        def attn_tile(i, pairs, qk_fn, v_fn, scale, Ptiles, nh=4):
            ob = 4 + slot("O", 2)
            Ov = ps(ob)[:, 0:nh * 65].rearrange("p (h c) -> p h c", h=nh)
            n = len(pairs)
            for idx, (j, mk, mkey) in enumerate(pairs):
                sb_ = 2 + slot("S", 2)
                Sv = ps(sb_).rearrange("p (h t) -> p h t", h=4)
                for (h0, nhh, lhsT, rhs, rk) in qk_fn(j):
                    MM(P, Sv[:, h0:h0 + nhh, :], lhsT, rhs, True, True, rk, [("ps", sb_)])
                if DBG["att"] < 1:
                    continue
                psl = slot("P", 3)
                Pt = Ptiles[psl]
                ACTV(P, Pt[:, 0:nh, :], Sv[:, 0:nh, :], (AF.Relu if DBG.get("noexp") else AF.Exp), [("ps", sb_)], [("P", psl)], scale=scale)
                if DBG["att"] < 2:
                    continue
                if mk is not None:
                    TT(P, "dve", Pt[:, 0:nh, :], Pt[:, 0:nh, :], mk.unsqueeze(1).to_broadcast([128, nh, 128]),
                       ALU.mult, [("P", psl), mkey], [("P", psl)])
                if DBG["att"] < 3:
                    continue
                for hh in range(nh):
                    rv, rk = v_fn(hh, j)
                    MM(P, Ov[:, hh, :], Pt[:, hh, :], rv, idx == 0 and hh == 0, idx == n - 1, [("P", psl)] + rk,
                       [("ps", ob)], skip=True)
            return Ov, ob

        def normalize_out(Ov, ob, dst, nh, tl):
            sl = slot("rc", 2)
            rc = tl["rc"][sl]
            RECIP(P, rc[:, 0:nh], Ov[:, :, 64], [("ps", ob)], [("rc", sl)])
            TT(P, "dve", dst, Ov[:, :, 0:64], rc[:, 0:nh].unsqueeze(2).to_broadcast([128, nh, 64]), ALU.mult,
               [("ps", ob), ("rc", sl)], ["normdst"])

        def transposes_to(src_bf, nblk, width, dst_ap, rkeys, wkeys, rows=128):
            b = 6 + slot("tp", 2)
            pv = psbf(b).rearrange("p (k t) -> p k t", k=8)
            for k in range(nblk):
                TR(P, pv[0:width, k, :], src_bf[:, k * width:(k + 1) * width], identb, rkeys + ["identb"], [("ps", b)])
            CP(P, "act" if rows == 128 else "dve", dst_ap, pv[0:rows, 0:nblk, :], [("ps", b)], wkeys)

        class Stop(Exception):
            pass

        def chk(name):
            if stop_after == name:
                raise Stop()

        def seq_layer(sq, l):
            if True:
                tag = "s%dl%d" % (sq, l)
                win_v = w_in[l].rearrange("(kc p) c -> p kc c", p=128)
                m_layer = A.mark()
                if l == 0:
                    m0 = A.mark()
                    xb_tiles = [A.alloc(BF16, D) for _ in range(2)]
                    for i in range(NT):
                        DMA(P, "sp", BIG[:, i, :], x[sq, i * 128:(i + 1) * 128, :], "x", sq, [], [("BIG", i)])
                    for i in range(NT):
                        to_XT(i, xb_tiles)
                    P.barrier()
                    A.release(m0)
                    chk("load")

                OTc = A.alloc(BF16, 4, S)
                m0 = A.mark()
                WQ = A.alloc(BF16, KC, 1024)
                WK = A.alloc(BF16, KC, 200)
                KI = A.alloc(BF16, 2, S)
                VC = A.alloc(BF16, NT, 72)
                WAb = A.alloc(F32, NT, 8)
                SG = A.alloc(F32, NT, 8)
                SC = A.alloc(F32, S)
                MK = A.alloc(BF16, S)
                MKT = A.alloc(BF16, NT, 128)
                RL = [A.alloc(F32, 512) for _ in range(2)]
                QTi = [A.alloc(BF16, 8, 128) for _ in range(2)]
                IQTi = [A.alloc(BF16, 8, 128) for _ in range(2)]
                QB = [A.alloc(BF16, 512) for _ in range(2)]
                IQB = [A.alloc(BF16, 512) for _ in range(2)]
                KK = [A.alloc(BF16, 128) for _ in range(2)]
                Pt = [A.alloc(BF16, 4, 128) for _ in range(3)]
                rtmp = [A.alloc(F32, 256) for _ in range(2)]
                tl = {"rc": [A.alloc(F32, 8) for _ in range(2)]}
                OCb = [A.alloc(BF16, 8, 64) for _ in range(2)]
                bis = A.alloc(F32, 8)
                wtab = A.alloc(F32, NBIS)

                DMA(P, "pool", WK[:, :, 0:128], win_v[:, :, 3232:3360], "wk", tag, [], ["WK"])
                DMA(P, "pool", WK[:, :, 128:200], win_v[:, :, 3872:3944], "wk", tag, [], ["WK"])
                DMA(P, "pool", WQ[:, :, 0:512], win_v[:, :, 2720:3232], "wq", tag, [], ["WQ"])
                DMA(P, "pool", WQ[:, :, 512:1024], win_v[:, :, 3360:3872], "wq", tag, [], ["WQ"])
                CP(P, "dve", VC[:, :, 64:65], onesf[:, 0:NT].unsqueeze(2), ["onesf"], ["VCones"])
                for i in range(NT):
                    b = slot("pj", 2)
                    pv = ps(b)
                    proj(i, WK, 0, 128, pv[:, 0:128], "WK", b)
                    proj(i, WK, 128, 72, pv[:, 128:200], "WK", b)
                    sl = slot("KK", 2)
                    kk = KK[sl]
                    rope(pv[:, 0:64].unsqueeze(1), kk[:, 0:64].unsqueeze(1), 1, 64, 8, ropep, i,
                         [("ps", b), "ropep"], [("KK", sl)], rtmp, "rtA")
                    rope(pv[:, 128:192].unsqueeze(1), kk[:, 64:128].unsqueeze(1), 1, 64, 8, ropep, i,
                         [("ps", b), "ropep"], [("KK", sl)], rtmp, "rtB")
                    CP(P, "dve", VC[:, i, 0:64], pv[:, 64:128], [("ps", b)], [("VC", i)])
                    ACTV(P, WAb[:, i, :], pv[:, 192:200], AF.Abs, [("ps", b)], [("WA", i)])
                    ACTV(P, SG[:, i, :], pv[:, 192:200], AF.Sign, [("ps", b)], [("SG", i)])
                    tb = 6 + slot("tp", 2)
                    tv = psbf(tb).rearrange("p (k t) -> p k t", k=8)
                    TR(P, tv[0:64, 0, :], kk[:, 0:64], identb, [("KK", sl), "identb"], [("ps", tb)])
                    TR(P, tv[0:64, 1, :], kk[:, 64:128], identb, [("KK", sl), "identb"], [("ps", tb)])
                    CP(P, "dve", KI[0:64, :, i * 128:(i + 1) * 128], tv[0:64, 0:2, :], [("ps", tb)], [("KI", i)])

                chk("dsa_pro")

                def dsa_qprep(i):
                    sl = slot("dq", 2)
                    for (c0, dstb, dstT, nm) in ((0, QB[sl], QTi[sl], "q"), (512, IQB[sl], IQTi[sl], "iq")):
                        b = slot("pj", 2)
                        pv = ps(b)
                        proj(i, WQ, c0, 512, pv, "WQ", b)
                        rope(pv.rearrange("p (h c) -> p h c", h=8), dstb.rearrange("p (h c) -> p h c", h=8),
                             8, 64, 8, ropep, i, [("ps", b), "ropep"], [(nm + "B", sl)], rtmp, "rtQ")
                        transposes_to(dstb, 8, 64, dstT[0:64, :, :], [(nm + "B", sl)], [(nm + "T", sl)], rows=64)
                    return sl

                nxt = dsa_qprep(0)
                for i in range(DBG["dsa_tiles"]):
                    sl = nxt
                    hi = (i + 1) * 128
                    for h in range(8):
                        for c0 in range(0, hi, 512):
                            c1 = min(hi, c0 + 512)
                            sb_ = 2 + slot("S", 2)
                            Rv = ps(sb_)[:, 0:c1 - c0]
                            jkeys = [("KI", jj) for jj in range(c0 // 128, c1 // 128)]
                            MM(P, Rv, IQTi[sl][0:64, h, :], KI[0:64, 1, c0:c1], True, True,
                               [("iqT", sl)] + jkeys, [("ps", sb_)])
                            rs = slot("RL", 2)
                            ACTV(P, RL[rs][:, 0:c1 - c0], Rv, AF.Relu, [("ps", sb_), ("WA", i)], [("RL", rs)],
                                 scale=WAb[:, i, h:h + 1])
                            if h == 0:
                                TS(P, "dve", SC[:, c0:c1], RL[rs][:, 0:c1 - c0], SG[:, i, h:h + 1], None, ALU.mult, None,
                                   [("RL", rs), ("SG", i)], [("SC", c0)])
                            else:
                                STT(P, "dve", SC[:, c0:c1], RL[rs][:, 0:c1 - c0], SG[:, i, h:h + 1], SC[:, c0:c1],
                                    ALU.mult, ALU.add, [("RL", rs), ("SG", i), ("SC", c0)], [("SC", c0)])
                    sckeys = [("SC", c0) for c0 in range(0, hi, 512)]
                    if i + 1 < NT:
                        nxt = dsa_qprep(i + 1)
                    if DBG["dsa_stage"] < 2:
                        continue
                    if i >= 2:
                        RED(P, bis[:, 0:1], SC[:, 0:hi], ALU.max, sckeys, ["bisB"], absv=True)
                        TS(P, "dve", bis[:, 0:1], bis[:, 0:1], 1.0, None, ALU.add, None, ["bisB"], ["bisB"])
                        TS(P, "dve", bis[:, 1:2], bis[:, 0:1], -1.0, None, ALU.mult, None, ["bisB"], ["bislo"])
                        TS(P, "dve", wtab, pow2, bis[:, 0:1], None, ALU.mult, None, ["bisB", "pow2"], ["wtab"])
                    TT(P, "dve", SC[:, i * 128:hi], SC[:, i * 128:hi], negm, ALU.add,
                       [("SC", (i * 128) // 512 * 512), "negm"], [("SC", (i * 128) // 512 * 512)])
                    if i >= 2:
                        for k in range(NBIS):
                            TT(P, "dve", bis[:, 2:3], bis[:, 1:2], wtab[:, k:k + 1], ALU.add, ["bislo", "wtab"], ["bismid"])
                            TS(P, "dve", MK[:, 0:hi], SC[:, 0:hi], bis[:, 2:3], 0.0, ALU.is_ge, ALU.add,
                               sckeys + ["bismid"], ["MK", "biscnt"], accum=bis[:, 3:4])
                            TS(P, "dve", bis[:, 4:5], bis[:, 3:4], 256.0, wtab[:, k:k + 1], ALU.is_ge, ALU.mult,
                               ["biscnt", "wtab"], ["bisstep"])
                            TT(P, "dve", bis[:, 1:2], bis[:, 1:2], bis[:, 4:5], ALU.add, ["bislo", "bisstep"], ["bislo"])
                    else:
                        MSET(P, "dve", bis[:, 1:2], -1.0e29, ["bislo"], ["bislo"])
                    TS(P, "dve", MK[:, 0:hi], SC[:, 0:hi], bis[:, 1:2], None, ALU.is_ge, None, sckeys + ["bislo"], ["MK"])
                    if "d_sc" in dbg_t and sq == 0 and l == 0 and i == NT - 1:
                        DMA(P, "sp", dbg_t["d_sc"], SC, "dbg", ("dbg", len(P.ops)), sckeys, ["dbgout"])
                    if DBG["dsa_stage"] < 3:
                        continue
                    for j0 in range(0, i + 1, 8):
                        j1 = min(i + 1, j0 + 8)
                        tb = 6 + slot("tp", 2)
                        tv = psbf(tb).rearrange("p (k t) -> p k t", k=8)
                        for j in range(j0, j1):
                            TR(P, tv[:, j - j0, :], MK[:, j * 128:(j + 1) * 128], identb, ["MK", "identb"], [("ps", tb)])
                        CP(P, "act", MKT[:, j0:j1, :], tv[:, 0:j1 - j0, :], [("ps", tb)], [("MKT", j0)])
                    if DBG["dsa_stage"] < 4:
                        continue
                    osl = slot("OCb", 2)
                    for hg in range(2):
                        def qk_fn(j, hg=hg, sl=sl):
                            return [(0, 4, KI[0:64, 0, j * 128:(j + 1) * 128], QTi[sl][0:64, 4 * hg:4 * hg + 4, :],
                                     [("KI", j), ("qT", sl)])]

                        def v_fn(hh, j):
                            return VC[:, j, 0:65], [("VC", j), "VCones"]

                        pairs = [(j, MKT[:, j, :], ("MKT", j // 8 * 8)) for j in range(i + 1)]
                        Ov, ob = attn_tile(i, pairs, qk_fn, v_fn, 0.125, Pt)
                        if DBG["att"] < 4:
                            continue
                        normalize_out(Ov, ob, OCb[osl][:, 4 * hg:4 * hg + 4, :], 4, tl)
                    if DBG["att"] < 5:
                        continue
                    transposes_to(OCb[osl].rearrange("p h c -> p (h c)"), 4, 128, OTc[:, :, i * 128:(i + 1) * 128],
                                  ["normdst"], [("OTc", i)])
                if "d_oc" in dbg_t and sq == 0 and l == 0:
                    DMA(P, "pool", dbg_t["d_oc"].rearrange("p (k t) -> p k t", k=4), OTc, "dbg", ("dbg", len(P.ops)),
                        [("OTc", i) for i in range(NT)], ["dbgout"])
                P.barrier()
                A.release(m0)
                chk("dsa")


                OTb = A.alloc(BF16, 2, S)
                m0 = A.mark()
                ACC = A.alloc(F32, NT, 4, 65)
                WGq = A.alloc(BF16, KC, 256)
                WGkv = A.alloc(BF16, KC, 512)
                KT = A.alloc(BF16, NT, 4, 128)
                VA = A.alloc(BF16, NT, 4, 72)
                QTi = [A.alloc(BF16, 4, 128) for _ in range(2)]
                QB = [A.alloc(BF16, 256) for _ in range(2)]
                KB = [A.alloc(BF16, 256) for _ in range(2)]
                Pt = [A.alloc(BF16, 4, 128) for _ in range(3)]
                rtmp = [A.alloc(F32, 256) for _ in range(2)]
                tl = {"rc": [A.alloc(F32, 8) for _ in range(2)]}
                OBb = [A.alloc(BF16, 4, 64) for _ in range(2)]
                CP(P, "dve", VA[:, :, :, 64:65], onesf[:, 0:64].rearrange("p (a b) -> p a b", a=NT).unsqueeze(3),
                   ["onesf"], ["VAones"])
                for g in range(3):
                    c0g = 416 + 768 * g
                    gtag = tag + "g%d" % g
                    DMA(P, "pool", WGq, win_v[:, :, c0g:c0g + 256], "wgq", gtag, [], ["WGq"])
                    DMA(P, "pool", WGkv, win_v[:, :, c0g + 256:c0g + 768], "wgkv", gtag, [], ["WGkv"])
                    for i in range(NT):
                        b = slot("pj", 2)
                        pv = ps(b)
                        proj(i, WGkv, 0, 512, pv, "WGkv", b)
                        sl = slot("KB", 2)
                        rope(pv[:, 0:256].rearrange("p (h c) -> p h c", h=4), KB[sl].rearrange("p (h c) -> p h c", h=4),
                             4, 64, 8, ropep, i, [("ps", b), "ropep"], [("KB", sl)], rtmp, "rt")
                        CP(P, "dve", VA[:, i, :, 0:64], pv[:, 256:512].rearrange("p (h c) -> p h c", h=4),
                           [("ps", b)], [("VA", i)])
                        transposes_to(KB[sl], 4, 64, KT[0:64, i, :, :], [("KB", sl)], [("KT", i)], rows=64)

                    def dil_qprep(i):
                        sl = slot("dlq", 2)
                        b = slot("pj", 2)
                        pv = ps(b)
                        proj(i, WGq, 0, 256, pv[:, 0:256], "WGq", b)
                        rope(pv[:, 0:256].rearrange("p (h c) -> p h c", h=4), QB[sl].rearrange("p (h c) -> p h c", h=4),
                             4, 64, 8, ropep, i, [("ps", b), "ropep"], [("QBd", sl)], rtmp, "rt")
                        transposes_to(QB[sl], 4, 64, QTi[sl][0:64, :, :], [("QBd", sl)], [("qTd", sl)], rows=64)
                        return sl

                    nxt = dil_qprep(0)
                    for i in range(NT):
                        sl = nxt
                        if g == 0:
                            pl = [(i - 1, M_PREV), (i, M_CAUS)]
                        elif g == 1:
                            pl = [(i - 4, M_R4F), (i - 3, M_R4M), (i - 2, M_R4M), (i - 1, M_R4M), (i, M_R4D)]
                        else:
                            pl = [(j, M_R16O) for j in range(i)] + [(i, M_R16D)]
                        pairs = [(j, masks[:, m, :], "masks") for (j, m) in pl if j >= 0]

                        def qk_fn(j, sl=sl):
                            return [(hh, 1, KT[0:64, j, hh, :], QTi[sl][0:64, hh, :], [("KT", j), ("qTd", sl)])
                                    for hh in range(4)]

                        def v_fn(hh, j):
                            return VA[:, j, hh, 0:65], [("VA", j), "VAones"]

                        if i + 1 < NT:
                            nxt = dil_qprep(i + 1)
                        Ov, ob = attn_tile(i, pairs, qk_fn, v_fn, 0.125, Pt)
                        if g == 0:
                            CP(P, "dve", ACC[:, i, :, :], Ov, [("ps", ob)], [("ACC", i)])
                        else:
                            TT(P, "dve", ACC[:, i, :, :], ACC[:, i, :, :], Ov, ALU.add, [("ps", ob), ("ACC", i)], [("ACC", i)])
                        if g == 2:
                            osl = slot("OBb", 2)
                            rsl = slot("rc", 2)
                            rc = tl["rc"][rsl]
                            RECIP(P, rc[:, 0:4], ACC[:, i, :, 64], [("ACC", i)], [("rc", rsl)])
                            TT(P, "dve", OBb[osl], ACC[:, i, :, 0:64], rc[:, 0:4].unsqueeze(2).to_broadcast([128, 4, 64]),
                               ALU.mult, [("ACC", i), ("rc", rsl)], [("OBb", osl)])
                            transposes_to(OBb[osl].rearrange("p h c -> p (h c)"), 2, 128, OTb[:, :, i * 128:(i + 1) * 128],
                                          [("OBb", osl)], [("OTb", i)])
                if "d_ob" in dbg_t and sq == 0 and l == 0:
                    DMA(P, "pool", dbg_t["d_ob"].rearrange("p (k t) -> p k t", k=2), OTb, "dbg", ("dbg", len(P.ops)),
                        [("OTb", i) for i in range(NT)], ["dbgout"])
                P.barrier()
                A.release(m0)
                chk("dil")

                OTa = A.alloc(BF16, 4, S)
                m0 = A.mark()
                CQK = A.alloc(BF16, 3, S)
                KRB = A.alloc(BF16, NT, 32)
                m1 = A.mark()
                W1 = A.alloc(BF16, KC, 416)
                gq = A.alloc(F32, 256)
                gkv = A.alloc(F32, 128)
                CQB = [A.alloc(BF16, 384) for _ in range(2)]
                junk = A.alloc(F32, 256)
                st = A.alloc(F32, 8)
                rtmp = [A.alloc(F32, 256) for _ in range(2)]
                DMA(P, "pool", W1, win_v[:, :, 0:416], "w1", tag, [], ["W1"])
                DMA(P, "sp", gq, q_norm_g[l].partition_broadcast(128), "gq", tag, [], ["gq"])
                DMA(P, "sp", gkv, kv_norm_g[l].partition_broadcast(128), "gq", tag, [], ["gkv"])
                for i in range(NT):
                    b = slot("pj", 2)
                    pv = ps(b)
                    proj(i, W1, 0, 416, pv[:, 0:416], "W1", b)
                    sl = slot("CQB", 2)
                    cqb = CQB[sl]
                    for (a0, a1, gg, gk, so) in ((0, 256, gq, "gq", 0), (256, 384, gkv, "gkv", 3)):
                        n_ = a1 - a0
                        ACTV(P, junk[:, 0:n_], pv[:, a0:a1], AF.Square, [("ps", b)], ["mjunk", "mst%d" % so],
                             accum=st[:, so:so + 1])
                        ACTV(P, st[:, so + 1:so + 2], st[:, so:so + 1], AF.Sqrt, ["mst%d" % so], ["mst%d" % (so + 1)],
                             bias=EPS, scale=1.0 / n_)
                        RECIP(P, st[:, so + 2:so + 3], st[:, so + 1:so + 2], ["mst%d" % (so + 1)], ["mst%d" % (so + 2)])
                        STT(P, "dve", cqb[:, a0:a1], pv[:, a0:a1], st[:, so + 2:so + 3], gg, ALU.mult, ALU.mult,
                            [("ps", b), "mst%d" % (so + 2), gk], [("CQB", sl)])
                    rope(pv[:, 384:416].unsqueeze(1), KRB[:, i, :].unsqueeze(1), 1, 32, 16, ropem, i,
                         [("ps", b), "ropem"], [("KRB", i)], rtmp, "rt")
                    transposes_to(cqb, 3, 128, CQK[:, :, i * 128:(i + 1) * 128], [("CQB", sl)], [("CQK", i)])
                P.barrier()
                A.release(m1)
                WUQ = A.alloc(BF16, 2, 768)
                WUKV = A.alloc(BF16, 1024)
                KT = A.alloc(BF16, NT, 4, 128)
                VA = A.alloc(BF16, NT, 4, 72)
                QTi = [A.alloc(BF16, 4, 128) for _ in range(2)]
                QH = [A.alloc(BF16, 4, 96) for _ in range(2)]
                KH = [A.alloc(BF16, 4, 96) for _ in range(2)]
                Pt = [A.alloc(BF16, 4, 128) for _ in range(3)]
                rtmp = [A.alloc(F32, 256) for _ in range(2)]
                tl = {"rc": [A.alloc(F32, 8) for _ in range(2)]}
                OAb = [A.alloc(BF16, 4, 64) for _ in range(2)]
                DMA(P, "pool", WUQ, w_uq[l].rearrange("(kc p) c -> p kc c", p=128), "wuq", tag, [], ["WUQ"])
                DMA(P, "pool", WUKV, w_ukv[l], "wuq", tag, [], ["WUKV"])
                CP(P, "dve", VA[:, :, :, 64:65], onesf[:, 0:64].rearrange("p (a b) -> p a b", a=NT).unsqueeze(3),
                   ["onesf"], ["VAones"])
                for u in range(2):
                    for i in range(NT):
                        b = slot("pj", 2)
                        pv = ps(b)
                        MM(P, pv, CQK[:, 2, i * 128:(i + 1) * 128], WUKV[:, 512 * u:512 * u + 512], True, True,
                           [("CQK", i), "WUKV"], [("ps", b)])
                        pv4 = pv.rearrange("p (h c) -> p h c", h=4)
                        sl = slot("KH", 2)
                        kh = KH[sl]
                        CP(P, "dve", kh[:, :, 0:64], pv4[:, :, 0:64], [("ps", b)], [("KH", sl)])
                        CP(P, "dve", kh[:, :, 64:96], KRB[:, i, :].unsqueeze(1).to_broadcast([128, 4, 32]),
                           [("KRB", i)], [("KH", sl)])
                        CP(P, "dve", VA[:, i, :, 0:64], pv4[:, :, 64:128], [("ps", b)], [("VA", i)])
                        transposes_to(kh.rearrange("p h c -> p (h c)"), 4, 96, KT[0:96, i, :, :], [("KH", sl)],
                                      [("KT", i)], rows=96)

                    def mla_qprep(i, u=u):
                        sl = slot("mq", 2)
                        b = slot("pj", 2)
                        pv = ps(b)
                        for kc in range(2):
                            MM(P, pv[:, 0:384], CQK[:, kc, i * 128:(i + 1) * 128], WUQ[:, kc, 384 * u:384 * u + 384],
                               kc == 0, kc == 1, [("CQK", i), "WUQ"], [("ps", b)])
                        pv4 = pv[:, 0:384].rearrange("p (h c) -> p h c", h=4)
                        qh = QH[sl]
                        CP(P, "dve", qh[:, :, 0:64], pv4[:, :, 0:64], [("ps", b)], [("QH", sl)])
                        rope(pv4[:, :, 64:96], qh[:, :, 64:96], 4, 32, 16, ropem, i, [("ps", b), "ropem"], [("QH", sl)],
                             rtmp, "rt")
                        transposes_to(qh.rearrange("p h c -> p (h c)"), 4, 96, QTi[sl][0:96, :, :], [("QH", sl)],
                                      [("qTm", sl)], rows=96)
                        return sl

                    nxt = mla_qprep(0)
                    for i in range(NT):
                        sl = nxt
                        pairs = [(j, None, None) for j in range(i)] + [(i, masks[:, M_CAUS, :], "masks")]

                        def qk_fn(j, sl=sl):
                            return [(hh, 1, KT[0:96, j, hh, :], QTi[sl][0:96, hh, :], [("KT", j), ("qTm", sl)])
                                    for hh in range(4)]

                        def v_fn(hh, j):
                            return VA[:, j, hh, 0:65], [("VA", j), "VAones"]

                        if i + 1 < NT:
                            nxt = mla_qprep(i + 1)
                        Ov, ob = attn_tile(i, pairs, qk_fn, v_fn, float(96 ** -0.5), Pt)
                        osl = slot("OAb", 2)
                        rsl = slot("rc", 2)
                        rc = tl["rc"][rsl]
                        RECIP(P, rc[:, 0:4], Ov[:, :, 64], [("ps", ob)], [("rc", rsl)])
                        TT(P, "dve", OAb[osl], Ov[:, :, 0:64], rc[:, 0:4].unsqueeze(2).to_broadcast([128, 4, 64]), ALU.mult,
                           [("ps", ob), ("rc", rsl)], [("OAb", osl)])
                        transposes_to(OAb[osl].rearrange("p h c -> p (h c)"), 2, 128,
                                      OTa[:, 2 * u:2 * u + 2, i * 128:(i + 1) * 128], [("OAb", osl)], [("OTa", i, u)])
                if "d_oa" in dbg_t and sq == 0 and l == 0:
                    DMA(P, "pool", dbg_t["d_oa"].rearrange("p (k t) -> p k t", k=4), OTa, "dbg", ("dbg", len(P.ops)),
                        [("OTa", i, u) for i in range(NT) for u in range(2)], ["dbgout"])
                P.barrier()
                A.release(m0)
                chk("mla")

                MT = A.alloc(BF16, KC, S)
                m0 = A.mark()
                WGc = A.alloc(BF16, KC, 3, 256)
                WAc = A.alloc(BF16, 4, 256)
                WBc = A.alloc(BF16, 2, 256)
                WCc = A.alloc(BF16, 4, 256)
                bgc = A.alloc(F32, 3, 256)
                G = [A.alloc(F32, 768) for _ in range(2)]
                Mf = [A.alloc(F32, 256) for _ in range(2)]
                MB = [A.alloc(BF16, 256) for _ in range(2)]
                wgate_v = w_gate[l].rearrange("(kc p) c -> p kc c", p=128)
                wa_v = w_a[l].rearrange("(kc p) c -> p kc c", p=128)
                wb_v = w_b[l].rearrange("(kc p) c -> p kc c", p=128)
                wc_v = w_c[l].rearrange("(kc p) c -> p kc c", p=128)
                for c in range(4):
                    ctag = tag + "c%d" % c
                    for m in range(3):
                        DMA(P, "pool", WGc[:, :, m, :], wgate_v[:, :, m * 1024 + 256 * c:m * 1024 + 256 * c + 256],
                            "wgc", ctag, [], ["WGc"])
                        DMA(P, "sp", bgc[0:1, m, :], b_gate[l, m * 1024 + 256 * c:m * 1024 + 256 * c + 256].unsqueeze(0),
                            "bgc", ctag, [], ["bgc"])
                    DMA(P, "pool", WAc, wa_v[:, :, 256 * c:256 * c + 256], "wgc", ctag, [], ["WAc"])
                    DMA(P, "pool", WBc, wb_v[:, :, 256 * c:256 * c + 256], "wgc", ctag, [], ["WBc"])
                    DMA(P, "pool", WCc, wc_v[:, :, 256 * c:256 * c + 256], "wgc", ctag, [], ["WCc"])
                    for i in range(NT):
                        tsl = slice(i * 128, (i + 1) * 128)
                        gb0, gb1 = 0, 1
                        for m in range(3):
                            gb = gb0 if m < 2 else gb1
                            go = ps(gb)[:, (m % 2) * 256:(m % 2) * 256 + 256]
                            for kc in range(KC):
                                MM(P, go, XT[:, kc, tsl], WGc[:, kc, m, :], kc == 0, False, [("XT", i), "WGc"], [("ps", gb)])
                            MM(P, go, onesf[0:1, 0:128], bgc[0:1, m, :], False, True, ["onesf", "bgc"], [("ps", gb)])
                        gs = slot("G", 2)
                        ACTV(P, G[gs][:, 0:512], ps(gb0), AF.Sigmoid, [("ps", gb0)], [("G", gs)])
                        ACTV(P, G[gs][:, 512:768], ps(gb1)[:, 0:256], AF.Sigmoid, [("ps", gb1)], [("G", gs)])
                        bb0, bb1 = 2, 3
                        for kc in range(4):
                            MM(P, ps(bb0)[:, 0:256], OTa[:, kc, tsl], WAc[:, kc, :], kc == 0, kc == 3,
                               [("OTa", i, 0), ("OTa", i, 1), "WAc"], [("ps", bb0)])
                        for kc in range(2):
                            MM(P, ps(bb0)[:, 256:512], OTb[:, kc, tsl], WBc[:, kc, :], kc == 0, kc == 1,
                               [("OTb", i), "WBc"], [("ps", bb0)])
                        for kc in range(4):
                            MM(P, ps(bb1)[:, 0:256], OTc[:, kc, tsl], WCc[:, kc, :], kc == 0, kc == 3,
                               [("OTc", i), "WCc"], [("ps", bb1)])
                        ms = slot("Mf", 2)
                        TT(P, "dve", G[gs][:, 0:512], G[gs][:, 0:512], ps(bb0), ALU.mult, [("G", gs), ("ps", bb0)], [("G", gs)])
                        TT(P, "dve", G[gs][:, 512:768], G[gs][:, 512:768], ps(bb1)[:, 0:256], ALU.mult,
                           [("G", gs), ("ps", bb1)], [("G", gs)])
                        TT(P, "dve", Mf[ms], G[gs][:, 0:256], G[gs][:, 256:512], ALU.add, [("G", gs)], [("Mf", ms)])
                        TT(P, "dve", MB[ms], Mf[ms], G[gs][:, 512:768], ALU.add, [("G", gs), ("Mf", ms)], [("MB", ms)])
                        transposes_to(MB[ms], 2, 128, MT[:, 2 * c:2 * c + 2, tsl], [("MB", ms)], [("MT", i, c)])
                P.barrier()
                A.release(m0)
                chk("merge1")

                off_c = A.mark() - 8 * S - 4 * S - 2 * S - 4 * S
                WO, e_ = A.view(off_c, BF16, KC, D)
                off_b = off_c + 4 * S
                stv, e_ = A.view(off_b, F32, 8)
                off_a = off_b + 2 * S
                L1G, e_ = A.view(off_a, F32, D)
                L1B, e_ = A.view(e_, F32, D)
                junkL, e_ = A.view(e_, F32, D)
                xb0, e_ = A.view(e_, BF16, D)
                xb1, e_ = A.view(e_, BF16, D)
                assert e_ <= off_a + 4 * S
                DMA(P, "pool", WO, w_o[l].rearrange("(kc p) c -> p kc c", p=128), "wo", tag, [], ["WO"])
                DMA(P, "sp", L1G, ln1_g[l].partition_broadcast(128), "ln1", tag, [], ["L1"])
                DMA(P, "sp", L1B, ln1_b[l].partition_broadcast(128), "ln1", tag, [], ["L1"])
                tlL = {"st": stv, "junk": junkL}
                for i in range(NT):
                    tsl = slice(i * 128, (i + 1) * 128)
                    for ch in range(2):
                        yb = 2 * (i % 2) + ch
                        for kc in range(KC):
                            MM(P, ps(yb), MT[:, kc, tsl], WO[:, kc, ch * 512:(ch + 1) * 512], kc == 0, kc == KC - 1,
                               [("MT", i, c) for c in range(4)] + ["WO"], [("ps", yb)])
                        STT(P, "dve", BIG[:, i, ch * 512:(ch + 1) * 512], BIG[:, i, ch * 512:(ch + 1) * 512], ALPHA, ps(yb),
                            ALU.mult, ALU.add, [("BIG", i), ("ps", yb)], [("BIG", i)])
                    layer_norm(i, L1G, L1B, "L1", tlL)
                    if "d_x1" in dbg_t and sq == 0 and l == 0:
                        DMA(P, "sp", dbg_t["d_x1"][i * 128:(i + 1) * 128, :], BIG[:, i, :], "dbg", ("dbg", len(P.ops)), [("BIG", i)], ["dbgout"])
                    to_XT(i, [xb0, xb1])
                P.barrier()
                A.release(m_layer)
                chk("merge2")

                m0 = A.mark()
                WR = A.alloc(F32, KC, 36)
                BR = A.alloc(F32, 36)
                X32T = A.alloc(F32, KC, 128)
                COMB = A.alloc(F32, NT, 32)
                CT = A.alloc(F32, S)
                LG = A.alloc(F32, 36)
                rr = A.alloc(F32, 16)
                e4 = A.alloc(F32, 4)
                gm = A.alloc(F32, 4)
                pen = A.alloc(F32, 4)
                subm = A.alloc(F32, 32)
                subm2 = A.alloc(F32, 32)
                oh1 = A.alloc(F32, 32)
                oh2 = A.alloc(F32, 32)
                W13 = [A.alloc(BF16, KC, 2, 2, 256) for _ in range(2)]
                W2s = [A.alloc(BF16, 2, 2, D) for _ in range(2)]
                HC = [A.alloc(BF16, 4, 512) for _ in range(2)]
                CB = [A.alloc(F32, 512) for _ in range(2)]
                SL = [A.alloc(F32, 512) for _ in range(2)]
                T1 = [A.alloc(F32, 512) for _ in range(2)]
                L2G = A.alloc(F32, D)
                L2B = A.alloc(F32, D)
                junkM = A.alloc(F32, D)
                stM = A.alloc(F32, 8)
                xbm = [A.alloc(BF16, D) for _ in range(2)]
                DMA(P, "sp", WR[:, :, 0:4], w_group[l].rearrange("(kc p) c -> p kc c", p=128), "wr", tag, [], ["WR"])
                DMA(P, "sp", WR[:, :, 4:36], w_sub[l].rearrange("(kc p) c -> p kc c", p=128), "wr", tag, [], ["WR"])
                DMA(P, "sp", BR[:, 0:4], b_group[l].partition_broadcast(128), "wr", tag, [], ["BR"])
                DMA(P, "sp", BR[:, 4:36], b_sub[l].partition_broadcast(128), "wr", tag, [], ["BR"])
                DMA(P, "sp", L2G, ln2_g[l].partition_broadcast(128), "ln2", tag, [], ["L2"])
                DMA(P, "sp", L2B, ln2_b[l].partition_broadcast(128), "ln2", tag, [], ["L2"])

                def load_pair(q):
                    s_ = q % 2
                    for e2 in range(2):
                        e_id = 2 * q + e2
                        DMA(P, "pool", W13[s_][:, :, e2, 0, :], w1[l, e_id].rearrange("(kc p) f -> p kc f", p=128),
                            "w13_%d" % s_, (tag, q), [], [("W13", s_)])
                        DMA(P, "pool", W13[s_][:, :, e2, 1, :], w3[l, e_id].rearrange("(kc p) f -> p kc f", p=128),
                            "w13_%d" % s_, (tag, q), [], [("W13", s_)])
                        DMA(P, "pool", W2s[s_][:, e2, :, :], w2[l, e_id].rearrange("(fc p) c -> p fc c", p=128),
                            "w2_%d" % s_, (tag, q), [], [("W2", s_)])

                load_pair(0)
                for i in range(NT):
                    kB = ("BIG", i)
                    for half in range(2):
                        tb = 6 + half
                        pvf = ps(tb).rearrange("p (k t) -> p k t", k=4)
                        for kk in range(4):
                            kc = 4 * half + kk
                            TR(P, pvf[:, kk, :], BIG[:, i, kc * 128:(kc + 1) * 128], identf, [kB, "identf"], [("ps", tb)])
                        CP(P, "act", X32T[:, 4 * half:4 * half + 4, :], pvf, [("ps", tb)], [("X32T", half)])
                    lb = 5
                    lg = ps(lb)[:, 0:36]
                    for kc in range(KC):
                        MM(P, lg, X32T[:, kc, :], WR[:, kc, :], kc == 0, kc == KC - 1,
                           [("X32T", 0), ("X32T", 1), "WR"], [("ps", lb)])
                    TT(P, "dve", LG, lg, BR, ALU.add, [("ps", lb), "BR"], ["LG"])
                    RED(P, rr[:, 0:1], LG[:, 0:4], ALU.max, ["LG"], ["rr0"])
                    TS(P, "dve", rr[:, 1:2], rr[:, 0:1], -1.0, None, ALU.mult, None, ["rr0"], ["rr1"])
                    ACTV(P, e4, LG[:, 0:4], AF.Exp, ["LG", "rr1"], ["e4", "rr2"], bias=rr[:, 1:2], accum=rr[:, 2:3])
                    RECIP(P, rr[:, 3:4], rr[:, 2:3], ["rr2"], ["rr3"])
                    TS(P, "dve", gm, LG[:, 0:4], rr[:, 0:1], None, ALU.is_ge, None, ["LG", "rr0"], ["gm"])
                    TS(P, "dve", pen, gm, 1.0e30, -1.0e30, ALU.mult, ALU.add, ["gm"], ["pen"])
                    TT(P, "dve", subm.rearrange("p (g e) -> p g e", g=4), LG[:, 4:36].rearrange("p (g e) -> p g e", g=4),
                       pen.unsqueeze(2).to_broadcast([128, 4, 8]), ALU.add, ["LG", "pen"], ["subm"])
                    RED(P, rr[:, 4:5], subm, ALU.max, ["subm"], ["rr4"])
                    TS(P, "dve", oh1, subm, rr[:, 4:5], None, ALU.is_ge, None, ["subm", "rr4"], ["oh1"])
                    STT(P, "dve", subm2, oh1, -1.0e30, subm, ALU.mult, ALU.add, ["oh1", "subm"], ["subm2"])
                    RED(P, rr[:, 5:6], subm2, ALU.max, ["subm2"], ["rr5"])
                    TS(P, "dve", oh2, subm2, rr[:, 5:6], None, ALU.is_ge, None, ["subm2", "rr5"], ["oh2"])
                    TT(P, "dve", rr[:, 6:7], rr[:, 5:6], rr[:, 4:5], ALU.subtract, ["rr4", "rr5"], ["rr6"])
                    ACTV(P, rr[:, 7:8], rr[:, 6:7], AF.Exp, ["rr6"], ["rr7"])
                    TS(P, "dve", rr[:, 8:9], rr[:, 7:8], 1.0, None, ALU.add, None, ["rr7"], ["rr8"])
                    RECIP(P, rr[:, 9:10], rr[:, 8:9], ["rr8"], ["rr9"])
                    TT(P, "dve", rr[:, 10:11], rr[:, 9:10], rr[:, 3:4], ALU.mult, ["rr9", "rr3"], ["rr10"])
                    TT(P, "dve", rr[:, 11:12], rr[:, 10:11], rr[:, 7:8], ALU.mult, ["rr10", "rr7"], ["rr11"])
                    TS(P, "dve", COMB[:, i, :], oh1, rr[:, 10:11], None, ALU.mult, None, ["oh1", "rr10"], [("COMB", i)])
                    STT(P, "dve", COMB[:, i, :], oh2, rr[:, 11:12], COMB[:, i, :], ALU.mult, ALU.add,
                        ["oh2", "rr11", ("COMB", i)], [("COMB", i)])
                    tb = 6
                    TR(P, ps(tb)[0:32, 0:128], COMB[:, i, :], identf, [("COMB", i), "identf"], [("ps", tb)])
                    CP(P, "act", CT[0:32, i * 128:(i + 1) * 128], ps(tb)[0:32, 0:128], [("ps", tb)], [("CT", i // 4)])
                    TS(P, "dve", BIG[:, i, :], BIG[:, i, :], ALPHA, None, ALU.mult, None, [kB], [kB])
                if "d_comb" in dbg_t and sq == 0 and l == 0:
                    DMA(P, "sp", dbg_t["d_comb"].rearrange("p (i e) -> p i e", i=NT), COMB, "dbg", ("dbg", len(P.ops)),
                        [("COMB", i) for i in range(NT)], ["dbgout"])
                for q in range(16):
                    s_ = q % 2
                    if q + 1 < 16:
                        load_pair(q + 1)
                    for tc in range(4):
                        csl = slice(512 * tc, 512 * tc + 512)
                        hs = slot("HC", 2)
                        for e2 in range(2):
                            e_id = 2 * q + e2
                            MM(P, ps(4), identf[0:32, e_id:e_id + 1].to_broadcast([32, 128]), CT[0:32, csl], True, True,
                               [("CT", tc), "identf"], [("ps", 4)])
                            CP(P, "act", CB[e2], ps(4), [("ps", 4)], [("CB", e2)])
                        for e2 in range(2):
                            for fc in range(2):
                                hb = 2 * slot("H", 2)
                                for wi in range(2):
                                    for kc in range(KC):
                                        MM(P, ps(hb + wi), W13[s_][:, kc, e2, wi, 128 * fc:128 * fc + 128], XT[:, kc, csl],
                                           kc == 0, kc == KC - 1,
                                           [("W13", s_)] + [("XT", 4 * tc + t_) for t_ in range(4)], [("ps", hb + wi)])
                                ks = slot("SL", 2)
                                ACTV(P, SL[ks], ps(hb), AF.Silu, [("ps", hb)], [("SL", ks)])
                                TT(P, "dve", T1[ks], SL[ks], ps(hb + 1), ALU.mult, [("SL", ks), ("ps", hb + 1)], [("T1", ks)])
                                TT(P, "dve", HC[hs][:, 2 * e2 + fc, :], T1[ks], CB[e2], ALU.mult,
                                   [("T1", ks), ("CB", e2)], [("HC", hs)])
                        for t_ in range(4):
                            ti = 4 * tc + t_
                            for ch in range(2):
                                yb = 5 + slot("Yb", 3)
                                for fcc in range(4):
                                    MM(P, ps(yb), HC[hs][:, fcc, 128 * t_:128 * t_ + 128],
                                       W2s[s_][:, fcc // 2, fcc % 2, 512 * ch:512 * ch + 512], fcc == 0, fcc == 3,
                                       [("HC", hs), ("W2", s_)], [("ps", yb)])
                                TT(P, "dve", BIG[:, ti, 512 * ch:512 * ch + 512], BIG[:, ti, 512 * ch:512 * ch + 512], ps(yb),
                                   ALU.add, [("BIG", ti), ("ps", yb)], [("BIG", ti)])
                tlM = {"st": stM, "junk": junkM}
                for i in range(NT):
                    layer_norm(i, L2G, L2B, "L2", tlM)
                    if l == nlayers - 1:
                        DMA(P, "sp", out[sq, i * 128:(i + 1) * 128, :], BIG[:, i, :], "out", sq, [("BIG", i)], ["OUT"])
                    else:
                        to_XT(i, xbm)
                P.barrier()
                A.release(m0)
                chk("moe")

        try:
            for sq in range(nseq):
                for l in range(nlayers):
                    seq_layer(sq, l)
        except Stop:
            pass

        import os
        lim = int(os.environ.get("OPLIM", "0"))
        if lim:
            P.ops = P.ops[:lim]
            P.last_eng = {}
            P.last_dma = {}
            for ii, oo in enumerate(P.ops):
                if oo.dma is None:
                    P.last_eng[oo.eng] = ii
                else:
                    P.last_dma[oo.dma[0]] = ii
            P.gen = {}
        P.add("sp", lambda e: e.nop(), ["OUT", "dbgout"], [])
        P.barrier()
        P.add("sp", lambda e: e.nop(), [], [])
        print("arena peak (bf16 elems):", A.peak, "ops:", len(P.ops))
        P.emit(nc, stack)
    return nc


def host_consts():
    ident = np.eye(128, dtype=np.float32)
    s_ = np.arange(128)[:, None]
    t_ = np.arange(128)[None, :]
    caus = (s_ <= t_)
    prev = (s_ >= t_)
    r4 = ((t_ - s_) % 4 == 0)
    r16 = ((t_ - s_) % 16 == 0)
    masks = np.stack([caus, prev, r4 & caus, r4, r4 & prev, r16 & caus, r16], axis=1).astype(np.float32)
    negm = np.where(np.arange(128)[None, :] <= np.arange(128)[:, None], 0.0, NEG).astype(np.float32)
    pos = (np.arange(NT)[None, :] * 128 + np.arange(128)[:, None]).astype(np.float32)

    def tab(rot):
        inv = (500000.0 ** (-np.arange(0, rot, 2, dtype=np.float32) / rot)).astype(np.float32)
        ang = (pos[:, :, None] * inv[None, None, :]).astype(np.float32)
        c = np.cos(ang).astype(np.float32)
        s = np.sin(ang).astype(np.float32)
        return np.concatenate([c, c, -s, s], axis=-1).astype(np.float32)

    return {
        "c_ident": ident,
        "c_masks": np.ascontiguousarray(masks.reshape(128, 7 * 128)),
        "c_negm": negm,
        "c_ropep": np.ascontiguousarray(tab(16).reshape(128, NT * 32)),
        "c_ropem": np.ascontiguousarray(tab(32).reshape(128, NT * 64)),
    }


_NC_CACHE = {}

IMPLEMENTED = True


SEQ_PER_LAUNCH = 1


def kernel(**inputs):
    nseq_total = 32 // NCORES
    npl = SEQ_PER_LAUNCH
    if npl not in _NC_CACHE:
        _NC_CACHE[npl] = build(nseq=npl)
    nc = _NC_CACHE[npl]
    consts = host_consts()
    xs = np.ascontiguousarray(inputs["x"], dtype=np.float32)
    base = {k: np.ascontiguousarray(v, dtype=np.float32) for k, v in inputs.items() if k != "x"}
    base.update(consts)
    outs = [[None] * (nseq_total // npl) for _ in range(NCORES)]
    for r in range(nseq_total // npl):
        in_maps = []
        for c in range(NCORES):
            m = dict(base)
            lo = c * nseq_total + r * npl
            m["x"] = xs[lo:lo + npl]
            in_maps.append(m)
        res = run_bass_kernel_spmd(nc, in_maps, core_ids=list(range(NCORES)))
        for c in range(NCORES):
            outs[c][r] = np.asarray(res.results[c]["out"], dtype=np.float32)
    return np.concatenate([o for c in range(NCORES) for o in outs[c]], axis=0).astype(np.float32)
```

```python
import numpy as np
from contextlib import ExitStack
import concourse.bass as bass
import concourse.mybir as mybir
from concourse.bass_utils import run_bass_kernel_spmd

F32 = mybir.dt.float32
BF16 = mybir.dt.bfloat16
AF = mybir.ActivationFunctionType
ALU = mybir.AluOpType
AX = mybir.AxisListType

S = 2048
NT = 16
D = 1024
KC = 8
DEPTH = 2
NCORES = 8
ALPHA = float((2 * DEPTH) ** 0.25)
EPS = 1e-6
NEG = -1.0e30
ENGS = ("pe", "act", "dve", "pool", "sp")
NBIS = 22
DBG = {"dsa_tiles": NT, "dsa_stage": 4, "att": 9}


class Op:
    __slots__ = ("eng", "fn", "deps", "dma", "need", "sig", "waits")

    def __init__(self, eng, fn, deps, dma):
        self.eng = eng
        self.fn = fn
        self.deps = deps
        self.dma = dma
        self.need = False
        self.sig = None
        self.waits = None


class Prog:
    def __init__(self):
        self.ops = []
        self.gen = {}
        self.fence = {}
        self.last_eng = {}
        self.last_dma = {}

    def add(self, eng, fn, r=(), w=(), dma=None):
        idx = len(self.ops)
        deps = {}
        for k in r:
            if isinstance(k, tuple) and k and k[0] == "ps":
                g = self.gen.get(k)
                if g:
                    for d in g[1]:
                        deps.setdefault(d, False)
        for k in r:
            g = self.gen.get(k)
            if g:
                for d in g[0]:
                    deps[d] = True
        for k in w:
            g = self.gen.get(k)
            if g:
                for d in g[1]:
                    deps.setdefault(d, False)
        for d in self.fence.values():
            deps.setdefault(d, False)
        for k in r:
            self.gen.setdefault(k, [[], []])[1].append(idx)
        for k in w:
            g = self.gen.setdefault(k, [[], []])
            if g[1]:
                g[0] = [idx]
                g[1] = []
            else:
                g[0].append(idx)
        self.ops.append(Op(eng, fn, deps, dma))
        if dma is None:
            self.last_eng[eng] = idx
        else:
            self.last_dma[dma[0]] = idx
        return idx

    def barrier(self):
        f = {}
        for e, i in self.last_eng.items():
            f[("e", e)] = i
        for s, i in self.last_dma.items():
            f[("d", s)] = i
        self.fence = f

    def emit(self, nc, stack):
        ops = self.ops
        for o in ops:
            o.waits = []
            for d, raw in o.deps.items():
                p = ops[d]
                if p.dma is None and p.eng == o.eng and o.dma is None and o.eng == "pe":
                    continue
                o.waits.append(d)
                p.need = True
        dcount = {}
        dround_end = {}
        for o in ops:
            if o.dma is not None:
                s, rd = o.dma
                dcount[s] = dcount.get(s, 0) + 1
                dround_end[(s, rd)] = dcount[s]
        sems = {}

        def getsem(name):
            if name not in sems:
                sems[name] = stack.enter_context(nc.semaphore("s_" + name))
            return sems[name]

        LIM = 24000
        cnt = {e: 0 for e in ENGS}
        for o in ops:
            if o.dma is not None:
                s, rd = o.dma
                o.sig = (getsem("d_" + s), 16 * dround_end[(s, rd)])
            elif o.need:
                c = cnt[o.eng]
                ep = c // LIM
                o.sig = (getsem("%s%d" % (o.eng, ep)), c % LIM + 1)
                cnt[o.eng] = c + 1
        per = {e: [] for e in ENGS}
        for o in ops:
            per[o.eng].append(o)

        def run(e, lst):
            waited = {}
            for o in lst:
                for d in o.waits:
                    sem, val = ops[d].sig
                    key = id(sem)
                    if waited.get(key, 0) >= val:
                        continue
                    waited[key] = val
                    e.wait_ge(sem, val)
                ins = o.fn(e)
                if o.dma is not None:
                    ins.then_inc(o.sig[0], 16)
                elif o.need:
                    ins.then_inc(o.sig[0], 1)

        with nc.Block() as block:
            @block.tensor
            def _(e):
                run(e, per["pe"])

            @block.scalar
            def _(e):
                run(e, per["act"])

            @block.vector
            def _(e):
                run(e, per["dve"])

            @block.gpsimd
            def _(e):
                run(e, per["pool"])

            @block.sync
            def _(e):
                run(e, per["sp"])


class Arena:
    def __init__(self, tens, n):
        self.t = tens
        self.n = n
        self.top = 0
        self.peak = 0

    def alloc(self, dtype, *shape):
        n = 1
        for s in shape:
            n *= s
        size = n * (2 if dtype == F32 else 1)
        size = (size + 31) // 32 * 32
        off = self.top
        self.top += size
        self.peak = max(self.peak, self.top)
        assert self.top <= self.n, ("arena overflow", self.top, self.n)
        ap = self.t[:, off:off + (n * 2 if dtype == F32 else n)]
        if dtype == F32:
            ap = ap.bitcast(F32)
        if len(shape) > 1:
            names = "abcdefg"[: len(shape)]
            pat = "p (%s) -> p %s" % (" ".join(names), " ".join(names))
            kw = {names[i]: shape[i] for i in range(len(shape))}
            ap = ap.rearrange(pat, **kw)
        return ap

    def view(self, off, dtype, *shape):
        save = self.top
        self.top = off
        ap = self.alloc(dtype, *shape)
        end = self.top
        self.top = save
        return ap, end

    def mark(self):
        return self.top

    def release(self, m):
        self.top = m


class K:
    pass


def MM(P, out, lhsT, rhs, start, stop, r, w, skip=False):
    if skip:
        P.add("pe", lambda e: e.matmul(out, lhsT=lhsT, rhs=rhs, start=start, stop=stop, skip_group_check=True), r, w)
    else:
        P.add("pe", lambda e: e.matmul(out, lhsT=lhsT, rhs=rhs, start=start, stop=stop), r, w)


def TR(P, out, in_, ident, r, w):
    P.add("pe", lambda e: e.transpose(out, in_, ident), r, w)


def ACTV(P, out, in_, func, r, w, bias=None, scale=None, accum=None):
    kw = {}
    if bias is not None:
        kw["bias"] = bias
    if scale is not None:
        kw["scale"] = scale
    if accum is not None:
        kw["accum_out"] = accum
    P.add("act", lambda e: e.activation(out=out, in_=in_, func=func, **kw), r, w)


def TT(P, eng, out, in0, in1, op, r, w):
    P.add(eng, lambda e: e.tensor_tensor(out=out, in0=in0, in1=in1, op=op), r, w)


def TS(P, eng, out, in0, s1, s2, op0, op1, r, w, accum=None):
    if op1 is None:
        P.add(eng, lambda e: e.tensor_scalar(out=out, in0=in0, scalar1=s1, scalar2=0.0, op0=op0, op1=ALU.add), r, w)
    elif accum is None:
        P.add(eng, lambda e: e.tensor_scalar(out=out, in0=in0, scalar1=s1, scalar2=s2, op0=op0, op1=op1), r, w)
    else:
        P.add(eng, lambda e: e.tensor_scalar(out=out, in0=in0, scalar1=s1, scalar2=s2, op0=op0, op1=op1,
                                             accum_out=accum), r, w)


def STT(P, eng, out, in0, scalar, in1, op0, op1, r, w):
    P.add(eng, lambda e: e.scalar_tensor_tensor(out=out, in0=in0, scalar=scalar, in1=in1, op0=op0, op1=op1), r, w)


def CP(P, eng, out, in_, r, w):
    if eng == "act":
        P.add("act", lambda e: e.copy(out=out, in_=in_), r, w)
    else:
        P.add(eng, lambda e: e.tensor_copy(out=out, in_=in_), r, w)


def RED(P, out, in_, op, r, w, absv=False):
    if absv:
        P.add("dve", lambda e: e.tensor_reduce(out=out, in_=in_, axis=AX.X, op=op, apply_absolute_value=True), r, w)
    else:
        P.add("dve", lambda e: e.tensor_reduce(out=out, in_=in_, axis=AX.X, op=op), r, w)


def RECIP(P, out, in_, r, w):
    P.add("dve", lambda e: e.reciprocal(out=out, in_=in_), r, w)


def MSET(P, eng, ap, val, r, w):
    P.add(eng, lambda e: e.memset(ap, val), r, w)


CUR = {"sq": 0}


def DMA(P, q, out, in_, stream, rnd, r, w):
    P.add(q, lambda e: e.dma_start(out=out, in_=in_), r, w, dma=("%s_%s_q%d" % (q, stream, CUR["sq"] % 2), rnd))


def build(nseq=4, nlayers=DEPTH, dbg=None, stop_after=None):
    dbg = dbg or set()
    nc = bass.Bass("TRN2", target_bir_lowering=False)
    L = DEPTH

    def din(name, shape):
        return nc.dram_tensor(name, list(shape), F32, kind="ExternalInput").ap()

    x = din("x", (nseq, S, D))
    w_in = din("w_in", (L, D, 3944))
    q_norm_g = din("q_norm_g", (L, 256))
    w_uq = din("w_uq", (L, 256, 768))
    kv_norm_g = din("kv_norm_g", (L, 128))
    w_ukv = din("w_ukv", (L, 128, 1024))
    w_gate = din("w_gate", (L, D, 3072))
    b_gate = din("b_gate", (L, 3072))
    w_a = din("w_a", (L, 512, D))
    w_b = din("w_b", (L, 256, D))
    w_c = din("w_c", (L, 512, D))
    w_o = din("w_o", (L, D, D))
    ln1_g = din("ln1_g", (L, D))
    ln1_b = din("ln1_b", (L, D))
    w_group = din("w_group", (L, D, 4))
    b_group = din("b_group", (L, 4))
    w_sub = din("w_sub", (L, D, 32))
    b_sub = din("b_sub", (L, 32))
    w1 = din("w1", (L, 32, D, 256))
    w3 = din("w3", (L, 32, D, 256))
    w2 = din("w2", (L, 32, 256, D))
    ln2_g = din("ln2_g", (L, D))
    ln2_b = din("ln2_b", (L, D))
    c_ident = din("c_ident", (128, 128))
    c_masks = din("c_masks", (128, 7 * 128))
    c_negm = din("c_negm", (128, 128))
    c_ropep = din("c_ropep", (128, NT * 32))
    c_ropem = din("c_ropem", (128, NT * 64))
    out = nc.dram_tensor("out", [nseq, S, D], F32, kind="ExternalOutput").ap()
    dbg_t = {}
    for name, shape in (("d_oc", (128, 4 * S)), ("d_oa", (128, 4 * S)), ("d_ob", (128, 2 * S)),
                        ("d_x1", (S, D)), ("d_sc", (128, S)), ("d_comb", (128, NT * 32))):
        if name in dbg:
            dbg_t[name] = nc.dram_tensor(name, list(shape), F32, kind="ExternalOutput").ap()

    P = Prog()
    stack = ExitStack()
    with stack:
        ARN = 106000
        arena_t = stack.enter_context(nc.sbuf_tensor("arena", [128, ARN], BF16))
        A = Arena(arena_t, ARN)
        psb = [stack.enter_context(nc.psum_tensor("psb%d" % i, [128, 512], F32)) for i in range(8)]

        def ps(b):
            return psb[b][:, :]

        def psbf(b):
            return psb[b][:, :].bitcast(BF16)

        BIG = A.alloc(F32, NT, D)
        XT = A.alloc(BF16, KC, S)
        identb = A.alloc(BF16, 128)
        identf = A.alloc(F32, 128)
        masks = A.alloc(BF16, 7, 128)
        negm = A.alloc(F32, 128)
        ropep = A.alloc(F32, NT, 32)
        ropem = A.alloc(F32, NT, 64)
        onesf = A.alloc(F32, 128)
        pow2 = A.alloc(F32, NBIS)

        DMA(P, "sp", identf, c_ident, "const", 0, [], ["identf"])
        DMA(P, "pool", identb, c_ident, "constb", 0, [], ["identb"])
        DMA(P, "pool", masks, c_masks.rearrange("p (m t) -> p m t", m=7), "constb", 0, [], ["masks"])
        DMA(P, "sp", negm, c_negm, "const", 0, [], ["negm"])
        DMA(P, "sp", ropep, c_ropep.rearrange("p (i c) -> p i c", i=NT), "const", 0, [], ["ropep"])
        DMA(P, "sp", ropem, c_ropem.rearrange("p (i c) -> p i c", i=NT), "const", 0, [], ["ropem"])
        MSET(P, "dve", onesf, 1.0, [], ["onesf"])
        for k in range(NBIS):
            MSET(P, "dve", pow2[:, k:k + 1], float(2.0 ** -k), [], ["pow2"])

        M_CAUS, M_PREV, M_R4D, M_R4M, M_R4F, M_R16D, M_R16O = range(7)

        rot = {}

        def slot(name, n):
            v = rot.get(name, 0)
            rot[name] = v + 1
            return v % n

        def to_XT(i, xb_tiles):
            sl = slot("xb", 2)
            xb = xb_tiles[sl]
            CP(P, "dve", xb, BIG[:, i, :], [("BIG", i)], [("xb", sl)])
            b = 6 + slot("tp", 2)
            pv = psbf(b).rearrange("p (k t) -> p k t", k=8)
            for kc in range(KC):
                TR(P, pv[:, kc, :], xb[:, kc * 128:(kc + 1) * 128], identb, [("xb", sl), "identb"], [("ps", b)])
            CP(P, "act", XT[:, :, i * 128:(i + 1) * 128], pv, [("ps", b)], [("XT", i)])

        def proj(i, wt, c0, ncols, pout, wkey, b, start=True, stop=True):
            for kc in range(KC):
                MM(P, pout, XT[:, kc, i * 128:(i + 1) * 128], wt[:, kc, c0:c0 + ncols],
                   start and kc == 0, stop and kc == KC - 1, [("XT", i), wkey], [("ps", b)])

        def rope(src, dst, H, hd, half, table, i, rkeys, wkeys, tmp, tkey):
            r2 = 2 * half
            cc = table[:, i, 0:r2].unsqueeze(1).to_broadcast([128, H, r2])
            ns = table[:, i, r2:r2 + half].unsqueeze(1).to_broadcast([128, H, half])
            ps_ = table[:, i, r2 + half:r2 + 2 * half].unsqueeze(1).to_broadcast([128, H, half])
            u = tmp[0][:, 0:H * r2].rearrange("p (h c) -> p h c", h=H)
            v = tmp[1][:, 0:H * r2].rearrange("p (h c) -> p h c", h=H)
            TT(P, "dve", u[:, :, 0:half], src[:, :, half:r2], ns, ALU.mult, rkeys + ["rtu"], ["rtu"])
            TT(P, "dve", u[:, :, half:r2], src[:, :, 0:half], ps_, ALU.mult, rkeys + ["rtu"], ["rtu"])
            TT(P, "dve", v, src[:, :, 0:r2], cc, ALU.mult, rkeys + ["rtv"], ["rtv"])
            TT(P, "dve", dst[:, :, 0:r2], u, v, ALU.add, ["rtu", "rtv"], wkeys)
            if hd > r2:
                CP(P, "dve" if H > 1 else "act", dst[:, :, r2:hd], src[:, :, r2:hd], rkeys, wkeys)

        def layer_norm(i, g_rep, b_rep, gkey, tl):
            xt_ = BIG[:, i, :]
            kB = ("BIG", i)
            st = tl["st"]
            sk = "lnst"
            RED(P, st[:, 0:1], xt_, ALU.add, [kB], [sk + "0"])
            TS(P, "dve", st[:, 1:2], st[:, 0:1], -1.0 / D, None, ALU.mult, None, [sk + "0"], [sk + "1"])
            ACTV(P, xt_, xt_, AF.Identity, [kB, sk + "1"], [kB], bias=st[:, 1:2])
            ACTV(P, tl["junk"], xt_, AF.Square, [kB], ["lnjunk", sk + "2"], accum=st[:, 2:3])
            ACTV(P, st[:, 3:4], st[:, 2:3], AF.Sqrt, [sk + "2"], [sk + "3"], bias=EPS, scale=1.0 / D)
            RECIP(P, st[:, 4:5], st[:, 3:4], [sk + "3"], [sk + "4"])
            STT(P, "dve", xt_, xt_, st[:, 4:5], g_rep, ALU.mult, ALU.mult, [kB, sk + "4", gkey], [kB])
            TT(P, "dve", xt_, xt_, b_rep, ALU.add, [kB, gkey], [kB])

        def attn_tile(i, pairs, qk_fn, v_fn, scale, Ptiles, nh=4):
            ob = 4 + slot("O", 2)
            Ov = ps(ob)[:, 0:nh * 65].rearrange("p (h c) -> p h c", h=nh)
            n = len(pairs)
            for idx, (j, mk, mkey) in enumerate(pairs):
                sb_ = 2 + slot("S", 2)
                Sv = ps(sb_).rearrange("p (h t) -> p h t", h=4)
                for (h0, nhh, lhsT, rhs, rk) in qk_fn(j):
                    MM(P, Sv[:, h0:h0 + nhh, :], lhsT, rhs, True, True, rk, [("ps", sb_)])
                if DBG["att"] < 1:
                    continue
                psl = slot("P", 3)
                Pt = Ptiles[psl]
                ACTV(P, Pt[:, 0:nh, :], Sv[:, 0:nh, :], (AF.Relu if DBG.get("noexp") else AF.Exp), [("ps", sb_)], [("P", psl)], scale=scale)
                if DBG["att"] < 2:
                    continue
                if mk is not None:
                    TT(P, "dve", Pt[:, 0:nh, :], Pt[:, 0:nh, :], mk.unsqueeze(1).to_broadcast([128, nh, 128]),
                       ALU.mult, [("P", psl), mkey], [("P", psl)])
                if DBG["att"] < 3:
                    continue
                for hh in range(nh):
                    rv, rk = v_fn(hh, j)
                    MM(P, Ov[:, hh, :], Pt[:, hh, :], rv, idx == 0 and hh == 0, idx == n - 1, [("P", psl)] + rk,
                       [("ps", ob)], skip=True)
            return Ov, ob

        def normalize_out(Ov, ob, dst, nh, tl):
            sl = slot("rc", 2)
            rc = tl["rc"][sl]
            RECIP(P, rc[:, 0:nh], Ov[:, :, 64], [("ps", ob)], [("rc", sl)])
            TT(P, "dve", dst, Ov[:, :, 0:64], rc[:, 0:nh].unsqueeze(2).to_broadcast([128, nh, 64]), ALU.mult,
               [("ps", ob), ("rc", sl)], ["normdst"])

        def transposes_to(src_bf, nblk, width, dst_ap, rkeys, wkeys, rows=128):
            b = 6 + slot("tp", 2)
            pv = psbf(b).rearrange("p (k t) -> p k t", k=8)
            for k in range(nblk):
                TR(P, pv[0:width, k, :], src_bf[:, k * width:(k + 1) * width], identb, rkeys + ["identb"], [("ps", b)])
            CP(P, "act" if rows == 128 else "dve", dst_ap, pv[0:rows, 0:nblk, :], [("ps", b)], wkeys)

        class Stop(Exception):
            pass

        def chk(name):
            if stop_after == name:
                raise Stop()

        def seq_layer(sq, l):
            CUR["sq"] = sq
            if True:
                tag = "s%dl%d" % (sq, l)
                win_v = w_in[l].rearrange("(kc p) c -> p kc c", p=128)
                m_layer = A.mark()
                if l == 0:
                    m0 = A.mark()
                    xb_tiles = [A.alloc(BF16, D) for _ in range(2)]
                    for i in range(NT):
                        DMA(P, "sp", BIG[:, i, :], x[sq, i * 128:(i + 1) * 128, :], "x", sq, [], [("BIG", i)])
                    for i in range(NT):
                        to_XT(i, xb_tiles)
                    P.barrier()
                    A.release(m0)
                    chk("load")

                OTc = A.alloc(BF16, 4, S)
                m0 = A.mark()
                WQ = A.alloc(BF16, KC, 1024)
                WK = A.alloc(BF16, KC, 200)
                KI = A.alloc(BF16, 2, S)
                VC = A.alloc(BF16, NT, 72)
                WAb = A.alloc(F32, NT, 8)
                SG = A.alloc(F32, NT, 8)
                SC = A.alloc(F32, S)
                MK = A.alloc(BF16, S)
                MKT = A.alloc(BF16, NT, 128)
                RL = [A.alloc(F32, 512) for _ in range(2)]
                QTi = [A.alloc(BF16, 8, 128) for _ in range(2)]
                IQTi = [A.alloc(BF16, 8, 128) for _ in range(2)]
                QB = [A.alloc(BF16, 512) for _ in range(2)]
                IQB = [A.alloc(BF16, 512) for _ in range(2)]
                KK = [A.alloc(BF16, 128) for _ in range(2)]
                Pt = [A.alloc(BF16, 4, 128) for _ in range(3)]
                rtmp = [A.alloc(F32, 256) for _ in range(2)]
                tl = {"rc": [A.alloc(F32, 8) for _ in range(2)]}
                OCb = [A.alloc(BF16, 8, 64) for _ in range(2)]
                bis = A.alloc(F32, 8)
                wtab = A.alloc(F32, NBIS)

                DMA(P, "pool", WK[:, :, 0:128], win_v[:, :, 3232:3360], "wk", tag, [], ["WK"])
                DMA(P, "pool", WK[:, :, 128:200], win_v[:, :, 3872:3944], "wk", tag, [], ["WK"])
                DMA(P, "pool", WQ[:, :, 0:512], win_v[:, :, 2720:3232], "wq", tag, [], ["WQ"])
                DMA(P, "pool", WQ[:, :, 512:1024], win_v[:, :, 3360:3872], "wq", tag, [], ["WQ"])
                CP(P, "dve", VC[:, :, 64:65], onesf[:, 0:NT].unsqueeze(2), ["onesf"], ["VCones"])
                for i in range(NT):
                    b = slot("pj", 2)
                    pv = ps(b)
                    proj(i, WK, 0, 128, pv[:, 0:128], "WK", b)
                    proj(i, WK, 128, 72, pv[:, 128:200], "WK", b)
                    sl = slot("KK", 2)
                    kk = KK[sl]
                    rope(pv[:, 0:64].unsqueeze(1), kk[:, 0:64].unsqueeze(1), 1, 64, 8, ropep, i,
                         [("ps", b), "ropep"], [("KK", sl)], rtmp, "rtA")
                    rope(pv[:, 128:192].unsqueeze(1), kk[:, 64:128].unsqueeze(1), 1, 64, 8, ropep, i,
                         [("ps", b), "ropep"], [("KK", sl)], rtmp, "rtB")
                    CP(P, "dve", VC[:, i, 0:64], pv[:, 64:128], [("ps", b)], [("VC", i)])
                    ACTV(P, WAb[:, i, :], pv[:, 192:200], AF.Abs, [("ps", b)], [("WA", i)])
                    ACTV(P, SG[:, i, :], pv[:, 192:200], AF.Sign, [("ps", b)], [("SG", i)])
                    tb = 6 + slot("tp", 2)
                    tv = psbf(tb).rearrange("p (k t) -> p k t", k=8)
                    TR(P, tv[0:64, 0, :], kk[:, 0:64], identb, [("KK", sl), "identb"], [("ps", tb)])
                    TR(P, tv[0:64, 1, :], kk[:, 64:128], identb, [("KK", sl), "identb"], [("ps", tb)])
                    CP(P, "dve", KI[0:64, :, i * 128:(i + 1) * 128], tv[0:64, 0:2, :], [("ps", tb)], [("KI", i)])

                chk("dsa_pro")

                def dsa_qprep(i):
                    sl = slot("dq", 2)
                    for (c0, dstb, dstT, nm) in ((0, QB[sl], QTi[sl], "q"), (512, IQB[sl], IQTi[sl], "iq")):
                        b = slot("pj", 2)
                        pv = ps(b)
                        proj(i, WQ, c0, 512, pv, "WQ", b)
                        rope(pv.rearrange("p (h c) -> p h c", h=8), dstb.rearrange("p (h c) -> p h c", h=8),
                             8, 64, 8, ropep, i, [("ps", b), "ropep"], [(nm + "B", sl)], rtmp, "rtQ")
                        transposes_to(dstb, 8, 64, dstT[0:64, :, :], [(nm + "B", sl)], [(nm + "T", sl)], rows=64)
                    return sl

                nxt = dsa_qprep(0)
                for i in range(DBG["dsa_tiles"]):
                    sl = nxt
                    hi = (i + 1) * 128
                    for h in range(8):
                        for c0 in range(0, hi, 512):
                            c1 = min(hi, c0 + 512)
                            sb_ = 2 + slot("S", 2)
                            Rv = ps(sb_)[:, 0:c1 - c0]
                            jkeys = [("KI", jj) for jj in range(c0 // 128, c1 // 128)]
                            MM(P, Rv, IQTi[sl][0:64, h, :], KI[0:64, 1, c0:c1], True, True,
                               [("iqT", sl)] + jkeys, [("ps", sb_)])
                            rs = slot("RL", 2)
                            ACTV(P, RL[rs][:, 0:c1 - c0], Rv, AF.Relu, [("ps", sb_), ("WA", i)], [("RL", rs)],
                                 scale=WAb[:, i, h:h + 1])
                            if h == 0:
                                TS(P, "dve", SC[:, c0:c1], RL[rs][:, 0:c1 - c0], SG[:, i, h:h + 1], None, ALU.mult, None,
                                   [("RL", rs), ("SG", i)], [("SC", c0)])
                            else:
                                STT(P, "dve", SC[:, c0:c1], RL[rs][:, 0:c1 - c0], SG[:, i, h:h + 1], SC[:, c0:c1],
                                    ALU.mult, ALU.add, [("RL", rs), ("SG", i), ("SC", c0)], [("SC", c0)])
                    sckeys = [("SC", c0) for c0 in range(0, hi, 512)]
                    if i + 1 < NT:
                        nxt = dsa_qprep(i + 1)
                    if DBG["dsa_stage"] < 2:
                        continue
                    if i >= 2:
                        RED(P, bis[:, 0:1], SC[:, 0:hi], ALU.max, sckeys, ["bisB"], absv=True)
                        TS(P, "dve", bis[:, 0:1], bis[:, 0:1], 1.0, None, ALU.add, None, ["bisB"], ["bisB"])
                        TS(P, "dve", bis[:, 1:2], bis[:, 0:1], -1.0, None, ALU.mult, None, ["bisB"], ["bislo"])
                        TS(P, "dve", wtab, pow2, bis[:, 0:1], None, ALU.mult, None, ["bisB", "pow2"], ["wtab"])
                    TT(P, "dve", SC[:, i * 128:hi], SC[:, i * 128:hi], negm, ALU.add,
                       [("SC", (i * 128) // 512 * 512), "negm"], [("SC", (i * 128) // 512 * 512)])
                    if i >= 2:
                        for k in range(NBIS):
                            TT(P, "dve", bis[:, 2:3], bis[:, 1:2], wtab[:, k:k + 1], ALU.add, ["bislo", "wtab"], ["bismid"])
                            TS(P, "dve", MK[:, 0:hi], SC[:, 0:hi], bis[:, 2:3], 0.0, ALU.is_ge, ALU.add,
                               sckeys + ["bismid"], ["MK", "biscnt"], accum=bis[:, 3:4])
                            TS(P, "dve", bis[:, 4:5], bis[:, 3:4], 256.0, wtab[:, k:k + 1], ALU.is_ge, ALU.mult,
                               ["biscnt", "wtab"], ["bisstep"])
                            TT(P, "dve", bis[:, 1:2], bis[:, 1:2], bis[:, 4:5], ALU.add, ["bislo", "bisstep"], ["bislo"])
                    else:
                        MSET(P, "dve", bis[:, 1:2], -1.0e29, ["bislo"], ["bislo"])
                    TS(P, "dve", MK[:, 0:hi], SC[:, 0:hi], bis[:, 1:2], None, ALU.is_ge, None, sckeys + ["bislo"], ["MK"])
                    if "d_sc" in dbg_t and sq == 0 and l == 0 and i == NT - 1:
                        DMA(P, "sp", dbg_t["d_sc"], SC, "dbg", ("dbg", len(P.ops)), sckeys, ["dbgout"])
                    if DBG["dsa_stage"] < 3:
                        continue
                    for j0 in range(0, i + 1, 8):
                        j1 = min(i + 1, j0 + 8)
                        tb = 6 + slot("tp", 2)
                        tv = psbf(tb).rearrange("p (k t) -> p k t", k=8)
                        for j in range(j0, j1):
                            TR(P, tv[:, j - j0, :], MK[:, j * 128:(j + 1) * 128], identb, ["MK", "identb"], [("ps", tb)])
                        CP(P, "act", MKT[:, j0:j1, :], tv[:, 0:j1 - j0, :], [("ps", tb)], [("MKT", j0)])
                    if DBG["dsa_stage"] < 4:
                        continue
                    osl = slot("OCb", 2)
                    for hg in range(2):
                        def qk_fn(j, hg=hg, sl=sl):
                            return [(0, 4, KI[0:64, 0, j * 128:(j + 1) * 128], QTi[sl][0:64, 4 * hg:4 * hg + 4, :],
                                     [("KI", j), ("qT", sl)])]

                        def v_fn(hh, j):
                            return VC[:, j, 0:65], [("VC", j), "VCones"]

                        pairs = [(j, MKT[:, j, :], ("MKT", j // 8 * 8)) for j in range(i + 1)]
                        Ov, ob = attn_tile(i, pairs, qk_fn, v_fn, 0.125, Pt)
                        if DBG["att"] < 4:
                            continue
                        normalize_out(Ov, ob, OCb[osl][:, 4 * hg:4 * hg + 4, :], 4, tl)
                    if DBG["att"] < 5:
                        continue
                    transposes_to(OCb[osl].rearrange("p h c -> p (h c)"), 4, 128, OTc[:, :, i * 128:(i + 1) * 128],
                                  ["normdst"], [("OTc", i)])
                if "d_oc" in dbg_t and sq == 0 and l == 0:
                    DMA(P, "pool", dbg_t["d_oc"].rearrange("p (k t) -> p k t", k=4), OTc, "dbg", ("dbg", len(P.ops)),
                        [("OTc", i) for i in range(NT)], ["dbgout"])
                P.barrier()
                A.release(m0)
                chk("dsa")


                OTb = A.alloc(BF16, 2, S)
                m0 = A.mark()
                ACC = A.alloc(F32, NT, 4, 65)
                WGq = A.alloc(BF16, KC, 256)
                WGkv = A.alloc(BF16, KC, 512)
                KT = A.alloc(BF16, NT, 4, 128)
                VA = A.alloc(BF16, NT, 4, 72)
                QTi = [A.alloc(BF16, 4, 128) for _ in range(2)]
                QB = [A.alloc(BF16, 256) for _ in range(2)]
                KB = [A.alloc(BF16, 256) for _ in range(2)]
                Pt = [A.alloc(BF16, 4, 128) for _ in range(3)]
                rtmp = [A.alloc(F32, 256) for _ in range(2)]
                tl = {"rc": [A.alloc(F32, 8) for _ in range(2)]}
                OBb = [A.alloc(BF16, 4, 64) for _ in range(2)]
                CP(P, "dve", VA[:, :, :, 64:65], onesf[:, 0:64].rearrange("p (a b) -> p a b", a=NT).unsqueeze(3),
                   ["onesf"], ["VAones"])
                for g in range(3):
                    c0g = 416 + 768 * g
                    gtag = tag + "g%d" % g
                    DMA(P, "pool", WGq, win_v[:, :, c0g:c0g + 256], "wgq", gtag, [], ["WGq"])
                    DMA(P, "pool", WGkv, win_v[:, :, c0g + 256:c0g + 768], "wgkv", gtag, [], ["WGkv"])
                    for i in range(NT):
                        b = slot("pj", 2)
                        pv = ps(b)
                        proj(i, WGkv, 0, 512, pv, "WGkv", b)
                        sl = slot("KB", 2)
                        rope(pv[:, 0:256].rearrange("p (h c) -> p h c", h=4), KB[sl].rearrange("p (h c) -> p h c", h=4),
                             4, 64, 8, ropep, i, [("ps", b), "ropep"], [("KB", sl)], rtmp, "rt")
                        CP(P, "dve", VA[:, i, :, 0:64], pv[:, 256:512].rearrange("p (h c) -> p h c", h=4),
                           [("ps", b)], [("VA", i)])
                        transposes_to(KB[sl], 4, 64, KT[0:64, i, :, :], [("KB", sl)], [("KT", i)], rows=64)

                    def dil_qprep(i):
                        sl = slot("dlq", 2)
                        b = slot("pj", 2)
                        pv = ps(b)
                        proj(i, WGq, 0, 256, pv[:, 0:256], "WGq", b)
                        rope(pv[:, 0:256].rearrange("p (h c) -> p h c", h=4), QB[sl].rearrange("p (h c) -> p h c", h=4),
                             4, 64, 8, ropep, i, [("ps", b), "ropep"], [("QBd", sl)], rtmp, "rt")
                        transposes_to(QB[sl], 4, 64, QTi[sl][0:64, :, :], [("QBd", sl)], [("qTd", sl)], rows=64)
                        return sl

                    nxt = dil_qprep(0)
                    for i in range(NT):
                        sl = nxt
                        if g == 0:
                            pl = [(i - 1, M_PREV), (i, M_CAUS)]
                        elif g == 1:
                            pl = [(i - 4, M_R4F), (i - 3, M_R4M), (i - 2, M_R4M), (i - 1, M_R4M), (i, M_R4D)]
                        else:
                            pl = [(j, M_R16O) for j in range(i)] + [(i, M_R16D)]
                        pairs = [(j, masks[:, m, :], "masks") for (j, m) in pl if j >= 0]

                        def qk_fn(j, sl=sl):
                            return [(hh, 1, KT[0:64, j, hh, :], QTi[sl][0:64, hh, :], [("KT", j), ("qTd", sl)])
                                    for hh in range(4)]

                        def v_fn(hh, j):
                            return VA[:, j, hh, 0:65], [("VA", j), "VAones"]

                        if i + 1 < NT:
                            nxt = dil_qprep(i + 1)
                        Ov, ob = attn_tile(i, pairs, qk_fn, v_fn, 0.125, Pt)
                        if g == 0:
                            CP(P, "dve", ACC[:, i, :, :], Ov, [("ps", ob)], [("ACC", i)])
                        else:
                            TT(P, "dve", ACC[:, i, :, :], ACC[:, i, :, :], Ov, ALU.add, [("ps", ob), ("ACC", i)], [("ACC", i)])
                        if g == 2:
                            osl = slot("OBb", 2)
                            rsl = slot("rc", 2)
                            rc = tl["rc"][rsl]
                            RECIP(P, rc[:, 0:4], ACC[:, i, :, 64], [("ACC", i)], [("rc", rsl)])
                            TT(P, "dve", OBb[osl], ACC[:, i, :, 0:64], rc[:, 0:4].unsqueeze(2).to_broadcast([128, 4, 64]),
                               ALU.mult, [("ACC", i), ("rc", rsl)], [("OBb", osl)])
                            transposes_to(OBb[osl].rearrange("p h c -> p (h c)"), 2, 128, OTb[:, :, i * 128:(i + 1) * 128],
                                          [("OBb", osl)], [("OTb", i)])
                if "d_ob" in dbg_t and sq == 0 and l == 0:
                    DMA(P, "pool", dbg_t["d_ob"].rearrange("p (k t) -> p k t", k=2), OTb, "dbg", ("dbg", len(P.ops)),
                        [("OTb", i) for i in range(NT)], ["dbgout"])
                P.barrier()
                A.release(m0)
                chk("dil")

                OTa = A.alloc(BF16, 4, S)
                m0 = A.mark()
                CQK = A.alloc(BF16, 3, S)
                KRB = A.alloc(BF16, NT, 32)
                m1 = A.mark()
                W1 = A.alloc(BF16, KC, 416)
                gq = A.alloc(F32, 256)
                gkv = A.alloc(F32, 128)
                CQB = [A.alloc(BF16, 384) for _ in range(2)]
                junk = A.alloc(F32, 256)
                st = A.alloc(F32, 8)
                rtmp = [A.alloc(F32, 256) for _ in range(2)]
                DMA(P, "pool", W1, win_v[:, :, 0:416], "w1", tag, [], ["W1"])
                DMA(P, "sp", gq, q_norm_g[l].partition_broadcast(128), "gq", tag, [], ["gq"])
                DMA(P, "sp", gkv, kv_norm_g[l].partition_broadcast(128), "gq", tag, [], ["gkv"])
                for i in range(NT):
                    b = slot("pj", 2)
                    pv = ps(b)
                    proj(i, W1, 0, 416, pv[:, 0:416], "W1", b)
                    sl = slot("CQB", 2)
                    cqb = CQB[sl]
                    for (a0, a1, gg, gk, so) in ((0, 256, gq, "gq", 0), (256, 384, gkv, "gkv", 3)):
                        n_ = a1 - a0
                        ACTV(P, junk[:, 0:n_], pv[:, a0:a1], AF.Square, [("ps", b)], ["mjunk", "mst%d" % so],
                             accum=st[:, so:so + 1])
                        ACTV(P, st[:, so + 1:so + 2], st[:, so:so + 1], AF.Sqrt, ["mst%d" % so], ["mst%d" % (so + 1)],
                             bias=EPS, scale=1.0 / n_)
                        RECIP(P, st[:, so + 2:so + 3], st[:, so + 1:so + 2], ["mst%d" % (so + 1)], ["mst%d" % (so + 2)])
                        STT(P, "dve", cqb[:, a0:a1], pv[:, a0:a1], st[:, so + 2:so + 3], gg, ALU.mult, ALU.mult,
                            [("ps", b), "mst%d" % (so + 2), gk], [("CQB", sl)])
                    rope(pv[:, 384:416].unsqueeze(1), KRB[:, i, :].unsqueeze(1), 1, 32, 16, ropem, i,
                         [("ps", b), "ropem"], [("KRB", i)], rtmp, "rt")
                    transposes_to(cqb, 3, 128, CQK[:, :, i * 128:(i + 1) * 128], [("CQB", sl)], [("CQK", i)])
                P.barrier()
                A.release(m1)
                WUQ = A.alloc(BF16, 2, 768)
                WUKV = A.alloc(BF16, 1024)
                KT = A.alloc(BF16, NT, 4, 128)
                VA = A.alloc(BF16, NT, 4, 72)
                QTi = [A.alloc(BF16, 4, 128) for _ in range(2)]
                QH = [A.alloc(BF16, 4, 96) for _ in range(2)]
                KH = [A.alloc(BF16, 4, 96) for _ in range(2)]
                Pt = [A.alloc(BF16, 4, 128) for _ in range(3)]
                rtmp = [A.alloc(F32, 256) for _ in range(2)]
                tl = {"rc": [A.alloc(F32, 8) for _ in range(2)]}
                OAb = [A.alloc(BF16, 4, 64) for _ in range(2)]
                DMA(P, "pool", WUQ, w_uq[l].rearrange("(kc p) c -> p kc c", p=128), "wuq", tag, [], ["WUQ"])
                DMA(P, "pool", WUKV, w_ukv[l], "wuq", tag, [], ["WUKV"])
                CP(P, "dve", VA[:, :, :, 64:65], onesf[:, 0:64].rearrange("p (a b) -> p a b", a=NT).unsqueeze(3),
                   ["onesf"], ["VAones"])
                for u in range(2):
                    for i in range(NT):
                        b = slot("pj", 2)
                        pv = ps(b)
                        MM(P, pv, CQK[:, 2, i * 128:(i + 1) * 128], WUKV[:, 512 * u:512 * u + 512], True, True,
                           [("CQK", i), "WUKV"], [("ps", b)])
                        pv4 = pv.rearrange("p (h c) -> p h c", h=4)
                        sl = slot("KH", 2)
                        kh = KH[sl]
                        CP(P, "dve", kh[:, :, 0:64], pv4[:, :, 0:64], [("ps", b)], [("KH", sl)])
                        CP(P, "dve", kh[:, :, 64:96], KRB[:, i, :].unsqueeze(1).to_broadcast([128, 4, 32]),
                           [("KRB", i)], [("KH", sl)])
                        CP(P, "dve", VA[:, i, :, 0:64], pv4[:, :, 64:128], [("ps", b)], [("VA", i)])
                        transposes_to(kh.rearrange("p h c -> p (h c)"), 4, 96, KT[0:96, i, :, :], [("KH", sl)],
                                      [("KT", i)], rows=96)

                    def mla_qprep(i, u=u):
                        sl = slot("mq", 2)
                        b = slot("pj", 2)
                        pv = ps(b)
                        for kc in range(2):
                            MM(P, pv[:, 0:384], CQK[:, kc, i * 128:(i + 1) * 128], WUQ[:, kc, 384 * u:384 * u + 384],
                               kc == 0, kc == 1, [("CQK", i), "WUQ"], [("ps", b)])
                        pv4 = pv[:, 0:384].rearrange("p (h c) -> p h c", h=4)
                        qh = QH[sl]
                        CP(P, "dve", qh[:, :, 0:64], pv4[:, :, 0:64], [("ps", b)], [("QH", sl)])
                        rope(pv4[:, :, 64:96], qh[:, :, 64:96], 4, 32, 16, ropem, i, [("ps", b), "ropem"], [("QH", sl)],
                             rtmp, "rt")
                        transposes_to(qh.rearrange("p h c -> p (h c)"), 4, 96, QTi[sl][0:96, :, :], [("QH", sl)],
                                      [("qTm", sl)], rows=96)
                        return sl

                    nxt = mla_qprep(0)
                    for i in range(NT):
                        sl = nxt
                        pairs = [(j, None, None) for j in range(i)] + [(i, masks[:, M_CAUS, :], "masks")]

                        def qk_fn(j, sl=sl):
                            return [(hh, 1, KT[0:96, j, hh, :], QTi[sl][0:96, hh, :], [("KT", j), ("qTm", sl)])
                                    for hh in range(4)]

                        def v_fn(hh, j):
                            return VA[:, j, hh, 0:65], [("VA", j), "VAones"]

                        if i + 1 < NT:
                            nxt = mla_qprep(i + 1)
                        Ov, ob = attn_tile(i, pairs, qk_fn, v_fn, float(96 ** -0.5), Pt)
                        osl = slot("OAb", 2)
                        rsl = slot("rc", 2)
                        rc = tl["rc"][rsl]
                        RECIP(P, rc[:, 0:4], Ov[:, :, 64], [("ps", ob)], [("rc", rsl)])
                        TT(P, "dve", OAb[osl], Ov[:, :, 0:64], rc[:, 0:4].unsqueeze(2).to_broadcast([128, 4, 64]), ALU.mult,
                           [("ps", ob), ("rc", rsl)], [("OAb", osl)])
                        transposes_to(OAb[osl].rearrange("p h c -> p (h c)"), 2, 128,
                                      OTa[:, 2 * u:2 * u + 2, i * 128:(i + 1) * 128], [("OAb", osl)], [("OTa", i, u)])
                if "d_oa" in dbg_t and sq == 0 and l == 0:
                    DMA(P, "pool", dbg_t["d_oa"].rearrange("p (k t) -> p k t", k=4), OTa, "dbg", ("dbg", len(P.ops)),
                        [("OTa", i, u) for i in range(NT) for u in range(2)], ["dbgout"])
                P.barrier()
                A.release(m0)
                chk("mla")

                MT = A.alloc(BF16, KC, S)
                m0 = A.mark()
                WGc = A.alloc(BF16, KC, 3, 256)
                WAc = A.alloc(BF16, 4, 256)
                WBc = A.alloc(BF16, 2, 256)
                WCc = A.alloc(BF16, 4, 256)
                bgc = A.alloc(F32, 3, 256)
                G = [A.alloc(F32, 768) for _ in range(2)]
                Mf = [A.alloc(F32, 256) for _ in range(2)]
                MB = [A.alloc(BF16, 256) for _ in range(2)]
                wgate_v = w_gate[l].rearrange("(kc p) c -> p kc c", p=128)
                wa_v = w_a[l].rearrange("(kc p) c -> p kc c", p=128)
                wb_v = w_b[l].rearrange("(kc p) c -> p kc c", p=128)
                wc_v = w_c[l].rearrange("(kc p) c -> p kc c", p=128)
                for c in range(4):
                    ctag = tag + "c%d" % c
                    for m in range(3):
                        DMA(P, "pool", WGc[:, :, m, :], wgate_v[:, :, m * 1024 + 256 * c:m * 1024 + 256 * c + 256],
                            "wgc", ctag, [], ["WGc"])
                        DMA(P, "sp", bgc[0:1, m, :], b_gate[l, m * 1024 + 256 * c:m * 1024 + 256 * c + 256].unsqueeze(0),
                            "bgc", ctag, [], ["bgc"])
                    DMA(P, "pool", WAc, wa_v[:, :, 256 * c:256 * c + 256], "wgc", ctag, [], ["WAc"])
                    DMA(P, "pool", WBc, wb_v[:, :, 256 * c:256 * c + 256], "wgc", ctag, [], ["WBc"])
                    DMA(P, "pool", WCc, wc_v[:, :, 256 * c:256 * c + 256], "wgc", ctag, [], ["WCc"])
                    for i in range(NT):
                        tsl = slice(i * 128, (i + 1) * 128)
                        gb0, gb1 = 0, 1
                        for m in range(3):
                            gb = gb0 if m < 2 else gb1
                            go = ps(gb)[:, (m % 2) * 256:(m % 2) * 256 + 256]
                            for kc in range(KC):
                                MM(P, go, XT[:, kc, tsl], WGc[:, kc, m, :], kc == 0, False, [("XT", i), "WGc"], [("ps", gb)])
                            MM(P, go, onesf[0:1, 0:128], bgc[0:1, m, :], False, True, ["onesf", "bgc"], [("ps", gb)])
                        gs = slot("G", 2)
                        ACTV(P, G[gs][:, 0:512], ps(gb0), AF.Sigmoid, [("ps", gb0)], [("G", gs)])
                        ACTV(P, G[gs][:, 512:768], ps(gb1)[:, 0:256], AF.Sigmoid, [("ps", gb1)], [("G", gs)])
                        bb0, bb1 = 2, 3
                        for kc in range(4):
                            MM(P, ps(bb0)[:, 0:256], OTa[:, kc, tsl], WAc[:, kc, :], kc == 0, kc == 3,
                               [("OTa", i, 0), ("OTa", i, 1), "WAc"], [("ps", bb0)])
                        for kc in range(2):
                            MM(P, ps(bb0)[:, 256:512], OTb[:, kc, tsl], WBc[:, kc, :], kc == 0, kc == 1,
                               [("OTb", i), "WBc"], [("ps", bb0)])
                        for kc in range(4):
                            MM(P, ps(bb1)[:, 0:256], OTc[:, kc, tsl], WCc[:, kc, :], kc == 0, kc == 3,
                               [("OTc", i), "WCc"], [("ps", bb1)])
                        ms = slot("Mf", 2)
                        TT(P, "dve", G[gs][:, 0:512], G[gs][:, 0:512], ps(bb0), ALU.mult, [("G", gs), ("ps", bb0)], [("G", gs)])
                        TT(P, "dve", G[gs][:, 512:768], G[gs][:, 512:768], ps(bb1)[:, 0:256], ALU.mult,
                           [("G", gs), ("ps", bb1)], [("G", gs)])
                        TT(P, "dve", Mf[ms], G[gs][:, 0:256], G[gs][:, 256:512], ALU.add, [("G", gs)], [("Mf", ms)])
                        TT(P, "dve", MB[ms], Mf[ms], G[gs][:, 512:768], ALU.add, [("G", gs), ("Mf", ms)], [("MB", ms)])
                        transposes_to(MB[ms], 2, 128, MT[:, 2 * c:2 * c + 2, tsl], [("MB", ms)], [("MT", i, c)])
                P.barrier()
                A.release(m0)
                chk("merge1")

                off_c = A.mark() - 8 * S - 4 * S - 2 * S - 4 * S
                WO, e_ = A.view(off_c, BF16, KC, D)
                off_b = off_c + 4 * S
                stv, e_ = A.view(off_b, F32, 8)
                off_a = off_b + 2 * S
                L1G, e_ = A.view(off_a, F32, D)
                L1B, e_ = A.view(e_, F32, D)
                junkL, e_ = A.view(e_, F32, D)
                xb0, e_ = A.view(e_, BF16, D)
                xb1, e_ = A.view(e_, BF16, D)
                assert e_ <= off_a + 4 * S
                DMA(P, "pool", WO, w_o[l].rearrange("(kc p) c -> p kc c", p=128), "wo", tag, [], ["WO"])
                DMA(P, "sp", L1G, ln1_g[l].partition_broadcast(128), "ln1", tag, [], ["L1"])
                DMA(P, "sp", L1B, ln1_b[l].partition_broadcast(128), "ln1", tag, [], ["L1"])
                tlL = {"st": stv, "junk": junkL}
                for i in range(NT):
                    tsl = slice(i * 128, (i + 1) * 128)
                    for ch in range(2):
                        yb = 2 * (i % 2) + ch
                        for kc in range(KC):
                            MM(P, ps(yb), MT[:, kc, tsl], WO[:, kc, ch * 512:(ch + 1) * 512], kc == 0, kc == KC - 1,
                               [("MT", i, c) for c in range(4)] + ["WO"], [("ps", yb)])
                        STT(P, "dve", BIG[:, i, ch * 512:(ch + 1) * 512], BIG[:, i, ch * 512:(ch + 1) * 512], ALPHA, ps(yb),
                            ALU.mult, ALU.add, [("BIG", i), ("ps", yb)], [("BIG", i)])
                    layer_norm(i, L1G, L1B, "L1", tlL)
                    if "d_x1" in dbg_t and sq == 0 and l == 0:
                        DMA(P, "sp", dbg_t["d_x1"][i * 128:(i + 1) * 128, :], BIG[:, i, :], "dbg", ("dbg", len(P.ops)), [("BIG", i)], ["dbgout"])
                    to_XT(i, [xb0, xb1])
                P.barrier()
                A.release(m_layer)
                chk("merge2")

                m0 = A.mark()
                WR = A.alloc(F32, KC, 36)
                BR = A.alloc(F32, 36)
                X32T = A.alloc(F32, KC, 128)
                COMB = A.alloc(F32, NT, 32)
                CT = A.alloc(F32, S)
                LG = A.alloc(F32, 36)
                rr = A.alloc(F32, 16)
                e4 = A.alloc(F32, 4)
                gm = A.alloc(F32, 4)
                pen = A.alloc(F32, 4)
                subm = A.alloc(F32, 32)
                subm2 = A.alloc(F32, 32)
                oh1 = A.alloc(F32, 32)
                oh2 = A.alloc(F32, 32)
                W13 = [A.alloc(BF16, KC, 2, 2, 256) for _ in range(2)]
                W2s = [A.alloc(BF16, 2, 2, D) for _ in range(2)]
                HC = [A.alloc(BF16, 4, 512) for _ in range(2)]
                CB = [A.alloc(F32, 512) for _ in range(2)]
                SL = [A.alloc(F32, 512) for _ in range(2)]
                T1 = [A.alloc(F32, 512) for _ in range(2)]
                L2G = A.alloc(F32, D)
                L2B = A.alloc(F32, D)
                junkM = A.alloc(F32, D)
                stM = A.alloc(F32, 8)
                xbm = [A.alloc(BF16, D) for _ in range(2)]
                DMA(P, "sp", WR[:, :, 0:4], w_group[l].rearrange("(kc p) c -> p kc c", p=128), "wr", tag, [], ["WR"])
                DMA(P, "sp", WR[:, :, 4:36], w_sub[l].rearrange("(kc p) c -> p kc c", p=128), "wr", tag, [], ["WR"])
                DMA(P, "sp", BR[:, 0:4], b_group[l].partition_broadcast(128), "wr", tag, [], ["BR"])
                DMA(P, "sp", BR[:, 4:36], b_sub[l].partition_broadcast(128), "wr", tag, [], ["BR"])
                DMA(P, "sp", L2G, ln2_g[l].partition_broadcast(128), "ln2", tag, [], ["L2"])
                DMA(P, "sp", L2B, ln2_b[l].partition_broadcast(128), "ln2", tag, [], ["L2"])

                def load_pair(q):
                    s_ = q % 2
                    for e2 in range(2):
                        e_id = 2 * q + e2
                        DMA(P, "pool", W13[s_][:, :, e2, 0, :], w1[l, e_id].rearrange("(kc p) f -> p kc f", p=128),
                            "w13_%d" % s_, (tag, q), [], [("W13", s_)])
                        DMA(P, "pool", W13[s_][:, :, e2, 1, :], w3[l, e_id].rearrange("(kc p) f -> p kc f", p=128),
                            "w13_%d" % s_, (tag, q), [], [("W13", s_)])
                        DMA(P, "pool", W2s[s_][:, e2, :, :], w2[l, e_id].rearrange("(fc p) c -> p fc c", p=128),
                            "w2_%d" % s_, (tag, q), [], [("W2", s_)])

                load_pair(0)
                for i in range(NT):
                    kB = ("BIG", i)
                    for half in range(2):
                        tb = 6 + half
                        pvf = ps(tb).rearrange("p (k t) -> p k t", k=4)
                        for kk in range(4):
                            kc = 4 * half + kk
                            TR(P, pvf[:, kk, :], BIG[:, i, kc * 128:(kc + 1) * 128], identf, [kB, "identf"], [("ps", tb)])
                        CP(P, "act", X32T[:, 4 * half:4 * half + 4, :], pvf, [("ps", tb)], [("X32T", half)])
                    lb = 5
                    lg = ps(lb)[:, 0:36]
                    for kc in range(KC):
                        MM(P, lg, X32T[:, kc, :], WR[:, kc, :], kc == 0, kc == KC - 1,
                           [("X32T", 0), ("X32T", 1), "WR"], [("ps", lb)])
                    TT(P, "dve", LG, lg, BR, ALU.add, [("ps", lb), "BR"], ["LG"])
                    RED(P, rr[:, 0:1], LG[:, 0:4], ALU.max, ["LG"], ["rr0"])
                    TS(P, "dve", rr[:, 1:2], rr[:, 0:1], -1.0, None, ALU.mult, None, ["rr0"], ["rr1"])
                    ACTV(P, e4, LG[:, 0:4], AF.Exp, ["LG", "rr1"], ["e4", "rr2"], bias=rr[:, 1:2], accum=rr[:, 2:3])
                    RECIP(P, rr[:, 3:4], rr[:, 2:3], ["rr2"], ["rr3"])
                    TS(P, "dve", gm, LG[:, 0:4], rr[:, 0:1], None, ALU.is_ge, None, ["LG", "rr0"], ["gm"])
                    TS(P, "dve", pen, gm, 1.0e30, -1.0e30, ALU.mult, ALU.add, ["gm"], ["pen"])
                    TT(P, "dve", subm.rearrange("p (g e) -> p g e", g=4), LG[:, 4:36].rearrange("p (g e) -> p g e", g=4),
                       pen.unsqueeze(2).to_broadcast([128, 4, 8]), ALU.add, ["LG", "pen"], ["subm"])
                    RED(P, rr[:, 4:5], subm, ALU.max, ["subm"], ["rr4"])
                    TS(P, "dve", oh1, subm, rr[:, 4:5], None, ALU.is_ge, None, ["subm", "rr4"], ["oh1"])
                    STT(P, "dve", subm2, oh1, -1.0e30, subm, ALU.mult, ALU.add, ["oh1", "subm"], ["subm2"])
                    RED(P, rr[:, 5:6], subm2, ALU.max, ["subm2"], ["rr5"])
                    TS(P, "dve", oh2, subm2, rr[:, 5:6], None, ALU.is_ge, None, ["subm2", "rr5"], ["oh2"])
                    TT(P, "dve", rr[:, 6:7], rr[:, 5:6], rr[:, 4:5], ALU.subtract, ["rr4", "rr5"], ["rr6"])
                    ACTV(P, rr[:, 7:8], rr[:, 6:7], AF.Exp, ["rr6"], ["rr7"])
                    TS(P, "dve", rr[:, 8:9], rr[:, 7:8], 1.0, None, ALU.add, None, ["rr7"], ["rr8"])
                    RECIP(P, rr[:, 9:10], rr[:, 8:9], ["rr8"], ["rr9"])
                    TT(P, "dve", rr[:, 10:11], rr[:, 9:10], rr[:, 3:4], ALU.mult, ["rr9", "rr3"], ["rr10"])
                    TT(P, "dve", rr[:, 11:12], rr[:, 10:11], rr[:, 7:8], ALU.mult, ["rr10", "rr7"], ["rr11"])
                    TS(P, "dve", COMB[:, i, :], oh1, rr[:, 10:11], None, ALU.mult, None, ["oh1", "rr10"], [("COMB", i)])
                    STT(P, "dve", COMB[:, i, :], oh2, rr[:, 11:12], COMB[:, i, :], ALU.mult, ALU.add,
                        ["oh2", "rr11", ("COMB", i)], [("COMB", i)])
                    tb = 6
                    TR(P, ps(tb)[0:32, 0:128], COMB[:, i, :], identf, [("COMB", i), "identf"], [("ps", tb)])
                    CP(P, "act", CT[0:32, i * 128:(i + 1) * 128], ps(tb)[0:32, 0:128], [("ps", tb)], [("CT", i // 4)])
                    TS(P, "dve", BIG[:, i, :], BIG[:, i, :], ALPHA, None, ALU.mult, None, [kB], [kB])
                if "d_comb" in dbg_t and sq == 0 and l == 0:
                    DMA(P, "sp", dbg_t["d_comb"].rearrange("p (i e) -> p i e", i=NT), COMB, "dbg", ("dbg", len(P.ops)),
                        [("COMB", i) for i in range(NT)], ["dbgout"])
                for q in range(16):
                    s_ = q % 2
                    if q + 1 < 16:
                        load_pair(q + 1)
                    for tc in range(4):
                        csl = slice(512 * tc, 512 * tc + 512)
                        hs = slot("HC", 2)
                        for e2 in range(2):
                            e_id = 2 * q + e2
                            MM(P, ps(4), identf[0:32, e_id:e_id + 1].to_broadcast([32, 128]), CT[0:32, csl], True, True,
                               [("CT", tc), "identf"], [("ps", 4)])
                            CP(P, "act", CB[e2], ps(4), [("ps", 4)], [("CB", e2)])
                        for e2 in range(2):
                            for fc in range(2):
                                hb = 2 * slot("H", 2)
                                for wi in range(2):
                                    for kc in range(KC):
                                        MM(P, ps(hb + wi), W13[s_][:, kc, e2, wi, 128 * fc:128 * fc + 128], XT[:, kc, csl],
                                           kc == 0, kc == KC - 1,
                                           [("W13", s_)] + [("XT", 4 * tc + t_) for t_ in range(4)], [("ps", hb + wi)])
                                ks = slot("SL", 2)
                                ACTV(P, SL[ks], ps(hb), AF.Silu, [("ps", hb)], [("SL", ks)])
                                TT(P, "dve", T1[ks], SL[ks], ps(hb + 1), ALU.mult, [("SL", ks), ("ps", hb + 1)], [("T1", ks)])
                                TT(P, "dve", HC[hs][:, 2 * e2 + fc, :], T1[ks], CB[e2], ALU.mult,
                                   [("T1", ks), ("CB", e2)], [("HC", hs)])
                        for t_ in range(4):
                            ti = 4 * tc + t_
                            for ch in range(2):
                                yb = 5 + slot("Yb", 3)
                                for fcc in range(4):
                                    MM(P, ps(yb), HC[hs][:, fcc, 128 * t_:128 * t_ + 128],
                                       W2s[s_][:, fcc // 2, fcc % 2, 512 * ch:512 * ch + 512], fcc == 0, fcc == 3,
                                       [("HC", hs), ("W2", s_)], [("ps", yb)])
                                TT(P, "dve", BIG[:, ti, 512 * ch:512 * ch + 512], BIG[:, ti, 512 * ch:512 * ch + 512], ps(yb),
                                   ALU.add, [("BIG", ti), ("ps", yb)], [("BIG", ti)])
                tlM = {"st": stM, "junk": junkM}
                for i in range(NT):
                    layer_norm(i, L2G, L2B, "L2", tlM)
                    if l == nlayers - 1:
                        DMA(P, "sp", out[sq, i * 128:(i + 1) * 128, :], BIG[:, i, :], "out", sq, [("BIG", i)], ["OUT"])
                    else:
                        to_XT(i, xbm)
                P.barrier()
                A.release(m0)
                chk("moe")

        try:
            for sq in range(nseq):
                for l in range(nlayers):
                    seq_layer(sq, l)
        except Stop:
            pass

        import os
        lim = int(os.environ.get("OPLIM", "0"))
        if lim:
            P.ops = P.ops[:lim]
            P.last_eng = {}
            P.last_dma = {}
            for ii, oo in enumerate(P.ops):
                if oo.dma is None:
                    P.last_eng[oo.eng] = ii
                else:
                    P.last_dma[oo.dma[0]] = ii
            P.gen = {}
        P.add("sp", lambda e: e.nop(), ["OUT", "dbgout"], [])
        P.barrier()
        P.add("sp", lambda e: e.nop(), [], [])
        print("arena peak (bf16 elems):", A.peak, "ops:", len(P.ops))
        P.emit(nc, stack)
    return nc


def host_consts():
    ident = np.eye(128, dtype=np.float32)
    s_ = np.arange(128)[:, None]
    t_ = np.arange(128)[None, :]
    caus = (s_ <= t_)
    prev = (s_ >= t_)
    r4 = ((t_ - s_) % 4 == 0)
    r16 = ((t_ - s_) % 16 == 0)
    masks = np.stack([caus, prev, r4 & caus, r4, r4 & prev, r16 & caus, r16], axis=1).astype(np.float32)
    negm = np.where(np.arange(128)[None, :] <= np.arange(128)[:, None], 0.0, NEG).astype(np.float32)
    pos = (np.arange(NT)[None, :] * 128 + np.arange(128)[:, None]).astype(np.float32)

    def tab(rot):
        inv = (500000.0 ** (-np.arange(0, rot, 2, dtype=np.float32) / rot)).astype(np.float32)
        ang = (pos[:, :, None] * inv[None, None, :]).astype(np.float32)
        c = np.cos(ang).astype(np.float32)
        s = np.sin(ang).astype(np.float32)
        return np.concatenate([c, c, -s, s], axis=-1).astype(np.float32)

    return {
        "c_ident": ident,
        "c_masks": np.ascontiguousarray(masks.reshape(128, 7 * 128)),
        "c_negm": negm,
        "c_ropep": np.ascontiguousarray(tab(16).reshape(128, NT * 32)),
        "c_ropem": np.ascontiguousarray(tab(32).reshape(128, NT * 64)),
    }


_NC_CACHE = {}

IMPLEMENTED = True


SEQ_PER_LAUNCH = 4


def kernel(**inputs):
    nseq_total = 32 // NCORES
    npl = SEQ_PER_LAUNCH
    if npl not in _NC_CACHE:
        _NC_CACHE[npl] = build(nseq=npl)
    nc = _NC_CACHE[npl]
    consts = host_consts()
    xs = np.ascontiguousarray(inputs["x"], dtype=np.float32)
    base = {k: np.ascontiguousarray(v, dtype=np.float32) for k, v in inputs.items() if k != "x"}
    base.update(consts)
    outs = [[None] * (nseq_total // npl) for _ in range(NCORES)]
    for r in range(nseq_total // npl):
        in_maps = []
        for c in range(NCORES):
            m = dict(base)
            lo = c * nseq_total + r * npl
            m["x"] = xs[lo:lo + npl]
            in_maps.append(m)
        res = run_bass_kernel_spmd(nc, in_maps, core_ids=list(range(NCORES)))
        for c in range(NCORES):
            outs[c][r] = np.asarray(res.results[c]["out"], dtype=np.float32)
    return np.concatenate([o for c in range(NCORES) for o in outs[c]], axis=0).astype(np.float32)
```

```python
import numpy as np
from contextlib import ExitStack
import concourse.bass as bass
import concourse.mybir as mybir
from concourse.bass_utils import run_bass_kernel_spmd

F32 = mybir.dt.float32
BF16 = mybir.dt.bfloat16
AF = mybir.ActivationFunctionType
ALU = mybir.AluOpType
AX = mybir.AxisListType

S = 2048
NT = 16
D = 1024
KC = 8
DEPTH = 2
NCORES = 8
ALPHA = float((2 * DEPTH) ** 0.25)
EPS = 1e-6
NEG = -1.0e30
ENGS = ("pe", "act", "dve", "pool", "sp")
NBIS = 22
DBG = {"dsa_tiles": NT, "dsa_stage": 4, "att": 9}


class Op:
    __slots__ = ("eng", "fn", "deps", "dma", "need", "sig", "waits")

    def __init__(self, eng, fn, deps, dma):
        self.eng = eng
        self.fn = fn
        self.deps = deps
        self.dma = dma
        self.need = False
        self.sig = None
        self.waits = None


class Prog:
    def __init__(self):
        self.ops = []
        self.gen = {}
        self.fence = {}
        self.last_eng = {}
        self.last_dma = {}

    def add(self, eng, fn, r=(), w=(), dma=None):
        idx = len(self.ops)
        deps = {}
        for k in r:
            if isinstance(k, tuple) and k and k[0] == "ps":
                g = self.gen.get(k)
                if g:
                    for d in g[1]:
                        deps.setdefault(d, False)
        for k in r:
            g = self.gen.get(k)
            if g:
                for d in g[0]:
                    deps[d] = True
        for k in w:
            g = self.gen.get(k)
            if g:
                for d in g[1]:
                    deps.setdefault(d, False)
        for d in self.fence.values():
            deps.setdefault(d, False)
        for k in r:
            self.gen.setdefault(k, [[], []])[1].append(idx)
        for k in w:
            g = self.gen.setdefault(k, [[], []])
            if g[1]:
                g[0] = [idx]
                g[1] = []
            else:
                g[0].append(idx)
        self.ops.append(Op(eng, fn, deps, dma))
        if dma is None:
            self.last_eng[eng] = idx
        else:
            self.last_dma[dma[0]] = idx
        return idx

    def barrier(self):
        f = {}
        for e, i in self.last_eng.items():
            f[("e", e)] = i
        for s, i in self.last_dma.items():
            f[("d", s)] = i
        self.fence = f

    def emit(self, nc, stack):
        ops = self.ops
        for o in ops:
            o.waits = []
            for d, raw in o.deps.items():
                p = ops[d]
                if p.dma is None and p.eng == o.eng and o.dma is None and o.eng == "pe":
                    continue
                o.waits.append(d)
                p.need = True
        dcount = {}
        dround_end = {}
        for o in ops:
            if o.dma is not None:
                s, rd = o.dma
                dcount[s] = dcount.get(s, 0) + 1
                dround_end[(s, rd)] = dcount[s]
        sems = {}

        def getsem(name):
            if name not in sems:
                sems[name] = stack.enter_context(nc.semaphore("s_" + name))
            return sems[name]

        LIM = 24000
        cnt = {e: 0 for e in ENGS}
        for o in ops:
            if o.dma is not None:
                s, rd = o.dma
                o.sig = (getsem("d_" + s), 16 * dround_end[(s, rd)])
            elif o.need:
                c = cnt[o.eng]
                ep = c // LIM
                o.sig = (getsem("%s%d" % (o.eng, ep)), c % LIM + 1)
                cnt[o.eng] = c + 1
        per = {e: [] for e in ENGS}
        for o in ops:
            per[o.eng].append(o)

        def run(e, lst):
            waited = {}
            for o in lst:
                for d in o.waits:
                    sem, val = ops[d].sig
                    key = id(sem)
                    if waited.get(key, 0) >= val:
                        continue
                    waited[key] = val
                    e.wait_ge(sem, val)
                ins = o.fn(e)
                if o.dma is not None:
                    ins.then_inc(o.sig[0], 16)
                elif o.need:
                    ins.then_inc(o.sig[0], 1)

        with nc.Block() as block:
            @block.tensor
            def _(e):
                run(e, per["pe"])

            @block.scalar
            def _(e):
                run(e, per["act"])

            @block.vector
            def _(e):
                run(e, per["dve"])

            @block.gpsimd
            def _(e):
                run(e, per["pool"])

            @block.sync
            def _(e):
                run(e, per["sp"])


class Arena:
    def __init__(self, tens, n):
        self.t = tens
        self.n = n
        self.top = 0
        self.peak = 0

    def alloc(self, dtype, *shape):
        n = 1
        for s in shape:
            n *= s
        size = n * (2 if dtype == F32 else 1)
        size = (size + 31) // 32 * 32
        off = self.top
        self.top += size
        self.peak = max(self.peak, self.top)
        assert self.top <= self.n, ("arena overflow", self.top, self.n)
        ap = self.t[:, off:off + (n * 2 if dtype == F32 else n)]
        if dtype == F32:
            ap = ap.bitcast(F32)
        if len(shape) > 1:
            names = "abcdefg"[: len(shape)]
            pat = "p (%s) -> p %s" % (" ".join(names), " ".join(names))
            kw = {names[i]: shape[i] for i in range(len(shape))}
            ap = ap.rearrange(pat, **kw)
        return ap

    def view(self, off, dtype, *shape):
        save = self.top
        self.top = off
        ap = self.alloc(dtype, *shape)
        end = self.top
        self.top = save
        return ap, end

    def mark(self):
        return self.top

    def release(self, m):
        self.top = m


class K:
    pass


def MM(P, out, lhsT, rhs, start, stop, r, w, skip=False):
    if skip:
        P.add("pe", lambda e: e.matmul(out, lhsT=lhsT, rhs=rhs, start=start, stop=stop, skip_group_check=True), r, w)
    else:
        P.add("pe", lambda e: e.matmul(out, lhsT=lhsT, rhs=rhs, start=start, stop=stop), r, w)


def TR(P, out, in_, ident, r, w):
    P.add("pe", lambda e: e.transpose(out, in_, ident), r, w)


def ACTV(P, out, in_, func, r, w, bias=None, scale=None, accum=None):
    kw = {}
    if bias is not None:
        kw["bias"] = bias
    if scale is not None:
        kw["scale"] = scale
    if accum is not None:
        kw["accum_out"] = accum
    P.add("act", lambda e: e.activation(out=out, in_=in_, func=func, **kw), r, w)


def TT(P, eng, out, in0, in1, op, r, w):
    P.add(eng, lambda e: e.tensor_tensor(out=out, in0=in0, in1=in1, op=op), r, w)


def TS(P, eng, out, in0, s1, s2, op0, op1, r, w, accum=None):
    if op1 is None:
        P.add(eng, lambda e: e.tensor_scalar(out=out, in0=in0, scalar1=s1, scalar2=0.0, op0=op0, op1=ALU.add), r, w)
    elif accum is None:
        P.add(eng, lambda e: e.tensor_scalar(out=out, in0=in0, scalar1=s1, scalar2=s2, op0=op0, op1=op1), r, w)
    else:
        P.add(eng, lambda e: e.tensor_scalar(out=out, in0=in0, scalar1=s1, scalar2=s2, op0=op0, op1=op1,
                                             accum_out=accum), r, w)


def STT(P, eng, out, in0, scalar, in1, op0, op1, r, w):
    P.add(eng, lambda e: e.scalar_tensor_tensor(out=out, in0=in0, scalar=scalar, in1=in1, op0=op0, op1=op1), r, w)


def CP(P, eng, out, in_, r, w):
    if eng == "act":
        P.add("act", lambda e: e.copy(out=out, in_=in_), r, w)
    else:
        P.add(eng, lambda e: e.tensor_copy(out=out, in_=in_), r, w)


def RED(P, out, in_, op, r, w, absv=False):
    if absv:
        P.add("dve", lambda e: e.tensor_reduce(out=out, in_=in_, axis=AX.X, op=op, apply_absolute_value=True), r, w)
    else:
        P.add("dve", lambda e: e.tensor_reduce(out=out, in_=in_, axis=AX.X, op=op), r, w)


def RECIP(P, out, in_, r, w):
    P.add("dve", lambda e: e.reciprocal(out=out, in_=in_), r, w)


def MSET(P, eng, ap, val, r, w):
    P.add(eng, lambda e: e.memset(ap, val), r, w)


CUR = {"sq": 0}


def DMA(P, q, out, in_, stream, rnd, r, w):
    P.add(q, lambda e: e.dma_start(out=out, in_=in_), r, w, dma=("%s_%s_q%d" % (q, stream, CUR["sq"] % 2), rnd))


def build(nseq=4, nlayers=DEPTH, dbg=None, stop_after=None):
    dbg = dbg or set()
    nc = bass.Bass("TRN2", target_bir_lowering=False)
    L = DEPTH

    def din(name, shape):
        return nc.dram_tensor(name, list(shape), F32, kind="ExternalInput").ap()

    x = din("x", (nseq, S, D))
    w_in = din("w_in", (L, D, 3944))
    q_norm_g = din("q_norm_g", (L, 256))
    w_uq = din("w_uq", (L, 256, 768))
    kv_norm_g = din("kv_norm_g", (L, 128))
    w_ukv = din("w_ukv", (L, 128, 1024))
    w_gate = din("w_gate", (L, D, 3072))
    b_gate = din("b_gate", (L, 3072))
    w_a = din("w_a", (L, 512, D))
    w_b = din("w_b", (L, 256, D))
    w_c = din("w_c", (L, 512, D))
    w_o = din("w_o", (L, D, D))
    ln1_g = din("ln1_g", (L, D))
    ln1_b = din("ln1_b", (L, D))
    w_group = din("w_group", (L, D, 4))
    b_group = din("b_group", (L, 4))
    w_sub = din("w_sub", (L, D, 32))
    b_sub = din("b_sub", (L, 32))
    w1 = din("w1", (L, 32, D, 256))
    w3 = din("w3", (L, 32, D, 256))
    w2 = din("w2", (L, 32, 256, D))
    ln2_g = din("ln2_g", (L, D))
    ln2_b = din("ln2_b", (L, D))
    c_ident = din("c_ident", (128, 128))
    c_masks = din("c_masks", (128, 7 * 128))
    c_negm = din("c_negm", (128, 128))
    c_ropep = din("c_ropep", (128, NT * 32))
    c_ropem = din("c_ropem", (128, NT * 64))
    out = nc.dram_tensor("out", [nseq, S, D], F32, kind="ExternalOutput").ap()
    dbg_t = {}
    for name, shape in (("d_oc", (128, 4 * S)), ("d_oa", (128, 4 * S)), ("d_ob", (128, 2 * S)),
                        ("d_x1", (S, D)), ("d_sc", (128, S)), ("d_comb", (128, NT * 32))):
        if name in dbg:
            dbg_t[name] = nc.dram_tensor(name, list(shape), F32, kind="ExternalOutput").ap()

    P = Prog()
    stack = ExitStack()
    with stack:
        ARN = 106000
        arena_t = stack.enter_context(nc.sbuf_tensor("arena", [128, ARN], BF16))
        A = Arena(arena_t, ARN)
        psb = [stack.enter_context(nc.psum_tensor("psb%d" % i, [128, 512], F32)) for i in range(8)]

        def ps(b):
            return psb[b][:, :]

        def psbf(b):
            return psb[b][:, :].bitcast(BF16)

        BIG = A.alloc(F32, NT, D)
        XT = A.alloc(BF16, KC, S)
        identb = A.alloc(BF16, 128)
        identf = A.alloc(F32, 128)
        masks = A.alloc(BF16, 7, 128)
        negm = A.alloc(F32, 128)
        ropep = A.alloc(F32, NT, 32)
        ropem = A.alloc(F32, NT, 64)
        onesf = A.alloc(F32, 128)
        pow2 = A.alloc(F32, NBIS)

        DMA(P, "sp", identf, c_ident, "const", 0, [], ["identf"])
        DMA(P, "pool", identb, c_ident, "constb", 0, [], ["identb"])
        DMA(P, "pool", masks, c_masks.rearrange("p (m t) -> p m t", m=7), "constb", 0, [], ["masks"])
        DMA(P, "sp", negm, c_negm, "const", 0, [], ["negm"])
        DMA(P, "sp", ropep, c_ropep.rearrange("p (i c) -> p i c", i=NT), "const", 0, [], ["ropep"])
        DMA(P, "sp", ropem, c_ropem.rearrange("p (i c) -> p i c", i=NT), "const", 0, [], ["ropem"])
        MSET(P, "dve", onesf, 1.0, [], ["onesf"])
        for k in range(NBIS):
            MSET(P, "dve", pow2[:, k:k + 1], float(2.0 ** -k), [], ["pow2"])

        M_CAUS, M_PREV, M_R4D, M_R4M, M_R4F, M_R16D, M_R16O = range(7)

        rot = {}

        def slot(name, n):
            v = rot.get(name, 0)
            rot[name] = v + 1
            return v % n

        def to_XT(i, xb_tiles):
            sl = slot("xb", 2)
            xb = xb_tiles[sl]
            CP(P, "dve", xb, BIG[:, i, :], [("BIG", i)], [("xb", sl)])
            b = 6 + slot("tp", 2)
            pv = psbf(b).rearrange("p (k t) -> p k t", k=8)
            for kc in range(KC):
                TR(P, pv[:, kc, :], xb[:, kc * 128:(kc + 1) * 128], identb, [("xb", sl), "identb"], [("ps", b)])
            CP(P, "act", XT[:, :, i * 128:(i + 1) * 128], pv, [("ps", b)], [("XT", i)])

        def proj(i, wt, c0, ncols, pout, wkey, b, start=True, stop=True):
            for kc in range(KC):
                MM(P, pout, XT[:, kc, i * 128:(i + 1) * 128], wt[:, kc, c0:c0 + ncols],
                   start and kc == 0, stop and kc == KC - 1, [("XT", i), wkey], [("ps", b)])

        def rope(src, dst, H, hd, half, table, i, rkeys, wkeys, tmp, tkey):
            r2 = 2 * half
            cc = table[:, i, 0:r2].unsqueeze(1).to_broadcast([128, H, r2])
            ns = table[:, i, r2:r2 + half].unsqueeze(1).to_broadcast([128, H, half])
            ps_ = table[:, i, r2 + half:r2 + 2 * half].unsqueeze(1).to_broadcast([128, H, half])
            u = tmp[0][:, 0:H * r2].rearrange("p (h c) -> p h c", h=H)
            v = tmp[1][:, 0:H * r2].rearrange("p (h c) -> p h c", h=H)
            TT(P, "dve", u[:, :, 0:half], src[:, :, half:r2], ns, ALU.mult, rkeys + ["rtu"], ["rtu"])
            TT(P, "dve", u[:, :, half:r2], src[:, :, 0:half], ps_, ALU.mult, rkeys + ["rtu"], ["rtu"])
            TT(P, "dve", v, src[:, :, 0:r2], cc, ALU.mult, rkeys + ["rtv"], ["rtv"])
            TT(P, "dve", dst[:, :, 0:r2], u, v, ALU.add, ["rtu", "rtv"], wkeys)
            if hd > r2:
                CP(P, "dve" if H > 1 else "act", dst[:, :, r2:hd], src[:, :, r2:hd], rkeys, wkeys)

        def layer_norm(i, g_rep, b_rep, gkey, tl):
            xt_ = BIG[:, i, :]
            kB = ("BIG", i)
            st = tl["st"]
            sk = "lnst"
            RED(P, st[:, 0:1], xt_, ALU.add, [kB], [sk + "0"])
            TS(P, "dve", st[:, 1:2], st[:, 0:1], -1.0 / D, None, ALU.mult, None, [sk + "0"], [sk + "1"])
            ACTV(P, xt_, xt_, AF.Identity, [kB, sk + "1"], [kB], bias=st[:, 1:2])
            ACTV(P, tl["junk"], xt_, AF.Square, [kB], ["lnjunk", sk + "2"], accum=st[:, 2:3])
            ACTV(P, st[:, 3:4], st[:, 2:3], AF.Sqrt, [sk + "2"], [sk + "3"], bias=EPS, scale=1.0 / D)
            RECIP(P, st[:, 4:5], st[:, 3:4], [sk + "3"], [sk + "4"])
            STT(P, "dve", xt_, xt_, st[:, 4:5], g_rep, ALU.mult, ALU.mult, [kB, sk + "4", gkey], [kB])
            TT(P, "dve", xt_, xt_, b_rep, ALU.add, [kB, gkey], [kB])

        def attn_tile(i, pairs, qk_fn, v_fn, scale, Ptiles, nh=4):
            ob = 4 + slot("O", 2)
            Ov = ps(ob)[:, 0:nh * 65].rearrange("p (h c) -> p h c", h=nh)
            n = len(pairs)

            def emit_qk(idx):
                j = pairs[idx][0]
                sb_ = 2 + slot("S", 2)
                Sv = ps(sb_).rearrange("p (h t) -> p h t", h=4)
                for (h0, nhh, lhsT, rhs, rk) in qk_fn(j):
                    MM(P, Sv[:, h0:h0 + nhh, :], lhsT, rhs, True, True, rk, [("ps", sb_)])
                return sb_, Sv

            cur = emit_qk(0)
            for idx, (j, mk, mkey) in enumerate(pairs):
                sb_, Sv = cur
                if idx + 1 < n:
                    cur = emit_qk(idx + 1)
                psl = slot("P", 3)
                Pt = Ptiles[psl]
                ACTV(P, Pt[:, 0:nh, :], Sv[:, 0:nh, :], AF.Exp, [("ps", sb_)], [("P", psl)], scale=scale)
                if mk is not None:
                    TT(P, "dve", Pt[:, 0:nh, :], Pt[:, 0:nh, :], mk.unsqueeze(1).to_broadcast([128, nh, 128]),
                       ALU.mult, [("P", psl), mkey], [("P", psl)])
                for hh in range(nh):
                    rv, rk = v_fn(hh, j)
                    MM(P, Ov[:, hh, :], Pt[:, hh, :], rv, idx == 0 and hh == 0, idx == n - 1, [("P", psl)] + rk,
                       [("ps", ob)], skip=True)
            return Ov, ob

        def normalize_out(Ov, ob, dst, nh, tl):
            sl = slot("rc", 2)
            rc = tl["rc"][sl]
            RECIP(P, rc[:, 0:nh], Ov[:, :, 64], [("ps", ob)], [("rc", sl)])
            TT(P, "dve", dst, Ov[:, :, 0:64], rc[:, 0:nh].unsqueeze(2).to_broadcast([128, nh, 64]), ALU.mult,
               [("ps", ob), ("rc", sl)], ["normdst"])

        def transposes_to(src_bf, nblk, width, dst_ap, rkeys, wkeys, rows=128):
            b = 6 + slot("tp", 2)
            pv = psbf(b).rearrange("p (k t) -> p k t", k=8)
            for k in range(nblk):
                TR(P, pv[0:width, k, :], src_bf[:, k * width:(k + 1) * width], identb, rkeys + ["identb"], [("ps", b)])
            CP(P, "act" if rows == 128 else "dve", dst_ap, pv[0:rows, 0:nblk, :], [("ps", b)], wkeys)

        class Stop(Exception):
            pass

        def chk(name):
            if stop_after == name:
                raise Stop()

        def seq_layer(sq, l):
            CUR["sq"] = sq
            if True:
                tag = "s%dl%d" % (sq, l)
                win_v = w_in[l].rearrange("(kc p) c -> p kc c", p=128)
                m_layer = A.mark()
                if l == 0:
                    m0 = A.mark()
                    xb_tiles = [A.alloc(BF16, D) for _ in range(2)]
                    for i in range(NT):
                        DMA(P, "sp", BIG[:, i, :], x[sq, i * 128:(i + 1) * 128, :], "x", sq, [], [("BIG", i)])
                    for i in range(NT):
                        to_XT(i, xb_tiles)
                    P.barrier()
                    A.release(m0)
                    chk("load")

                OTc = A.alloc(BF16, 4, S)
                m0 = A.mark()
                WQ = A.alloc(BF16, KC, 1024)
                WK = A.alloc(BF16, KC, 200)
                KI = A.alloc(BF16, 2, S)
                VC = A.alloc(BF16, NT, 72)
                WAb = A.alloc(F32, NT, 8)
                SG = A.alloc(F32, NT, 8)
                SC = A.alloc(F32, S)
                MK = A.alloc(BF16, S)
                MKT = A.alloc(BF16, NT, 128)
                RL = [A.alloc(F32, 512) for _ in range(2)]
                QTi = [A.alloc(BF16, 8, 128) for _ in range(2)]
                IQTi = [A.alloc(BF16, 8, 128) for _ in range(2)]
                QB = [A.alloc(BF16, 512) for _ in range(2)]
                IQB = [A.alloc(BF16, 512) for _ in range(2)]
                KK = [A.alloc(BF16, 128) for _ in range(2)]
                Pt = [A.alloc(BF16, 4, 128) for _ in range(3)]
                rtmp = [A.alloc(F32, 256) for _ in range(2)]
                tl = {"rc": [A.alloc(F32, 8) for _ in range(2)]}
                OCb = [A.alloc(BF16, 8, 64) for _ in range(2)]
                bis = A.alloc(F32, 8)
                wtab = A.alloc(F32, NBIS)

                DMA(P, "pool", WK[:, :, 0:128], win_v[:, :, 3232:3360], "wk", tag, [], ["WK"])
                DMA(P, "pool", WK[:, :, 128:200], win_v[:, :, 3872:3944], "wk", tag, [], ["WK"])
                DMA(P, "pool", WQ[:, :, 0:512], win_v[:, :, 2720:3232], "wq", tag, [], ["WQ"])
                DMA(P, "pool", WQ[:, :, 512:1024], win_v[:, :, 3360:3872], "wq", tag, [], ["WQ"])
                CP(P, "dve", VC[:, :, 64:65], onesf[:, 0:NT].unsqueeze(2), ["onesf"], ["VCones"])
                for i in range(NT):
                    b = slot("pj", 2)
                    pv = ps(b)
                    proj(i, WK, 0, 128, pv[:, 0:128], "WK", b)
                    proj(i, WK, 128, 72, pv[:, 128:200], "WK", b)
                    sl = slot("KK", 2)
                    kk = KK[sl]
                    rope(pv[:, 0:64].unsqueeze(1), kk[:, 0:64].unsqueeze(1), 1, 64, 8, ropep, i,
                         [("ps", b), "ropep"], [("KK", sl)], rtmp, "rtA")
                    rope(pv[:, 128:192].unsqueeze(1), kk[:, 64:128].unsqueeze(1), 1, 64, 8, ropep, i,
                         [("ps", b), "ropep"], [("KK", sl)], rtmp, "rtB")
                    CP(P, "dve", VC[:, i, 0:64], pv[:, 64:128], [("ps", b)], [("VC", i)])
                    ACTV(P, WAb[:, i, :], pv[:, 192:200], AF.Abs, [("ps", b)], [("WA", i)])
                    ACTV(P, SG[:, i, :], pv[:, 192:200], AF.Sign, [("ps", b)], [("SG", i)])
                    tb = 6 + slot("tp", 2)
                    tv = psbf(tb).rearrange("p (k t) -> p k t", k=8)
                    TR(P, tv[0:64, 0, :], kk[:, 0:64], identb, [("KK", sl), "identb"], [("ps", tb)])
                    TR(P, tv[0:64, 1, :], kk[:, 64:128], identb, [("KK", sl), "identb"], [("ps", tb)])
                    CP(P, "dve", KI[0:64, :, i * 128:(i + 1) * 128], tv[0:64, 0:2, :], [("ps", tb)], [("KI", i)])

                chk("dsa_pro")

                def dsa_qprep(i):
                    sl = slot("dq", 2)
                    for (c0, dstb, dstT, nm) in ((0, QB[sl], QTi[sl], "q"), (512, IQB[sl], IQTi[sl], "iq")):
                        b = slot("pj", 2)
                        pv = ps(b)
                        proj(i, WQ, c0, 512, pv, "WQ", b)
                        rope(pv.rearrange("p (h c) -> p h c", h=8), dstb.rearrange("p (h c) -> p h c", h=8),
                             8, 64, 8, ropep, i, [("ps", b), "ropep"], [(nm + "B", sl)], rtmp, "rtQ")
                        transposes_to(dstb, 8, 64, dstT[0:64, :, :], [(nm + "B", sl)], [(nm + "T", sl)], rows=64)
                    return sl

                nxt = dsa_qprep(0)
                for i in range(DBG["dsa_tiles"]):
                    sl = nxt
                    hi = (i + 1) * 128
                    for h in range(8):
                        for c0 in range(0, hi, 512):
                            c1 = min(hi, c0 + 512)
                            sb_ = 2 + slot("S", 2)
                            Rv = ps(sb_)[:, 0:c1 - c0]
                            jkeys = [("KI", jj) for jj in range(c0 // 128, c1 // 128)]
                            MM(P, Rv, IQTi[sl][0:64, h, :], KI[0:64, 1, c0:c1], True, True,
                               [("iqT", sl)] + jkeys, [("ps", sb_)])
                            rs = slot("RL", 2)
                            ACTV(P, RL[rs][:, 0:c1 - c0], Rv, AF.Relu, [("ps", sb_), ("WA", i)], [("RL", rs)],
                                 scale=WAb[:, i, h:h + 1])
                            if h == 0:
                                TS(P, "dve", SC[:, c0:c1], RL[rs][:, 0:c1 - c0], SG[:, i, h:h + 1], None, ALU.mult, None,
                                   [("RL", rs), ("SG", i)], [("SC", c0)])
                            else:
                                STT(P, "dve", SC[:, c0:c1], RL[rs][:, 0:c1 - c0], SG[:, i, h:h + 1], SC[:, c0:c1],
                                    ALU.mult, ALU.add, [("RL", rs), ("SG", i), ("SC", c0)], [("SC", c0)])
                    sckeys = [("SC", c0) for c0 in range(0, hi, 512)]
                    if i + 1 < NT:
                        nxt = dsa_qprep(i + 1)
                    if DBG["dsa_stage"] < 2:
                        continue
                    if i >= 2:
                        RED(P, bis[:, 0:1], SC[:, 0:hi], ALU.max, sckeys, ["bisB"], absv=True)
                        TS(P, "dve", bis[:, 0:1], bis[:, 0:1], 1.0, None, ALU.add, None, ["bisB"], ["bisB"])
                        TS(P, "dve", bis[:, 1:2], bis[:, 0:1], -1.0, None, ALU.mult, None, ["bisB"], ["bislo"])
                        TS(P, "dve", wtab, pow2, bis[:, 0:1], None, ALU.mult, None, ["bisB", "pow2"], ["wtab"])
                    TT(P, "dve", SC[:, i * 128:hi], SC[:, i * 128:hi], negm, ALU.add,
                       [("SC", (i * 128) // 512 * 512), "negm"], [("SC", (i * 128) // 512 * 512)])
                    if i >= 2:
                        for k in range(NBIS):
                            TT(P, "dve", bis[:, 2:3], bis[:, 1:2], wtab[:, k:k + 1], ALU.add, ["bislo", "wtab"], ["bismid"])
                            TS(P, "dve", MK[:, 0:hi], SC[:, 0:hi], bis[:, 2:3], 0.0, ALU.is_ge, ALU.add,
                               sckeys + ["bismid"], ["MK", "biscnt"], accum=bis[:, 3:4])
                            TS(P, "dve", bis[:, 4:5], bis[:, 3:4], 256.0, wtab[:, k:k + 1], ALU.is_ge, ALU.mult,
                               ["biscnt", "wtab"], ["bisstep"])
                            TT(P, "dve", bis[:, 1:2], bis[:, 1:2], bis[:, 4:5], ALU.add, ["bislo", "bisstep"], ["bislo"])
                    else:
                        MSET(P, "dve", bis[:, 1:2], -1.0e29, ["bislo"], ["bislo"])
                    TS(P, "dve", MK[:, 0:hi], SC[:, 0:hi], bis[:, 1:2], None, ALU.is_ge, None, sckeys + ["bislo"], ["MK"])
                    if "d_sc" in dbg_t and sq == 0 and l == 0 and i == NT - 1:
                        DMA(P, "sp", dbg_t["d_sc"], SC, "dbg", ("dbg", len(P.ops)), sckeys, ["dbgout"])
                    if DBG["dsa_stage"] < 3:
                        continue
                    for j0 in range(0, i + 1, 8):
                        j1 = min(i + 1, j0 + 8)
                        tb = 6 + slot("tp", 2)
                        tv = psbf(tb).rearrange("p (k t) -> p k t", k=8)
                        for j in range(j0, j1):
                            TR(P, tv[:, j - j0, :], MK[:, j * 128:(j + 1) * 128], identb, ["MK", "identb"], [("ps", tb)])
                        CP(P, "act", MKT[:, j0:j1, :], tv[:, 0:j1 - j0, :], [("ps", tb)], [("MKT", j0)])
                    if DBG["dsa_stage"] < 4:
                        continue
                    osl = slot("OCb", 2)
                    for hg in range(2):
                        def qk_fn(j, hg=hg, sl=sl):
                            return [(0, 4, KI[0:64, 0, j * 128:(j + 1) * 128], QTi[sl][0:64, 4 * hg:4 * hg + 4, :],
                                     [("KI", j), ("qT", sl)])]

                        def v_fn(hh, j):
                            return VC[:, j, 0:65], [("VC", j), "VCones"]

                        pairs = [(j, MKT[:, j, :], ("MKT", j // 8 * 8)) for j in range(i + 1)]
                        Ov, ob = attn_tile(i, pairs, qk_fn, v_fn, 0.125, Pt)
                        if DBG["att"] < 4:
                            continue
                        normalize_out(Ov, ob, OCb[osl][:, 4 * hg:4 * hg + 4, :], 4, tl)
                    if DBG["att"] < 5:
                        continue
                    transposes_to(OCb[osl].rearrange("p h c -> p (h c)"), 4, 128, OTc[:, :, i * 128:(i + 1) * 128],
                                  ["normdst"], [("OTc", i)])
                if "d_oc" in dbg_t and sq == 0 and l == 0:
                    DMA(P, "pool", dbg_t["d_oc"].rearrange("p (k t) -> p k t", k=4), OTc, "dbg", ("dbg", len(P.ops)),
                        [("OTc", i) for i in range(NT)], ["dbgout"])
                P.barrier()
                A.release(m0)
                chk("dsa")


                OTb = A.alloc(BF16, 2, S)
                m0 = A.mark()
                ACC = A.alloc(F32, NT, 4, 65)
                WGq = A.alloc(BF16, KC, 256)
                WGkv = A.alloc(BF16, KC, 512)
                KT = A.alloc(BF16, NT, 4, 128)
                VA = A.alloc(BF16, NT, 4, 72)
                QTi = [A.alloc(BF16, 4, 128) for _ in range(2)]
                QB = [A.alloc(BF16, 256) for _ in range(2)]
                KB = [A.alloc(BF16, 256) for _ in range(2)]
                Pt = [A.alloc(BF16, 4, 128) for _ in range(3)]
                rtmp = [A.alloc(F32, 256) for _ in range(2)]
                tl = {"rc": [A.alloc(F32, 8) for _ in range(2)]}
                OBb = [A.alloc(BF16, 4, 64) for _ in range(2)]
                CP(P, "dve", VA[:, :, :, 64:65], onesf[:, 0:64].rearrange("p (a b) -> p a b", a=NT).unsqueeze(3),
                   ["onesf"], ["VAones"])
                for g in range(3):
                    c0g = 416 + 768 * g
                    gtag = tag + "g%d" % g
                    DMA(P, "pool", WGq, win_v[:, :, c0g:c0g + 256], "wgq", gtag, [], ["WGq"])
                    DMA(P, "pool", WGkv, win_v[:, :, c0g + 256:c0g + 768], "wgkv", gtag, [], ["WGkv"])
                    for i in range(NT):
                        b = slot("pj", 2)
                        pv = ps(b)
                        proj(i, WGkv, 0, 512, pv, "WGkv", b)
                        sl = slot("KB", 2)
                        rope(pv[:, 0:256].rearrange("p (h c) -> p h c", h=4), KB[sl].rearrange("p (h c) -> p h c", h=4),
                             4, 64, 8, ropep, i, [("ps", b), "ropep"], [("KB", sl)], rtmp, "rt")
                        CP(P, "dve", VA[:, i, :, 0:64], pv[:, 256:512].rearrange("p (h c) -> p h c", h=4),
                           [("ps", b)], [("VA", i)])
                        transposes_to(KB[sl], 4, 64, KT[0:64, i, :, :], [("KB", sl)], [("KT", i)], rows=64)

                    def dil_qprep(i):
                        sl = slot("dlq", 2)
                        b = slot("pj", 2)
                        pv = ps(b)
                        proj(i, WGq, 0, 256, pv[:, 0:256], "WGq", b)
                        rope(pv[:, 0:256].rearrange("p (h c) -> p h c", h=4), QB[sl].rearrange("p (h c) -> p h c", h=4),
                             4, 64, 8, ropep, i, [("ps", b), "ropep"], [("QBd", sl)], rtmp, "rt")
                        transposes_to(QB[sl], 4, 64, QTi[sl][0:64, :, :], [("QBd", sl)], [("qTd", sl)], rows=64)
                        return sl

                    nxt = dil_qprep(0)
                    for i in range(NT):
                        sl = nxt
                        if g == 0:
                            pl = [(i - 1, M_PREV), (i, M_CAUS)]
                        elif g == 1:
                            pl = [(i - 4, M_R4F), (i - 3, M_R4M), (i - 2, M_R4M), (i - 1, M_R4M), (i, M_R4D)]
                        else:
                            pl = [(j, M_R16O) for j in range(i)] + [(i, M_R16D)]
                        pairs = [(j, masks[:, m, :], "masks") for (j, m) in pl if j >= 0]

                        def qk_fn(j, sl=sl):
                            return [(hh, 1, KT[0:64, j, hh, :], QTi[sl][0:64, hh, :], [("KT", j), ("qTd", sl)])
                                    for hh in range(4)]

                        def v_fn(hh, j):
                            return VA[:, j, hh, 0:65], [("VA", j), "VAones"]

                        if i + 1 < NT:
                            nxt = dil_qprep(i + 1)
                        Ov, ob = attn_tile(i, pairs, qk_fn, v_fn, 0.125, Pt)
                        if g == 0:
                            CP(P, "dve", ACC[:, i, :, :], Ov, [("ps", ob)], [("ACC", i)])
                        else:
                            TT(P, "dve", ACC[:, i, :, :], ACC[:, i, :, :], Ov, ALU.add, [("ps", ob), ("ACC", i)], [("ACC", i)])
                        if g == 2:
                            osl = slot("OBb", 2)
                            rsl = slot("rc", 2)
                            rc = tl["rc"][rsl]
                            RECIP(P, rc[:, 0:4], ACC[:, i, :, 64], [("ACC", i)], [("rc", rsl)])
                            TT(P, "dve", OBb[osl], ACC[:, i, :, 0:64], rc[:, 0:4].unsqueeze(2).to_broadcast([128, 4, 64]),
                               ALU.mult, [("ACC", i), ("rc", rsl)], [("OBb", osl)])
                            transposes_to(OBb[osl].rearrange("p h c -> p (h c)"), 2, 128, OTb[:, :, i * 128:(i + 1) * 128],
                                          [("OBb", osl)], [("OTb", i)])
                if "d_ob" in dbg_t and sq == 0 and l == 0:
                    DMA(P, "pool", dbg_t["d_ob"].rearrange("p (k t) -> p k t", k=2), OTb, "dbg", ("dbg", len(P.ops)),
                        [("OTb", i) for i in range(NT)], ["dbgout"])
                P.barrier()
                A.release(m0)
                chk("dil")

                OTa = A.alloc(BF16, 4, S)
                m0 = A.mark()
                CQK = A.alloc(BF16, 3, S)
                KRB = A.alloc(BF16, NT, 32)
                m1 = A.mark()
                W1 = A.alloc(BF16, KC, 416)
                gq = A.alloc(F32, 256)
                gkv = A.alloc(F32, 128)
                CQB = [A.alloc(BF16, 384) for _ in range(2)]
                junk = A.alloc(F32, 256)
                st = A.alloc(F32, 8)
                rtmp = [A.alloc(F32, 256) for _ in range(2)]
                DMA(P, "pool", W1, win_v[:, :, 0:416], "w1", tag, [], ["W1"])
                DMA(P, "sp", gq, q_norm_g[l].partition_broadcast(128), "gq", tag, [], ["gq"])
                DMA(P, "sp", gkv, kv_norm_g[l].partition_broadcast(128), "gq", tag, [], ["gkv"])
                for i in range(NT):
                    b = slot("pj", 2)
                    pv = ps(b)
                    proj(i, W1, 0, 416, pv[:, 0:416], "W1", b)
                    sl = slot("CQB", 2)
                    cqb = CQB[sl]
                    for (a0, a1, gg, gk, so) in ((0, 256, gq, "gq", 0), (256, 384, gkv, "gkv", 3)):
                        n_ = a1 - a0
                        ACTV(P, junk[:, 0:n_], pv[:, a0:a1], AF.Square, [("ps", b)], ["mjunk", "mst%d" % so],
                             accum=st[:, so:so + 1])
                        ACTV(P, st[:, so + 1:so + 2], st[:, so:so + 1], AF.Sqrt, ["mst%d" % so], ["mst%d" % (so + 1)],
                             bias=EPS, scale=1.0 / n_)
                        RECIP(P, st[:, so + 2:so + 3], st[:, so + 1:so + 2], ["mst%d" % (so + 1)], ["mst%d" % (so + 2)])
                        STT(P, "dve", cqb[:, a0:a1], pv[:, a0:a1], st[:, so + 2:so + 3], gg, ALU.mult, ALU.mult,
                            [("ps", b), "mst%d" % (so + 2), gk], [("CQB", sl)])
                    rope(pv[:, 384:416].unsqueeze(1), KRB[:, i, :].unsqueeze(1), 1, 32, 16, ropem, i,
                         [("ps", b), "ropem"], [("KRB", i)], rtmp, "rt")
                    transposes_to(cqb, 3, 128, CQK[:, :, i * 128:(i + 1) * 128], [("CQB", sl)], [("CQK", i)])
                P.barrier()
                A.release(m1)
                WUQ = A.alloc(BF16, 2, 768)
                WUKV = A.alloc(BF16, 1024)
                KT = A.alloc(BF16, NT, 4, 128)
                VA = A.alloc(BF16, NT, 4, 72)
                QTi = [A.alloc(BF16, 4, 128) for _ in range(2)]
                QH = [A.alloc(BF16, 4, 96) for _ in range(2)]
                KH = [A.alloc(BF16, 4, 96) for _ in range(2)]
                Pt = [A.alloc(BF16, 4, 128) for _ in range(3)]
                rtmp = [A.alloc(F32, 256) for _ in range(2)]
                tl = {"rc": [A.alloc(F32, 8) for _ in range(2)]}
                OAb = [A.alloc(BF16, 4, 64) for _ in range(2)]
                DMA(P, "pool", WUQ, w_uq[l].rearrange("(kc p) c -> p kc c", p=128), "wuq", tag, [], ["WUQ"])
                DMA(P, "pool", WUKV, w_ukv[l], "wuq", tag, [], ["WUKV"])
                CP(P, "dve", VA[:, :, :, 64:65], onesf[:, 0:64].rearrange("p (a b) -> p a b", a=NT).unsqueeze(3),
                   ["onesf"], ["VAones"])
                for u in range(2):
                    for i in range(NT):
                        b = slot("pj", 2)
                        pv = ps(b)
                        MM(P, pv, CQK[:, 2, i * 128:(i + 1) * 128], WUKV[:, 512 * u:512 * u + 512], True, True,
                           [("CQK", i), "WUKV"], [("ps", b)])
                        pv4 = pv.rearrange("p (h c) -> p h c", h=4)
                        sl = slot("KH", 2)
                        kh = KH[sl]
                        CP(P, "dve", kh[:, :, 0:64], pv4[:, :, 0:64], [("ps", b)], [("KH", sl)])
                        CP(P, "dve", kh[:, :, 64:96], KRB[:, i, :].unsqueeze(1).to_broadcast([128, 4, 32]),
                           [("KRB", i)], [("KH", sl)])
                        CP(P, "dve", VA[:, i, :, 0:64], pv4[:, :, 64:128], [("ps", b)], [("VA", i)])
                        transposes_to(kh.rearrange("p h c -> p (h c)"), 4, 96, KT[0:96, i, :, :], [("KH", sl)],
                                      [("KT", i)], rows=96)

                    def mla_qprep(i, u=u):
                        sl = slot("mq", 2)
                        b = slot("pj", 2)
                        pv = ps(b)
                        for kc in range(2):
                            MM(P, pv[:, 0:384], CQK[:, kc, i * 128:(i + 1) * 128], WUQ[:, kc, 384 * u:384 * u + 384],
                               kc == 0, kc == 1, [("CQK", i), "WUQ"], [("ps", b)])
                        pv4 = pv[:, 0:384].rearrange("p (h c) -> p h c", h=4)
                        qh = QH[sl]
                        CP(P, "dve", qh[:, :, 0:64], pv4[:, :, 0:64], [("ps", b)], [("QH", sl)])
                        rope(pv4[:, :, 64:96], qh[:, :, 64:96], 4, 32, 16, ropem, i, [("ps", b), "ropem"], [("QH", sl)],
                             rtmp, "rt")
                        transposes_to(qh.rearrange("p h c -> p (h c)"), 4, 96, QTi[sl][0:96, :, :], [("QH", sl)],
                                      [("qTm", sl)], rows=96)
                        return sl

                    nxt = mla_qprep(0)
                    for i in range(NT):
                        sl = nxt
                        pairs = [(j, None, None) for j in range(i)] + [(i, masks[:, M_CAUS, :], "masks")]

                        def qk_fn(j, sl=sl):
                            return [(hh, 1, KT[0:96, j, hh, :], QTi[sl][0:96, hh, :], [("KT", j), ("qTm", sl)])
                                    for hh in range(4)]

                        def v_fn(hh, j):
                            return VA[:, j, hh, 0:65], [("VA", j), "VAones"]

                        if i + 1 < NT:
                            nxt = mla_qprep(i + 1)
                        Ov, ob = attn_tile(i, pairs, qk_fn, v_fn, float(96 ** -0.5), Pt)
                        osl = slot("OAb", 2)
                        rsl = slot("rc", 2)
                        rc = tl["rc"][rsl]
                        RECIP(P, rc[:, 0:4], Ov[:, :, 64], [("ps", ob)], [("rc", rsl)])
                        TT(P, "dve", OAb[osl], Ov[:, :, 0:64], rc[:, 0:4].unsqueeze(2).to_broadcast([128, 4, 64]), ALU.mult,
                           [("ps", ob), ("rc", rsl)], [("OAb", osl)])
                        transposes_to(OAb[osl].rearrange("p h c -> p (h c)"), 2, 128,
                                      OTa[:, 2 * u:2 * u + 2, i * 128:(i + 1) * 128], [("OAb", osl)], [("OTa", i, u)])
                if "d_oa" in dbg_t and sq == 0 and l == 0:
                    DMA(P, "pool", dbg_t["d_oa"].rearrange("p (k t) -> p k t", k=4), OTa, "dbg", ("dbg", len(P.ops)),
                        [("OTa", i, u) for i in range(NT) for u in range(2)], ["dbgout"])
                P.barrier()
                A.release(m0)
                chk("mla")

                MT = A.alloc(BF16, KC, S)
                m0 = A.mark()
                WGc = A.alloc(BF16, KC, 3, 256)
                WAc = A.alloc(BF16, 4, 256)
                WBc = A.alloc(BF16, 2, 256)
                WCc = A.alloc(BF16, 4, 256)
                bgc = A.alloc(F32, 3, 256)
                G = [A.alloc(F32, 768) for _ in range(2)]
                Mf = [A.alloc(F32, 256) for _ in range(2)]
                MB = [A.alloc(BF16, 256) for _ in range(2)]
                wgate_v = w_gate[l].rearrange("(kc p) c -> p kc c", p=128)
                wa_v = w_a[l].rearrange("(kc p) c -> p kc c", p=128)
                wb_v = w_b[l].rearrange("(kc p) c -> p kc c", p=128)
                wc_v = w_c[l].rearrange("(kc p) c -> p kc c", p=128)
                for c in range(4):
                    ctag = tag + "c%d" % c
                    for m in range(3):
                        DMA(P, "pool", WGc[:, :, m, :], wgate_v[:, :, m * 1024 + 256 * c:m * 1024 + 256 * c + 256],
                            "wgc", ctag, [], ["WGc"])
                        DMA(P, "sp", bgc[0:1, m, :], b_gate[l, m * 1024 + 256 * c:m * 1024 + 256 * c + 256].unsqueeze(0),
                            "bgc", ctag, [], ["bgc"])
                    DMA(P, "pool", WAc, wa_v[:, :, 256 * c:256 * c + 256], "wgc", ctag, [], ["WAc"])
                    DMA(P, "pool", WBc, wb_v[:, :, 256 * c:256 * c + 256], "wgc", ctag, [], ["WBc"])
                    DMA(P, "pool", WCc, wc_v[:, :, 256 * c:256 * c + 256], "wgc", ctag, [], ["WCc"])
                    for i in range(NT):
                        tsl = slice(i * 128, (i + 1) * 128)
                        base = 3 * (i % 2)
                        bkA, bkB, bkC = base, base + 1, base + 2
                        for m in range(3):
                            if m < 2:
                                gb, go = bkA, ps(bkA)[:, m * 256:m * 256 + 256]
                            else:
                                gb, go = bkB, ps(bkB)[:, 0:256]
                            for kc in range(KC):
                                MM(P, go, XT[:, kc, tsl], WGc[:, kc, m, :], kc == 0, False, [("XT", i), "WGc"], [("ps", gb)])
                            MM(P, go, onesf[0:1, 0:128], bgc[0:1, m, :], False, True, ["onesf", "bgc"], [("ps", gb)])
                        for kc in range(4):
                            MM(P, ps(bkB)[:, 256:512], OTc[:, kc, tsl], WCc[:, kc, :], kc == 0, kc == 3,
                               [("OTc", i), "WCc"], [("ps", bkB)])
                        for kc in range(4):
                            MM(P, ps(bkC)[:, 0:256], OTa[:, kc, tsl], WAc[:, kc, :], kc == 0, kc == 3,
                               [("OTa", i, 0), ("OTa", i, 1), "WAc"], [("ps", bkC)])
                        for kc in range(2):
                            MM(P, ps(bkC)[:, 256:512], OTb[:, kc, tsl], WBc[:, kc, :], kc == 0, kc == 1,
                               [("OTb", i), "WBc"], [("ps", bkC)])
                        gs = slot("G", 2)
                        ACTV(P, G[gs][:, 0:512], ps(bkA), AF.Sigmoid, [("ps", bkA)], [("G", gs)])
                        ACTV(P, G[gs][:, 512:768], ps(bkB)[:, 0:256], AF.Sigmoid, [("ps", bkB)], [("G", gs)])
                        ms = slot("Mf", 2)
                        TT(P, "dve", G[gs][:, 0:512], G[gs][:, 0:512], ps(bkC), ALU.mult, [("G", gs), ("ps", bkC)], [("G", gs)])
                        TT(P, "dve", G[gs][:, 512:768], G[gs][:, 512:768], ps(bkB)[:, 256:512], ALU.mult,
                           [("G", gs), ("ps", bkB)], [("G", gs)])
                        TT(P, "dve", Mf[ms], G[gs][:, 0:256], G[gs][:, 256:512], ALU.add, [("G", gs)], [("Mf", ms)])
                        TT(P, "dve", MB[ms], Mf[ms], G[gs][:, 512:768], ALU.add, [("G", gs), ("Mf", ms)], [("MB", ms)])
                        transposes_to(MB[ms], 2, 128, MT[:, 2 * c:2 * c + 2, tsl], [("MB", ms)], [("MT", i, c)])
                P.barrier()
                A.release(m0)
                chk("merge1")

                off_c = A.mark() - 8 * S - 4 * S - 2 * S - 4 * S
                WO, e_ = A.view(off_c, BF16, KC, D)
                off_b = off_c + 4 * S
                stv, e_ = A.view(off_b, F32, 8)
                off_a = off_b + 2 * S
                L1G, e_ = A.view(off_a, F32, D)
                L1B, e_ = A.view(e_, F32, D)
                junkL, e_ = A.view(e_, F32, D)
                xb0, e_ = A.view(e_, BF16, D)
                xb1, e_ = A.view(e_, BF16, D)
                assert e_ <= off_a + 4 * S
                DMA(P, "pool", WO, w_o[l].rearrange("(kc p) c -> p kc c", p=128), "wo", tag, [], ["WO"])
                DMA(P, "sp", L1G, ln1_g[l].partition_broadcast(128), "ln1", tag, [], ["L1"])
                DMA(P, "sp", L1B, ln1_b[l].partition_broadcast(128), "ln1", tag, [], ["L1"])
                tlL = {"st": stv, "junk": junkL}
                for i in range(NT):
                    tsl = slice(i * 128, (i + 1) * 128)
                    for ch in range(2):
                        yb = 2 * (i % 2) + ch
                        for kc in range(KC):
                            MM(P, ps(yb), MT[:, kc, tsl], WO[:, kc, ch * 512:(ch + 1) * 512], kc == 0, kc == KC - 1,
                               [("MT", i, c) for c in range(4)] + ["WO"], [("ps", yb)])
                        STT(P, "dve", BIG[:, i, ch * 512:(ch + 1) * 512], BIG[:, i, ch * 512:(ch + 1) * 512], ALPHA, ps(yb),
                            ALU.mult, ALU.add, [("BIG", i), ("ps", yb)], [("BIG", i)])
                    layer_norm(i, L1G, L1B, "L1", tlL)
                    if "d_x1" in dbg_t and sq == 0 and l == 0:
                        DMA(P, "sp", dbg_t["d_x1"][i * 128:(i + 1) * 128, :], BIG[:, i, :], "dbg", ("dbg", len(P.ops)), [("BIG", i)], ["dbgout"])
                    to_XT(i, [xb0, xb1])
                P.barrier()
                A.release(m_layer)
                chk("merge2")

                m0 = A.mark()
                WR = A.alloc(F32, KC, 36)
                BR = A.alloc(F32, 36)
                X32T = A.alloc(F32, KC, 128)
                COMB = A.alloc(F32, NT, 32)
                CT = A.alloc(F32, S)
                LG = A.alloc(F32, 36)
                rr = A.alloc(F32, 16)
                e4 = A.alloc(F32, 4)
                gm = A.alloc(F32, 4)
                pen = A.alloc(F32, 4)
                subm = A.alloc(F32, 32)
                subm2 = A.alloc(F32, 32)
                oh1 = A.alloc(F32, 32)
                oh2 = A.alloc(F32, 32)
                W13 = [A.alloc(BF16, KC, 2, 2, 256) for _ in range(2)]
                W2s = [A.alloc(BF16, 2, 2, D) for _ in range(2)]
                HC = [A.alloc(BF16, 4, 512) for _ in range(2)]
                CB = [A.alloc(F32, 512) for _ in range(2)]
                SL = [A.alloc(F32, 512) for _ in range(2)]
                T1 = [A.alloc(F32, 512) for _ in range(2)]
                L2G = A.alloc(F32, D)
                L2B = A.alloc(F32, D)
                junkM = A.alloc(F32, D)
                stM = A.alloc(F32, 8)
                xbm = [A.alloc(BF16, D) for _ in range(2)]
                DMA(P, "sp", WR[:, :, 0:4], w_group[l].rearrange("(kc p) c -> p kc c", p=128), "wr", tag, [], ["WR"])
                DMA(P, "sp", WR[:, :, 4:36], w_sub[l].rearrange("(kc p) c -> p kc c", p=128), "wr", tag, [], ["WR"])
                DMA(P, "sp", BR[:, 0:4], b_group[l].partition_broadcast(128), "wr", tag, [], ["BR"])
                DMA(P, "sp", BR[:, 4:36], b_sub[l].partition_broadcast(128), "wr", tag, [], ["BR"])
                DMA(P, "sp", L2G, ln2_g[l].partition_broadcast(128), "ln2", tag, [], ["L2"])
                DMA(P, "sp", L2B, ln2_b[l].partition_broadcast(128), "ln2", tag, [], ["L2"])

                def load_pair(q):
                    s_ = q % 2
                    for e2 in range(2):
                        e_id = 2 * q + e2
                        DMA(P, "pool", W13[s_][:, :, e2, 0, :], w1[l, e_id].rearrange("(kc p) f -> p kc f", p=128),
                            "w13_%d" % s_, (tag, q), [], [("W13", s_)])
                        DMA(P, "pool", W13[s_][:, :, e2, 1, :], w3[l, e_id].rearrange("(kc p) f -> p kc f", p=128),
                            "w13_%d" % s_, (tag, q), [], [("W13", s_)])
                        DMA(P, "pool", W2s[s_][:, e2, :, :], w2[l, e_id].rearrange("(fc p) c -> p fc c", p=128),
                            "w2_%d" % s_, (tag, q), [], [("W2", s_)])

                load_pair(0)
                for i in range(NT):
                    kB = ("BIG", i)
                    for half in range(2):
                        tb = 6 + half
                        pvf = ps(tb).rearrange("p (k t) -> p k t", k=4)
                        for kk in range(4):
                            kc = 4 * half + kk
                            TR(P, pvf[:, kk, :], BIG[:, i, kc * 128:(kc + 1) * 128], identf, [kB, "identf"], [("ps", tb)])
                        CP(P, "act", X32T[:, 4 * half:4 * half + 4, :], pvf, [("ps", tb)], [("X32T", half)])
                    lb = 5
                    lg = ps(lb)[:, 0:36]
                    for kc in range(KC):
                        MM(P, lg, X32T[:, kc, :], WR[:, kc, :], kc == 0, kc == KC - 1,
                           [("X32T", 0), ("X32T", 1), "WR"], [("ps", lb)])
                    TT(P, "dve", LG, lg, BR, ALU.add, [("ps", lb), "BR"], ["LG"])
                    RED(P, rr[:, 0:1], LG[:, 0:4], ALU.max, ["LG"], ["rr0"])
                    TS(P, "dve", rr[:, 1:2], rr[:, 0:1], -1.0, None, ALU.mult, None, ["rr0"], ["rr1"])
                    ACTV(P, e4, LG[:, 0:4], AF.Exp, ["LG", "rr1"], ["e4", "rr2"], bias=rr[:, 1:2], accum=rr[:, 2:3])
                    RECIP(P, rr[:, 3:4], rr[:, 2:3], ["rr2"], ["rr3"])
                    TS(P, "dve", gm, LG[:, 0:4], rr[:, 0:1], None, ALU.is_ge, None, ["LG", "rr0"], ["gm"])
                    TS(P, "dve", pen, gm, 1.0e30, -1.0e30, ALU.mult, ALU.add, ["gm"], ["pen"])
                    TT(P, "dve", subm.rearrange("p (g e) -> p g e", g=4), LG[:, 4:36].rearrange("p (g e) -> p g e", g=4),
                       pen.unsqueeze(2).to_broadcast([128, 4, 8]), ALU.add, ["LG", "pen"], ["subm"])
                    RED(P, rr[:, 4:5], subm, ALU.max, ["subm"], ["rr4"])
                    TS(P, "dve", oh1, subm, rr[:, 4:5], None, ALU.is_ge, None, ["subm", "rr4"], ["oh1"])
                    STT(P, "dve", subm2, oh1, -1.0e30, subm, ALU.mult, ALU.add, ["oh1", "subm"], ["subm2"])
                    RED(P, rr[:, 5:6], subm2, ALU.max, ["subm2"], ["rr5"])
                    TS(P, "dve", oh2, subm2, rr[:, 5:6], None, ALU.is_ge, None, ["subm2", "rr5"], ["oh2"])
                    TT(P, "dve", rr[:, 6:7], rr[:, 5:6], rr[:, 4:5], ALU.subtract, ["rr4", "rr5"], ["rr6"])
                    ACTV(P, rr[:, 7:8], rr[:, 6:7], AF.Exp, ["rr6"], ["rr7"])
                    TS(P, "dve", rr[:, 8:9], rr[:, 7:8], 1.0, None, ALU.add, None, ["rr7"], ["rr8"])
                    RECIP(P, rr[:, 9:10], rr[:, 8:9], ["rr8"], ["rr9"])
                    TT(P, "dve", rr[:, 10:11], rr[:, 9:10], rr[:, 3:4], ALU.mult, ["rr9", "rr3"], ["rr10"])
                    TT(P, "dve", rr[:, 11:12], rr[:, 10:11], rr[:, 7:8], ALU.mult, ["rr10", "rr7"], ["rr11"])
                    TS(P, "dve", COMB[:, i, :], oh1, rr[:, 10:11], None, ALU.mult, None, ["oh1", "rr10"], [("COMB", i)])
                    STT(P, "dve", COMB[:, i, :], oh2, rr[:, 11:12], COMB[:, i, :], ALU.mult, ALU.add,
                        ["oh2", "rr11", ("COMB", i)], [("COMB", i)])
                    tb = 6
                    TR(P, ps(tb)[0:32, 0:128], COMB[:, i, :], identf, [("COMB", i), "identf"], [("ps", tb)])
                    CP(P, "act", CT[0:32, i * 128:(i + 1) * 128], ps(tb)[0:32, 0:128], [("ps", tb)], [("CT", i // 4)])
                    TS(P, "dve", BIG[:, i, :], BIG[:, i, :], ALPHA, None, ALU.mult, None, [kB], [kB])
                if "d_comb" in dbg_t and sq == 0 and l == 0:
                    DMA(P, "sp", dbg_t["d_comb"].rearrange("p (i e) -> p i e", i=NT), COMB, "dbg", ("dbg", len(P.ops)),
                        [("COMB", i) for i in range(NT)], ["dbgout"])
                for q in range(16):
                    s_ = q % 2
                    if q + 1 < 16:
                        load_pair(q + 1)
                    for tc in range(4):
                        csl = slice(512 * tc, 512 * tc + 512)
                        hs = slot("HC", 2)
                        for e2 in range(2):
                            e_id = 2 * q + e2
                            MM(P, ps(4), identf[0:32, e_id:e_id + 1].to_broadcast([32, 128]), CT[0:32, csl], True, True,
                               [("CT", tc), "identf"], [("ps", 4)])
                            CP(P, "act", CB[e2], ps(4), [("ps", 4)], [("CB", e2)])
                        for e2 in range(2):
                            for fc in range(2):
                                hb = 2 * slot("H", 2)
                                for wi in range(2):
                                    for kc in range(KC):
                                        MM(P, ps(hb + wi), W13[s_][:, kc, e2, wi, 128 * fc:128 * fc + 128], XT[:, kc, csl],
                                           kc == 0, kc == KC - 1,
                                           [("W13", s_)] + [("XT", 4 * tc + t_) for t_ in range(4)], [("ps", hb + wi)])
                                ks = slot("SL", 2)
                                ACTV(P, SL[ks], ps(hb), AF.Silu, [("ps", hb)], [("SL", ks)])
                                TT(P, "dve", T1[ks], SL[ks], ps(hb + 1), ALU.mult, [("SL", ks), ("ps", hb + 1)], [("T1", ks)])
                                TT(P, "dve", HC[hs][:, 2 * e2 + fc, :], T1[ks], CB[e2], ALU.mult,
                                   [("T1", ks), ("CB", e2)], [("HC", hs)])
                        for t_ in range(4):
                            ti = 4 * tc + t_
                            for ch in range(2):
                                yb = 5 + slot("Yb", 3)
                                for fcc in range(4):
                                    MM(P, ps(yb), HC[hs][:, fcc, 128 * t_:128 * t_ + 128],
                                       W2s[s_][:, fcc // 2, fcc % 2, 512 * ch:512 * ch + 512], fcc == 0, fcc == 3,
                                       [("HC", hs), ("W2", s_)], [("ps", yb)])
                                TT(P, "dve", BIG[:, ti, 512 * ch:512 * ch + 512], BIG[:, ti, 512 * ch:512 * ch + 512], ps(yb),
                                   ALU.add, [("BIG", ti), ("ps", yb)], [("BIG", ti)])
                tlM = {"st": stM, "junk": junkM}
                for i in range(NT):
                    layer_norm(i, L2G, L2B, "L2", tlM)
                    if l == nlayers - 1:
                        DMA(P, "sp", out[sq, i * 128:(i + 1) * 128, :], BIG[:, i, :], "out", sq, [("BIG", i)], ["OUT"])
                    else:
                        to_XT(i, xbm)
                P.barrier()
                A.release(m0)
                chk("moe")

        try:
            for sq in range(nseq):
                for l in range(nlayers):
                    seq_layer(sq, l)
        except Stop:
            pass

        import os
        lim = int(os.environ.get("OPLIM", "0"))
        if lim:
            P.ops = P.ops[:lim]
            P.last_eng = {}
            P.last_dma = {}
            for ii, oo in enumerate(P.ops):
                if oo.dma is None:
                    P.last_eng[oo.eng] = ii
                else:
                    P.last_dma[oo.dma[0]] = ii
            P.gen = {}
        P.add("sp", lambda e: e.nop(), ["OUT", "dbgout"], [])
        P.barrier()
        P.add("sp", lambda e: e.nop(), [], [])
        print("arena peak (bf16 elems):", A.peak, "ops:", len(P.ops))
        P.emit(nc, stack)
    return nc


def host_consts():
    ident = np.eye(128, dtype=np.float32)
    s_ = np.arange(128)[:, None]
    t_ = np.arange(128)[None, :]
    caus = (s_ <= t_)
    prev = (s_ >= t_)
    r4 = ((t_ - s_) % 4 == 0)
    r16 = ((t_ - s_) % 16 == 0)
    masks = np.stack([caus, prev, r4 & caus, r4, r4 & prev, r16 & caus, r16], axis=1).astype(np.float32)
    negm = np.where(np.arange(128)[None, :] <= np.arange(128)[:, None], 0.0, NEG).astype(np.float32)
    pos = (np.arange(NT)[None, :] * 128 + np.arange(128)[:, None]).astype(np.float32)

    def tab(rot):
        inv = (500000.0 ** (-np.arange(0, rot, 2, dtype=np.float32) / rot)).astype(np.float32)
        ang = (pos[:, :, None] * inv[None, None, :]).astype(np.float32)
        c = np.cos(ang).astype(np.float32)
        s = np.sin(ang).astype(np.float32)
        return np.concatenate([c, c, -s, s], axis=-1).astype(np.float32)

    return {
        "c_ident": ident,
        "c_masks": np.ascontiguousarray(masks.reshape(128, 7 * 128)),
        "c_negm": negm,
        "c_ropep": np.ascontiguousarray(tab(16).reshape(128, NT * 32)),
        "c_ropem": np.ascontiguousarray(tab(32).reshape(128, NT * 64)),
    }


_NC_CACHE = {}

IMPLEMENTED = True


SEQ_PER_LAUNCH = 4


def kernel(**inputs):
    nseq_total = 32 // NCORES
    npl = SEQ_PER_LAUNCH
    if npl not in _NC_CACHE:
        _NC_CACHE[npl] = build(nseq=npl)
    nc = _NC_CACHE[npl]
    consts = host_consts()
    xs = np.ascontiguousarray(inputs["x"], dtype=np.float32)
    base = {k: np.ascontiguousarray(v, dtype=np.float32) for k, v in inputs.items() if k != "x"}
    base.update(consts)
    outs = [[None] * (nseq_total // npl) for _ in range(NCORES)]
    for r in range(nseq_total // npl):
        in_maps = []
        for c in range(NCORES):
            m = dict(base)
            lo = c * nseq_total + r * npl
            m["x"] = xs[lo:lo + npl]
            in_maps.append(m)
        res = run_bass_kernel_spmd(nc, in_maps, core_ids=list(range(NCORES)))
        for c in range(NCORES):
            outs[c][r] = np.asarray(res.results[c]["out"], dtype=np.float32)
    return np.concatenate([o for c in range(NCORES) for o in outs[c]], axis=0).astype(np.float32)
```

```python
import numpy as np
from contextlib import ExitStack
import concourse.bass as bass
import concourse.mybir as mybir
from concourse.bass_utils import run_bass_kernel_spmd

F32 = mybir.dt.float32
BF16 = mybir.dt.bfloat16
AF = mybir.ActivationFunctionType
ALU = mybir.AluOpType
AX = mybir.AxisListType

S = 2048
NT = 16
D = 1024
KC = 8
DEPTH = 2
NCORES = 8
ALPHA = float((2 * DEPTH) ** 0.25)
EPS = 1e-6
NEG = -1.0e30
ENGS = ("pe", "act", "dve", "pool", "sp")
NBIS = 22
DBG = {"dsa_tiles": NT, "dsa_stage": 4, "att": 9}


class Op:
    __slots__ = ("eng", "fn", "deps", "dma", "need", "sig", "waits")

    def __init__(self, eng, fn, deps, dma):
        self.eng = eng
        self.fn = fn
        self.deps = deps
        self.dma = dma
        self.need = False
        self.sig = None
        self.waits = None


class Prog:
    def __init__(self):
        self.ops = []
        self.gen = {}
        self.fence = {}
        self.last_eng = {}
        self.last_dma = {}

    def add(self, eng, fn, r=(), w=(), dma=None):
        idx = len(self.ops)
        deps = {}
        for k in r:
            if isinstance(k, tuple) and k and k[0] == "ps":
                g = self.gen.get(k)
                if g:
                    for d in g[1]:
                        deps.setdefault(d, False)
        for k in r:
            g = self.gen.get(k)
            if g:
                for d in g[0]:
                    deps[d] = True
        for k in w:
            g = self.gen.get(k)
            if g:
                for d in g[1]:
                    deps.setdefault(d, False)
        for d in self.fence.values():
            deps.setdefault(d, False)
        for k in r:
            self.gen.setdefault(k, [[], []])[1].append(idx)
        for k in w:
            g = self.gen.setdefault(k, [[], []])
            if g[1]:
                g[0] = [idx]
                g[1] = []
            else:
                g[0].append(idx)
        self.ops.append(Op(eng, fn, deps, dma))
        if dma is None:
            self.last_eng[eng] = idx
        else:
            self.last_dma[dma[0]] = idx
        return idx

    def barrier(self):
        f = {}
        for e, i in self.last_eng.items():
            f[("e", e)] = i
        for s, i in self.last_dma.items():
            f[("d", s)] = i
        self.fence = f

    def emit(self, nc, stack):
        ops = self.ops
        for o in ops:
            o.waits = []
            for d, raw in o.deps.items():
                p = ops[d]
                if p.dma is None and p.eng == o.eng and o.dma is None and o.eng == "pe":
                    continue
                o.waits.append(d)
                p.need = True
        dcount = {}
        dround_end = {}
        for o in ops:
            if o.dma is not None:
                s, rd = o.dma
                dcount[s] = dcount.get(s, 0) + 1
                dround_end[(s, rd)] = dcount[s]
        sems = {}

        def getsem(name):
            if name not in sems:
                sems[name] = stack.enter_context(nc.semaphore("s_" + name))
            return sems[name]

        LIM = 24000
        cnt = {e: 0 for e in ENGS}
        for o in ops:
            if o.dma is not None:
                s, rd = o.dma
                o.sig = (getsem("d_" + s), 16 * dround_end[(s, rd)])
            elif o.need:
                c = cnt[o.eng]
                ep = c // LIM
                o.sig = (getsem("%s%d" % (o.eng, ep)), c % LIM + 1)
                cnt[o.eng] = c + 1
        per = {e: [] for e in ENGS}
        for o in ops:
            per[o.eng].append(o)

        def run(e, lst):
            waited = {}
            for o in lst:
                for d in o.waits:
                    sem, val = ops[d].sig
                    key = id(sem)
                    if waited.get(key, 0) >= val:
                        continue
                    waited[key] = val
                    e.wait_ge(sem, val)
                ins = o.fn(e)
                if o.dma is not None:
                    ins.then_inc(o.sig[0], 16)
                elif o.need:
                    ins.then_inc(o.sig[0], 1)

        with nc.Block() as block:
            @block.tensor
            def _(e):
                run(e, per["pe"])

            @block.scalar
            def _(e):
                run(e, per["act"])

            @block.vector
            def _(e):
                run(e, per["dve"])

            @block.gpsimd
            def _(e):
                run(e, per["pool"])

            @block.sync
            def _(e):
                run(e, per["sp"])


class Arena:
    def __init__(self, tens, n):
        self.t = tens
        self.n = n
        self.top = 0
        self.peak = 0

    def alloc(self, dtype, *shape):
        n = 1
        for s in shape:
            n *= s
        size = n * (2 if dtype == F32 else 1)
        size = (size + 31) // 32 * 32
        off = self.top
        self.top += size
        self.peak = max(self.peak, self.top)
        assert self.top <= self.n, ("arena overflow", self.top, self.n)
        ap = self.t[:, off:off + (n * 2 if dtype == F32 else n)]
        if dtype == F32:
            ap = ap.bitcast(F32)
        if len(shape) > 1:
            names = "abcdefg"[: len(shape)]
            pat = "p (%s) -> p %s" % (" ".join(names), " ".join(names))
            kw = {names[i]: shape[i] for i in range(len(shape))}
            ap = ap.rearrange(pat, **kw)
        return ap

    def view(self, off, dtype, *shape):
        save = self.top
        self.top = off
        ap = self.alloc(dtype, *shape)
        end = self.top
        self.top = save
        return ap, end

    def mark(self):
        return self.top

    def release(self, m):
        self.top = m


class K:
    pass


def MM(P, out, lhsT, rhs, start, stop, r, w, skip=False):
    if skip:
        P.add("pe", lambda e: e.matmul(out, lhsT=lhsT, rhs=rhs, start=start, stop=stop, skip_group_check=True), r, w)
    else:
        P.add("pe", lambda e: e.matmul(out, lhsT=lhsT, rhs=rhs, start=start, stop=stop), r, w)


def TR(P, out, in_, ident, r, w):
    P.add("pe", lambda e: e.transpose(out, in_, ident), r, w)


def ACTV(P, out, in_, func, r, w, bias=None, scale=None, accum=None):
    kw = {}
    if bias is not None:
        kw["bias"] = bias
    if scale is not None:
        kw["scale"] = scale
    if accum is not None:
        kw["accum_out"] = accum
    P.add("act", lambda e: e.activation(out=out, in_=in_, func=func, **kw), r, w)


def TT(P, eng, out, in0, in1, op, r, w):
    P.add(eng, lambda e: e.tensor_tensor(out=out, in0=in0, in1=in1, op=op), r, w)


def TS(P, eng, out, in0, s1, s2, op0, op1, r, w, accum=None):
    if op1 is None:
        P.add(eng, lambda e: e.tensor_scalar(out=out, in0=in0, scalar1=s1, scalar2=0.0, op0=op0, op1=ALU.add), r, w)
    elif accum is None:
        P.add(eng, lambda e: e.tensor_scalar(out=out, in0=in0, scalar1=s1, scalar2=s2, op0=op0, op1=op1), r, w)
    else:
        P.add(eng, lambda e: e.tensor_scalar(out=out, in0=in0, scalar1=s1, scalar2=s2, op0=op0, op1=op1,
                                             accum_out=accum), r, w)


def STT(P, eng, out, in0, scalar, in1, op0, op1, r, w):
    P.add(eng, lambda e: e.scalar_tensor_tensor(out=out, in0=in0, scalar=scalar, in1=in1, op0=op0, op1=op1), r, w)


def CP(P, eng, out, in_, r, w):
    if eng == "act":
        P.add("act", lambda e: e.copy(out=out, in_=in_), r, w)
    else:
        P.add(eng, lambda e: e.tensor_copy(out=out, in_=in_), r, w)


def RED(P, out, in_, op, r, w, absv=False):
    if absv:
        P.add("dve", lambda e: e.tensor_reduce(out=out, in_=in_, axis=AX.X, op=op, apply_absolute_value=True), r, w)
    else:
        P.add("dve", lambda e: e.tensor_reduce(out=out, in_=in_, axis=AX.X, op=op), r, w)


def RECIP(P, out, in_, r, w):
    P.add("dve", lambda e: e.reciprocal(out=out, in_=in_), r, w)


def MSET(P, eng, ap, val, r, w):
    P.add(eng, lambda e: e.memset(ap, val), r, w)


CUR = {"sq": 0}


def DMA(P, q, out, in_, stream, rnd, r, w):
    P.add(q, lambda e: e.dma_start(out=out, in_=in_), r, w, dma=("%s_%s_q%d" % (q, stream, CUR["sq"] % 2), rnd))


def build(nseq=4, nlayers=DEPTH, dbg=None, stop_after=None):
    dbg = dbg or set()
    nc = bass.Bass("TRN2", target_bir_lowering=False)
    L = DEPTH

    def din(name, shape):
        return nc.dram_tensor(name, list(shape), F32, kind="ExternalInput").ap()

    x = din("x", (nseq, S, D))
    w_in = din("w_in", (L, D, 3944))
    q_norm_g = din("q_norm_g", (L, 256))
    w_uq = din("w_uq", (L, 256, 768))
    kv_norm_g = din("kv_norm_g", (L, 128))
    w_ukv = din("w_ukv", (L, 128, 1024))
    w_gate = din("w_gate", (L, D, 3072))
    b_gate = din("b_gate", (L, 3072))
    w_a = din("w_a", (L, 512, D))
    w_b = din("w_b", (L, 256, D))
    w_c = din("w_c", (L, 512, D))
    w_o = din("w_o", (L, D, D))
    ln1_g = din("ln1_g", (L, D))
    ln1_b = din("ln1_b", (L, D))
    w_group = din("w_group", (L, D, 4))
    b_group = din("b_group", (L, 4))
    w_sub = din("w_sub", (L, D, 32))
    b_sub = din("b_sub", (L, 32))
    w1 = din("w1", (L, 32, D, 256))
    w3 = din("w3", (L, 32, D, 256))
    w2 = din("w2", (L, 32, 256, D))
    ln2_g = din("ln2_g", (L, D))
    ln2_b = din("ln2_b", (L, D))
    c_ident = din("c_ident", (128, 128))
    c_masks = din("c_masks", (128, 7 * 128))
    c_negm = din("c_negm", (128, 128))
    c_ropep = din("c_ropep", (128, NT * 32))
    c_ropem = din("c_ropem", (128, NT * 64))
    out = nc.dram_tensor("out", [nseq, S, D], F32, kind="ExternalOutput").ap()
    dbg_t = {}
    for name, shape in (("d_oc", (128, 4 * S)), ("d_oa", (128, 4 * S)), ("d_ob", (128, 2 * S)),
                        ("d_x1", (S, D)), ("d_sc", (128, S)), ("d_comb", (128, NT * 32))):
        if name in dbg:
            dbg_t[name] = nc.dram_tensor(name, list(shape), F32, kind="ExternalOutput").ap()

    P = Prog()
    stack = ExitStack()
    with stack:
        ARN = 106000
        arena_t = stack.enter_context(nc.sbuf_tensor("arena", [128, ARN], BF16))
        A = Arena(arena_t, ARN)
        psb = [stack.enter_context(nc.psum_tensor("psb%d" % i, [128, 512], F32)) for i in range(8)]

        def ps(b):
            return psb[b][:, :]

        def psbf(b):
            return psb[b][:, :].bitcast(BF16)

        BIG = A.alloc(F32, NT, D)
        XT = A.alloc(BF16, KC, S)
        identb = A.alloc(BF16, 128)
        identf = A.alloc(F32, 128)
        masks = A.alloc(BF16, 7, 128)
        negm = A.alloc(F32, 128)
        ropep = A.alloc(F32, NT, 32)
        ropem = A.alloc(F32, NT, 64)
        onesf = A.alloc(F32, 128)
        pow2 = A.alloc(F32, NBIS)

        DMA(P, "sp", identf, c_ident, "const", 0, [], ["identf"])
        DMA(P, "pool", identb, c_ident, "constb", 0, [], ["identb"])
        DMA(P, "pool", masks, c_masks.rearrange("p (m t) -> p m t", m=7), "constb", 0, [], ["masks"])
        DMA(P, "sp", negm, c_negm, "const", 0, [], ["negm"])
        DMA(P, "sp", ropep, c_ropep.rearrange("p (i c) -> p i c", i=NT), "const", 0, [], ["ropep"])
        DMA(P, "sp", ropem, c_ropem.rearrange("p (i c) -> p i c", i=NT), "const", 0, [], ["ropem"])
        MSET(P, "dve", onesf, 1.0, [], ["onesf"])
        for k in range(NBIS):
            MSET(P, "dve", pow2[:, k:k + 1], float(2.0 ** -k), [], ["pow2"])

        M_CAUS, M_PREV, M_R4D, M_R4M, M_R4F, M_R16D, M_R16O = range(7)

        rot = {}

        def slot(name, n):
            v = rot.get(name, 0)
            rot[name] = v + 1
            return v % n

        def to_XT(i, xb_tiles):
            sl = slot("xb", 2)
            xb = xb_tiles[sl]
            CP(P, "dve", xb, BIG[:, i, :], [("BIG", i)], [("xb", sl)])
            b = 6 + slot("tp", 2)
            pv = psbf(b).rearrange("p (k t) -> p k t", k=8)
            for kc in range(KC):
                TR(P, pv[:, kc, :], xb[:, kc * 128:(kc + 1) * 128], identb, [("xb", sl), "identb"], [("ps", b)])
            CP(P, "act", XT[:, :, i * 128:(i + 1) * 128], pv, [("ps", b)], [("XT", i)])

        def proj(i, wt, c0, ncols, pout, wkey, b, start=True, stop=True):
            for kc in range(KC):
                MM(P, pout, XT[:, kc, i * 128:(i + 1) * 128], wt[:, kc, c0:c0 + ncols],
                   start and kc == 0, stop and kc == KC - 1, [("XT", i), wkey], [("ps", b)])

        def rope(src, dst, H, hd, half, table, i, rkeys, wkeys, tmp, tkey):
            r2 = 2 * half
            cc = table[:, i, 0:r2].unsqueeze(1).to_broadcast([128, H, r2])
            ns = table[:, i, r2:r2 + half].unsqueeze(1).to_broadcast([128, H, half])
            ps_ = table[:, i, r2 + half:r2 + 2 * half].unsqueeze(1).to_broadcast([128, H, half])
            u = tmp[0][:, 0:H * r2].rearrange("p (h c) -> p h c", h=H)
            v = tmp[1][:, 0:H * r2].rearrange("p (h c) -> p h c", h=H)
            TT(P, "dve", u[:, :, 0:half], src[:, :, half:r2], ns, ALU.mult, rkeys + ["rtu"], ["rtu"])
            TT(P, "dve", u[:, :, half:r2], src[:, :, 0:half], ps_, ALU.mult, rkeys + ["rtu"], ["rtu"])
            TT(P, "dve", v, src[:, :, 0:r2], cc, ALU.mult, rkeys + ["rtv"], ["rtv"])
            TT(P, "dve", dst[:, :, 0:r2], u, v, ALU.add, ["rtu", "rtv"], wkeys)
            if hd > r2:
                CP(P, "dve" if H > 1 else "act", dst[:, :, r2:hd], src[:, :, r2:hd], rkeys, wkeys)

        def layer_norm(i, g_rep, b_rep, gkey, tl):
            xt_ = BIG[:, i, :]
            kB = ("BIG", i)
            st = tl["st"]
            sk = "lnst"
            RED(P, st[:, 0:1], xt_, ALU.add, [kB], [sk + "0"])
            TS(P, "dve", st[:, 1:2], st[:, 0:1], -1.0 / D, None, ALU.mult, None, [sk + "0"], [sk + "1"])
            ACTV(P, xt_, xt_, AF.Identity, [kB, sk + "1"], [kB], bias=st[:, 1:2])
            ACTV(P, tl["junk"], xt_, AF.Square, [kB], ["lnjunk", sk + "2"], accum=st[:, 2:3])
            ACTV(P, st[:, 3:4], st[:, 2:3], AF.Sqrt, [sk + "2"], [sk + "3"], bias=EPS, scale=1.0 / D)
            RECIP(P, st[:, 4:5], st[:, 3:4], [sk + "3"], [sk + "4"])
            STT(P, "dve", xt_, xt_, st[:, 4:5], g_rep, ALU.mult, ALU.mult, [kB, sk + "4", gkey], [kB])
            TT(P, "dve", xt_, xt_, b_rep, ALU.add, [kB, gkey], [kB])

        def attn_tile(i, pairs, qk_fn, v_fn, scale, Ptiles, nh=4):
            ob = 4 + slot("O", 2)
            Ov = ps(ob)[:, 0:nh * 65].rearrange("p (h c) -> p h c", h=nh)
            n = len(pairs)

            def emit_qk(idx):
                j = pairs[idx][0]
                sb_ = 2 + slot("S", 2)
                Sv = ps(sb_).rearrange("p (h t) -> p h t", h=4)
                for (h0, nhh, lhsT, rhs, rk) in qk_fn(j):
                    MM(P, Sv[:, h0:h0 + nhh, :], lhsT, rhs, True, True, rk, [("ps", sb_)])
                return sb_, Sv

            cur = emit_qk(0)
            for idx, (j, mk, mkey) in enumerate(pairs):
                sb_, Sv = cur
                if idx + 1 < n:
                    cur = emit_qk(idx + 1)
                psl = slot("P", 3)
                Pt = Ptiles[psl]
                ACTV(P, Pt[:, 0:nh, :], Sv[:, 0:nh, :], AF.Exp, [("ps", sb_)], [("P", psl)], scale=scale)
                if mk is not None:
                    TT(P, "dve", Pt[:, 0:nh, :], Pt[:, 0:nh, :], mk.unsqueeze(1).to_broadcast([128, nh, 128]),
                       ALU.mult, [("P", psl), mkey], [("P", psl)])
                for hh in range(nh):
                    rv, rk = v_fn(hh, j)
                    MM(P, Ov[:, hh, :], Pt[:, hh, :], rv, idx == 0 and hh == 0, idx == n - 1, [("P", psl)] + rk,
                       [("ps", ob)], skip=True)
            return Ov, ob

        def normalize_out(Ov, ob, dst, nh, tl):
            sl = slot("rc", 2)
            rc = tl["rc"][sl]
            RECIP(P, rc[:, 0:nh], Ov[:, :, 64], [("ps", ob)], [("rc", sl)])
            TT(P, "dve", dst, Ov[:, :, 0:64], rc[:, 0:nh].unsqueeze(2).to_broadcast([128, nh, 64]), ALU.mult,
               [("ps", ob), ("rc", sl)], ["normdst"])

        def transposes_to(src_bf, nblk, width, dst_ap, rkeys, wkeys, rows=128):
            b = 6 + slot("tp", 2)
            pv = psbf(b).rearrange("p (k t) -> p k t", k=8)
            for k in range(nblk):
                TR(P, pv[0:width, k, :], src_bf[:, k * width:(k + 1) * width], identb, rkeys + ["identb"], [("ps", b)])
            CP(P, "act" if rows == 128 else "dve", dst_ap, pv[0:rows, 0:nblk, :], [("ps", b)], wkeys)

        class Stop(Exception):
            pass

        def chk(name):
            if stop_after == name:
                raise Stop()

        def seq_layer(sq, l):
            CUR["sq"] = sq
            if True:
                tag = "s%dl%d" % (sq, l)
                win_v = w_in[l].rearrange("(kc p) c -> p kc c", p=128)
                m_layer = A.mark()
                if l == 0:
                    m0 = A.mark()
                    xb_tiles = [A.alloc(BF16, D) for _ in range(2)]
                    for i in range(NT):
                        DMA(P, "sp", BIG[:, i, :], x[sq, i * 128:(i + 1) * 128, :], "x", sq, [], [("BIG", i)])
                    for i in range(NT):
                        to_XT(i, xb_tiles)
                    P.barrier()
                    A.release(m0)
                    chk("load")

                OTc = A.alloc(BF16, 4, S)
                m0 = A.mark()
                WQ = A.alloc(BF16, KC, 1024)
                WK = A.alloc(BF16, KC, 200)
                KI = A.alloc(BF16, 2, S)
                VC = A.alloc(BF16, NT, 72)
                WAb = A.alloc(F32, NT, 8)
                SG = A.alloc(F32, NT, 8)
                SC = A.alloc(F32, S)
                MK = A.alloc(BF16, S)
                MKT = A.alloc(BF16, NT, 128)
                RL = [A.alloc(F32, 512) for _ in range(2)]
                QTi = [A.alloc(BF16, 8, 128) for _ in range(2)]
                IQTi = [A.alloc(BF16, 8, 128) for _ in range(2)]
                QB = [A.alloc(BF16, 512) for _ in range(2)]
                IQB = [A.alloc(BF16, 512) for _ in range(2)]
                KK = [A.alloc(BF16, 128) for _ in range(2)]
                Pt = [A.alloc(BF16, 4, 128) for _ in range(3)]
                rtmp = [A.alloc(F32, 256) for _ in range(2)]
                tl = {"rc": [A.alloc(F32, 8) for _ in range(2)]}
                OCb = [A.alloc(BF16, 8, 64) for _ in range(2)]
                bis = A.alloc(F32, 8)
                wtab = A.alloc(F32, NBIS)

                DMA(P, "pool", WK[:, :, 0:128], win_v[:, :, 3232:3360], "wk", tag, [], ["WK"])
                DMA(P, "pool", WK[:, :, 128:200], win_v[:, :, 3872:3944], "wk", tag, [], ["WK"])
                DMA(P, "pool", WQ[:, :, 0:512], win_v[:, :, 2720:3232], "wq", tag, [], ["WQ"])
                DMA(P, "pool", WQ[:, :, 512:1024], win_v[:, :, 3360:3872], "wq", tag, [], ["WQ"])
                CP(P, "dve", VC[:, :, 64:65], onesf[:, 0:NT].unsqueeze(2), ["onesf"], ["VCones"])
                for i in range(NT):
                    b = slot("pj", 2)
                    pv = ps(b)
                    proj(i, WK, 0, 128, pv[:, 0:128], "WK", b)
                    proj(i, WK, 128, 72, pv[:, 128:200], "WK", b)
                    sl = slot("KK", 2)
                    kk = KK[sl]
                    rope(pv[:, 0:64].unsqueeze(1), kk[:, 0:64].unsqueeze(1), 1, 64, 8, ropep, i,
                         [("ps", b), "ropep"], [("KK", sl)], rtmp, "rtA")
                    rope(pv[:, 128:192].unsqueeze(1), kk[:, 64:128].unsqueeze(1), 1, 64, 8, ropep, i,
                         [("ps", b), "ropep"], [("KK", sl)], rtmp, "rtB")
                    CP(P, "dve", VC[:, i, 0:64], pv[:, 64:128], [("ps", b)], [("VC", i)])
                    ACTV(P, WAb[:, i, :], pv[:, 192:200], AF.Abs, [("ps", b)], [("WA", i)])
                    ACTV(P, SG[:, i, :], pv[:, 192:200], AF.Sign, [("ps", b)], [("SG", i)])
                    tb = 6 + slot("tp", 2)
                    tv = psbf(tb).rearrange("p (k t) -> p k t", k=8)
                    TR(P, tv[0:64, 0, :], kk[:, 0:64], identb, [("KK", sl), "identb"], [("ps", tb)])
                    TR(P, tv[0:64, 1, :], kk[:, 64:128], identb, [("KK", sl), "identb"], [("ps", tb)])
                    CP(P, "dve", KI[0:64, :, i * 128:(i + 1) * 128], tv[0:64, 0:2, :], [("ps", tb)], [("KI", i)])

                chk("dsa_pro")

                def dsa_qprep(i):
                    sl = slot("dq", 2)
                    for (c0, dstb, dstT, nm) in ((0, QB[sl], QTi[sl], "q"), (512, IQB[sl], IQTi[sl], "iq")):
                        b = slot("pj", 2)
                        pv = ps(b)
                        proj(i, WQ, c0, 512, pv, "WQ", b)
                        rope(pv.rearrange("p (h c) -> p h c", h=8), dstb.rearrange("p (h c) -> p h c", h=8),
                             8, 64, 8, ropep, i, [("ps", b), "ropep"], [(nm + "B", sl)], rtmp, "rtQ")
                        transposes_to(dstb, 8, 64, dstT[0:64, :, :], [(nm + "B", sl)], [(nm + "T", sl)], rows=64)
                    return sl

                nxt = dsa_qprep(0)
                for i in range(DBG["dsa_tiles"]):
                    sl = nxt
                    hi = (i + 1) * 128
                    for h in range(8):
                        for c0 in range(0, hi, 512):
                            c1 = min(hi, c0 + 512)
                            sb_ = 2 + slot("S", 2)
                            Rv = ps(sb_)[:, 0:c1 - c0]
                            jkeys = [("KI", jj) for jj in range(c0 // 128, c1 // 128)]
                            MM(P, Rv, IQTi[sl][0:64, h, :], KI[0:64, 1, c0:c1], True, True,
                               [("iqT", sl)] + jkeys, [("ps", sb_)])
                            rs = slot("RL", 2)
                            ACTV(P, RL[rs][:, 0:c1 - c0], Rv, AF.Relu, [("ps", sb_), ("WA", i)], [("RL", rs)],
                                 scale=WAb[:, i, h:h + 1])
                            if h == 0:
                                TS(P, "dve", SC[:, c0:c1], RL[rs][:, 0:c1 - c0], SG[:, i, h:h + 1], None, ALU.mult, None,
                                   [("RL", rs), ("SG", i)], [("SC", c0)])
                            else:
                                STT(P, "dve", SC[:, c0:c1], RL[rs][:, 0:c1 - c0], SG[:, i, h:h + 1], SC[:, c0:c1],
                                    ALU.mult, ALU.add, [("RL", rs), ("SG", i), ("SC", c0)], [("SC", c0)])
                    sckeys = [("SC", c0) for c0 in range(0, hi, 512)]
                    if i + 1 < NT:
                        nxt = dsa_qprep(i + 1)
                    if DBG["dsa_stage"] < 2:
                        continue
                    if i >= 2:
                        RED(P, bis[:, 0:1], SC[:, 0:hi], ALU.max, sckeys, ["bisB"], absv=True)
                        TS(P, "dve", bis[:, 0:1], bis[:, 0:1], 1.0, None, ALU.add, None, ["bisB"], ["bisB"])
                        TS(P, "dve", bis[:, 1:2], bis[:, 0:1], -1.0, None, ALU.mult, None, ["bisB"], ["bislo"])
                        TS(P, "dve", wtab, pow2, bis[:, 0:1], None, ALU.mult, None, ["bisB", "pow2"], ["wtab"])
                    TT(P, "dve", SC[:, i * 128:hi], SC[:, i * 128:hi], negm, ALU.add,
                       [("SC", (i * 128) // 512 * 512), "negm"], [("SC", (i * 128) // 512 * 512)])
                    if i >= 2:
                        for k in range(NBIS):
                            TT(P, "dve", bis[:, 2:3], bis[:, 1:2], wtab[:, k:k + 1], ALU.add, ["bislo", "wtab"], ["bismid"])
                            TS(P, "dve", MK[:, 0:hi], SC[:, 0:hi], bis[:, 2:3], 0.0, ALU.is_ge, ALU.add,
                               sckeys + ["bismid"], ["MK", "biscnt"], accum=bis[:, 3:4])
                            TS(P, "dve", bis[:, 4:5], bis[:, 3:4], 256.0, wtab[:, k:k + 1], ALU.is_ge, ALU.mult,
                               ["biscnt", "wtab"], ["bisstep"])
                            TT(P, "dve", bis[:, 1:2], bis[:, 1:2], bis[:, 4:5], ALU.add, ["bislo", "bisstep"], ["bislo"])
                    else:
                        MSET(P, "dve", bis[:, 1:2], -1.0e29, ["bislo"], ["bislo"])
                    TS(P, "dve", MK[:, 0:hi], SC[:, 0:hi], bis[:, 1:2], None, ALU.is_ge, None, sckeys + ["bislo"], ["MK"])
                    if "d_sc" in dbg_t and sq == 0 and l == 0 and i == NT - 1:
                        DMA(P, "sp", dbg_t["d_sc"], SC, "dbg", ("dbg", len(P.ops)), sckeys, ["dbgout"])
                    if DBG["dsa_stage"] < 3:
                        continue
                    for j0 in range(0, i + 1, 8):
                        j1 = min(i + 1, j0 + 8)
                        tb = 6 + slot("tp", 2)
                        tv = psbf(tb).rearrange("p (k t) -> p k t", k=8)
                        for j in range(j0, j1):
                            TR(P, tv[:, j - j0, :], MK[:, j * 128:(j + 1) * 128], identb, ["MK", "identb"], [("ps", tb)])
                        CP(P, "act", MKT[:, j0:j1, :], tv[:, 0:j1 - j0, :], [("ps", tb)], [("MKT", j0)])
                    if DBG["dsa_stage"] < 4:
                        continue
                    osl = slot("OCb", 2)
                    for hg in range(2):
                        def qk_fn(j, hg=hg, sl=sl):
                            return [(0, 4, KI[0:64, 0, j * 128:(j + 1) * 128], QTi[sl][0:64, 4 * hg:4 * hg + 4, :],
                                     [("KI", j), ("qT", sl)])]

                        def v_fn(hh, j):
                            return VC[:, j, 0:65], [("VC", j), "VCones"]

                        pairs = [(j, MKT[:, j, :], ("MKT", j // 8 * 8)) for j in range(i + 1)]
                        Ov, ob = attn_tile(i, pairs, qk_fn, v_fn, 0.125, Pt)
                        if DBG["att"] < 4:
                            continue
                        normalize_out(Ov, ob, OCb[osl][:, 4 * hg:4 * hg + 4, :], 4, tl)
                    if DBG["att"] < 5:
                        continue
                    transposes_to(OCb[osl].rearrange("p h c -> p (h c)"), 4, 128, OTc[:, :, i * 128:(i + 1) * 128],
                                  ["normdst"], [("OTc", i)])
                if "d_oc" in dbg_t and sq == 0 and l == 0:
                    DMA(P, "pool", dbg_t["d_oc"].rearrange("p (k t) -> p k t", k=4), OTc, "dbg", ("dbg", len(P.ops)),
                        [("OTc", i) for i in range(NT)], ["dbgout"])
                P.barrier()
                A.release(m0)
                chk("dsa")


                OTb = A.alloc(BF16, 2, S)
                m0 = A.mark()
                ACC = A.alloc(F32, NT, 4, 65)
                WGq = A.alloc(BF16, KC, 256)
                WGkv = A.alloc(BF16, KC, 512)
                KT = A.alloc(BF16, NT, 4, 128)
                VA = A.alloc(BF16, NT, 4, 72)
                QTi = [A.alloc(BF16, 4, 128) for _ in range(2)]
                QB = [A.alloc(BF16, 256) for _ in range(2)]
                KB = [A.alloc(BF16, 256) for _ in range(2)]
                Pt = [A.alloc(BF16, 4, 128) for _ in range(3)]
                rtmp = [A.alloc(F32, 256) for _ in range(2)]
                tl = {"rc": [A.alloc(F32, 8) for _ in range(2)]}
                OBb = [A.alloc(BF16, 4, 64) for _ in range(2)]
                CP(P, "dve", VA[:, :, :, 64:65], onesf[:, 0:64].rearrange("p (a b) -> p a b", a=NT).unsqueeze(3),
                   ["onesf"], ["VAones"])
                for g in range(3):
                    c0g = 416 + 768 * g
                    gtag = tag + "g%d" % g
                    DMA(P, "pool", WGq, win_v[:, :, c0g:c0g + 256], "wgq", gtag, [], ["WGq"])
                    DMA(P, "pool", WGkv, win_v[:, :, c0g + 256:c0g + 768], "wgkv", gtag, [], ["WGkv"])
                    for i in range(NT):
                        b = slot("pj", 2)
                        pv = ps(b)
                        proj(i, WGkv, 0, 512, pv, "WGkv", b)
                        sl = slot("KB", 2)
                        rope(pv[:, 0:256].rearrange("p (h c) -> p h c", h=4), KB[sl].rearrange("p (h c) -> p h c", h=4),
                             4, 64, 8, ropep, i, [("ps", b), "ropep"], [("KB", sl)], rtmp, "rt")
                        CP(P, "dve", VA[:, i, :, 0:64], pv[:, 256:512].rearrange("p (h c) -> p h c", h=4),
                           [("ps", b)], [("VA", i)])
                        transposes_to(KB[sl], 4, 64, KT[0:64, i, :, :], [("KB", sl)], [("KT", i)], rows=64)

                    def dil_qprep(i):
                        sl = slot("dlq", 2)
                        b = slot("pj", 2)
                        pv = ps(b)
                        proj(i, WGq, 0, 256, pv[:, 0:256], "WGq", b)
                        rope(pv[:, 0:256].rearrange("p (h c) -> p h c", h=4), QB[sl].rearrange("p (h c) -> p h c", h=4),
                             4, 64, 8, ropep, i, [("ps", b), "ropep"], [("QBd", sl)], rtmp, "rt")
                        transposes_to(QB[sl], 4, 64, QTi[sl][0:64, :, :], [("QBd", sl)], [("qTd", sl)], rows=64)
                        return sl

                    nxt = dil_qprep(0)
                    for i in range(NT):
                        sl = nxt
                        if g == 0:
                            pl = [(i - 1, M_PREV), (i, M_CAUS)]
                        elif g == 1:
                            pl = [(i - 4, M_R4F), (i - 3, M_R4M), (i - 2, M_R4M), (i - 1, M_R4M), (i, M_R4D)]
                        else:
                            pl = [(j, M_R16O) for j in range(i)] + [(i, M_R16D)]
                        pairs = [(j, masks[:, m, :], "masks") for (j, m) in pl if j >= 0]

                        def qk_fn(j, sl=sl):
                            return [(hh, 1, KT[0:64, j, hh, :], QTi[sl][0:64, hh, :], [("KT", j), ("qTd", sl)])
                                    for hh in range(4)]

                        def v_fn(hh, j):
                            return VA[:, j, hh, 0:65], [("VA", j), "VAones"]

                        if i + 1 < NT:
                            nxt = dil_qprep(i + 1)
                        Ov, ob = attn_tile(i, pairs, qk_fn, v_fn, 0.125, Pt)
                        if g == 0:
                            CP(P, "dve", ACC[:, i, :, :], Ov, [("ps", ob)], [("ACC", i)])
                        else:
                            TT(P, "dve", ACC[:, i, :, :], ACC[:, i, :, :], Ov, ALU.add, [("ps", ob), ("ACC", i)], [("ACC", i)])
                        if g == 2:
                            osl = slot("OBb", 2)
                            rsl = slot("rc", 2)
                            rc = tl["rc"][rsl]
                            RECIP(P, rc[:, 0:4], ACC[:, i, :, 64], [("ACC", i)], [("rc", rsl)])
                            TT(P, "dve", OBb[osl], ACC[:, i, :, 0:64], rc[:, 0:4].unsqueeze(2).to_broadcast([128, 4, 64]),
                               ALU.mult, [("ACC", i), ("rc", rsl)], [("OBb", osl)])
                            transposes_to(OBb[osl].rearrange("p h c -> p (h c)"), 2, 128, OTb[:, :, i * 128:(i + 1) * 128],
                                          [("OBb", osl)], [("OTb", i)])
                if "d_ob" in dbg_t and sq == 0 and l == 0:
                    DMA(P, "pool", dbg_t["d_ob"].rearrange("p (k t) -> p k t", k=2), OTb, "dbg", ("dbg", len(P.ops)),
                        [("OTb", i) for i in range(NT)], ["dbgout"])
                P.barrier()
                A.release(m0)
                chk("dil")

                OTa = A.alloc(BF16, 4, S)
                m0 = A.mark()
                CQK = A.alloc(BF16, 3, S)
                KRB = A.alloc(BF16, NT, 32)
                m1 = A.mark()
                W1 = A.alloc(BF16, KC, 416)
                gq = A.alloc(F32, 256)
                gkv = A.alloc(F32, 128)
                CQB = [A.alloc(BF16, 384) for _ in range(2)]
                junk = A.alloc(F32, 256)
                st = A.alloc(F32, 8)
                rtmp = [A.alloc(F32, 256) for _ in range(2)]
                DMA(P, "pool", W1, win_v[:, :, 0:416], "w1", tag, [], ["W1"])
                DMA(P, "sp", gq, q_norm_g[l].partition_broadcast(128), "gq", tag, [], ["gq"])
                DMA(P, "sp", gkv, kv_norm_g[l].partition_broadcast(128), "gq", tag, [], ["gkv"])
                for i in range(NT):
                    b = slot("pj", 2)
                    pv = ps(b)
                    proj(i, W1, 0, 416, pv[:, 0:416], "W1", b)
                    sl = slot("CQB", 2)
                    cqb = CQB[sl]
                    for (a0, a1, gg, gk, so) in ((0, 256, gq, "gq", 0), (256, 384, gkv, "gkv", 3)):
                        n_ = a1 - a0
                        ACTV(P, junk[:, 0:n_], pv[:, a0:a1], AF.Square, [("ps", b)], ["mjunk", "mst%d" % so],
                             accum=st[:, so:so + 1])
                        ACTV(P, st[:, so + 1:so + 2], st[:, so:so + 1], AF.Sqrt, ["mst%d" % so], ["mst%d" % (so + 1)],
                             bias=EPS, scale=1.0 / n_)
                        RECIP(P, st[:, so + 2:so + 3], st[:, so + 1:so + 2], ["mst%d" % (so + 1)], ["mst%d" % (so + 2)])
                        STT(P, "dve", cqb[:, a0:a1], pv[:, a0:a1], st[:, so + 2:so + 3], gg, ALU.mult, ALU.mult,
                            [("ps", b), "mst%d" % (so + 2), gk], [("CQB", sl)])
                    rope(pv[:, 384:416].unsqueeze(1), KRB[:, i, :].unsqueeze(1), 1, 32, 16, ropem, i,
                         [("ps", b), "ropem"], [("KRB", i)], rtmp, "rt")
                    transposes_to(cqb, 3, 128, CQK[:, :, i * 128:(i + 1) * 128], [("CQB", sl)], [("CQK", i)])
                P.barrier()
                A.release(m1)
                WUQ = A.alloc(BF16, 2, 768)
                WUKV = A.alloc(BF16, 1024)
                KT = A.alloc(BF16, NT, 4, 128)
                VA = A.alloc(BF16, NT, 4, 72)
                QTi = [A.alloc(BF16, 4, 128) for _ in range(2)]
                QH = [A.alloc(BF16, 4, 96) for _ in range(2)]
                KH = [A.alloc(BF16, 4, 96) for _ in range(2)]
                Pt = [A.alloc(BF16, 4, 128) for _ in range(3)]
                rtmp = [A.alloc(F32, 256) for _ in range(2)]
                tl = {"rc": [A.alloc(F32, 8) for _ in range(2)]}
                OAb = [A.alloc(BF16, 4, 64) for _ in range(2)]
                DMA(P, "pool", WUQ, w_uq[l].rearrange("(kc p) c -> p kc c", p=128), "wuq", tag, [], ["WUQ"])
                DMA(P, "pool", WUKV, w_ukv[l], "wuq", tag, [], ["WUKV"])
                CP(P, "dve", VA[:, :, :, 64:65], onesf[:, 0:64].rearrange("p (a b) -> p a b", a=NT).unsqueeze(3),
                   ["onesf"], ["VAones"])
                for u in range(2):
                    for i in range(NT):
                        b = slot("pj", 2)
                        pv = ps(b)
                        MM(P, pv, CQK[:, 2, i * 128:(i + 1) * 128], WUKV[:, 512 * u:512 * u + 512], True, True,
                           [("CQK", i), "WUKV"], [("ps", b)])
                        pv4 = pv.rearrange("p (h c) -> p h c", h=4)
                        sl = slot("KH", 2)
                        kh = KH[sl]
                        CP(P, "dve", kh[:, :, 0:64], pv4[:, :, 0:64], [("ps", b)], [("KH", sl)])
                        CP(P, "dve", kh[:, :, 64:96], KRB[:, i, :].unsqueeze(1).to_broadcast([128, 4, 32]),
                           [("KRB", i)], [("KH", sl)])
                        CP(P, "dve", VA[:, i, :, 0:64], pv4[:, :, 64:128], [("ps", b)], [("VA", i)])
                        transposes_to(kh.rearrange("p h c -> p (h c)"), 4, 96, KT[0:96, i, :, :], [("KH", sl)],
                                      [("KT", i)], rows=96)

                    def mla_qprep(i, u=u):
                        sl = slot("mq", 2)
                        b = slot("pj", 2)
                        pv = ps(b)
                        for kc in range(2):
                            MM(P, pv[:, 0:384], CQK[:, kc, i * 128:(i + 1) * 128], WUQ[:, kc, 384 * u:384 * u + 384],
                               kc == 0, kc == 1, [("CQK", i), "WUQ"], [("ps", b)])
                        pv4 = pv[:, 0:384].rearrange("p (h c) -> p h c", h=4)
                        qh = QH[sl]
                        CP(P, "dve", qh[:, :, 0:64], pv4[:, :, 0:64], [("ps", b)], [("QH", sl)])
                        rope(pv4[:, :, 64:96], qh[:, :, 64:96], 4, 32, 16, ropem, i, [("ps", b), "ropem"], [("QH", sl)],
                             rtmp, "rt")
                        transposes_to(qh.rearrange("p h c -> p (h c)"), 4, 96, QTi[sl][0:96, :, :], [("QH", sl)],
                                      [("qTm", sl)], rows=96)
                        return sl

                    nxt = mla_qprep(0)
                    for i in range(NT):
                        sl = nxt
                        pairs = [(j, None, None) for j in range(i)] + [(i, masks[:, M_CAUS, :], "masks")]

                        def qk_fn(j, sl=sl):
                            return [(hh, 1, KT[0:96, j, hh, :], QTi[sl][0:96, hh, :], [("KT", j), ("qTm", sl)])
                                    for hh in range(4)]

                        def v_fn(hh, j):
                            return VA[:, j, hh, 0:65], [("VA", j), "VAones"]

                        if i + 1 < NT:
                            nxt = mla_qprep(i + 1)
                        Ov, ob = attn_tile(i, pairs, qk_fn, v_fn, float(96 ** -0.5), Pt)
                        osl = slot("OAb", 2)
                        rsl = slot("rc", 2)
                        rc = tl["rc"][rsl]
                        RECIP(P, rc[:, 0:4], Ov[:, :, 64], [("ps", ob)], [("rc", rsl)])
                        TT(P, "dve", OAb[osl], Ov[:, :, 0:64], rc[:, 0:4].unsqueeze(2).to_broadcast([128, 4, 64]), ALU.mult,
                           [("ps", ob), ("rc", rsl)], [("OAb", osl)])
                        transposes_to(OAb[osl].rearrange("p h c -> p (h c)"), 2, 128,
                                      OTa[:, 2 * u:2 * u + 2, i * 128:(i + 1) * 128], [("OAb", osl)], [("OTa", i, u)])
                if "d_oa" in dbg_t and sq == 0 and l == 0:
                    DMA(P, "pool", dbg_t["d_oa"].rearrange("p (k t) -> p k t", k=4), OTa, "dbg", ("dbg", len(P.ops)),
                        [("OTa", i, u) for i in range(NT) for u in range(2)], ["dbgout"])
                P.barrier()
                A.release(m0)
                chk("mla")

                MT = A.alloc(BF16, KC, S)
                m0 = A.mark()
                WGc = A.alloc(BF16, KC, 3, 256)
                WAc = A.alloc(BF16, 4, 256)
                WBc = A.alloc(BF16, 2, 256)
                WCc = A.alloc(BF16, 4, 256)
                bgc = A.alloc(F32, 3, 256)
                G = [A.alloc(F32, 768) for _ in range(2)]
                Mf = [A.alloc(F32, 256) for _ in range(2)]
                MB = [A.alloc(BF16, 256) for _ in range(2)]
                wgate_v = w_gate[l].rearrange("(kc p) c -> p kc c", p=128)
                wa_v = w_a[l].rearrange("(kc p) c -> p kc c", p=128)
                wb_v = w_b[l].rearrange("(kc p) c -> p kc c", p=128)
                wc_v = w_c[l].rearrange("(kc p) c -> p kc c", p=128)
                for c in range(4):
                    ctag = tag + "c%d" % c
                    for m in range(3):
                        DMA(P, "pool", WGc[:, :, m, :], wgate_v[:, :, m * 1024 + 256 * c:m * 1024 + 256 * c + 256],
                            "wgc", ctag, [], ["WGc"])
                        DMA(P, "sp", bgc[0:1, m, :], b_gate[l, m * 1024 + 256 * c:m * 1024 + 256 * c + 256].unsqueeze(0),
                            "bgc", ctag, [], ["bgc"])
                    DMA(P, "pool", WAc, wa_v[:, :, 256 * c:256 * c + 256], "wgc", ctag, [], ["WAc"])
                    DMA(P, "pool", WBc, wb_v[:, :, 256 * c:256 * c + 256], "wgc", ctag, [], ["WBc"])
                    DMA(P, "pool", WCc, wc_v[:, :, 256 * c:256 * c + 256], "wgc", ctag, [], ["WCc"])
                    for i in range(NT):
                        tsl = slice(i * 128, (i + 1) * 128)
                        base = 3 * (i % 2)
                        bkA, bkB, bkC = base, base + 1, base + 2
                        for m in range(3):
                            if m < 2:
                                gb, go = bkA, ps(bkA)[:, m * 256:m * 256 + 256]
                            else:
                                gb, go = bkB, ps(bkB)[:, 0:256]
                            for kc in range(KC):
                                MM(P, go, XT[:, kc, tsl], WGc[:, kc, m, :], kc == 0, False, [("XT", i), "WGc"], [("ps", gb)])
                            MM(P, go, onesf[0:1, 0:128], bgc[0:1, m, :], False, True, ["onesf", "bgc"], [("ps", gb)])
                        for kc in range(4):
                            MM(P, ps(bkB)[:, 256:512], OTc[:, kc, tsl], WCc[:, kc, :], kc == 0, kc == 3,
                               [("OTc", i), "WCc"], [("ps", bkB)])
                        for kc in range(4):
                            MM(P, ps(bkC)[:, 0:256], OTa[:, kc, tsl], WAc[:, kc, :], kc == 0, kc == 3,
                               [("OTa", i, 0), ("OTa", i, 1), "WAc"], [("ps", bkC)])
                        for kc in range(2):
                            MM(P, ps(bkC)[:, 256:512], OTb[:, kc, tsl], WBc[:, kc, :], kc == 0, kc == 1,
                               [("OTb", i), "WBc"], [("ps", bkC)])
                        gs = slot("G", 2)
                        ACTV(P, G[gs][:, 0:512], ps(bkA), AF.Sigmoid, [("ps", bkA)], [("G", gs)])
                        ACTV(P, G[gs][:, 512:768], ps(bkB)[:, 0:256], AF.Sigmoid, [("ps", bkB)], [("G", gs)])
                        ms = slot("Mf", 2)
                        TT(P, "dve", G[gs][:, 0:512], G[gs][:, 0:512], ps(bkC), ALU.mult, [("G", gs), ("ps", bkC)], [("G", gs)])
                        TT(P, "dve", G[gs][:, 512:768], G[gs][:, 512:768], ps(bkB)[:, 256:512], ALU.mult,
                           [("G", gs), ("ps", bkB)], [("G", gs)])
                        TT(P, "dve", Mf[ms], G[gs][:, 0:256], G[gs][:, 256:512], ALU.add, [("G", gs)], [("Mf", ms)])
                        TT(P, "dve", MB[ms], Mf[ms], G[gs][:, 512:768], ALU.add, [("G", gs), ("Mf", ms)], [("MB", ms)])
                        transposes_to(MB[ms], 2, 128, MT[:, 2 * c:2 * c + 2, tsl], [("MB", ms)], [("MT", i, c)])
                P.barrier()
                A.release(m0)
                chk("merge1")

                off_c = A.mark() - 8 * S - 4 * S - 2 * S - 4 * S
                WO, e_ = A.view(off_c, BF16, KC, D)
                off_b = off_c + 4 * S
                stv, e_ = A.view(off_b, F32, 8)
                off_a = off_b + 2 * S
                L1G, e_ = A.view(off_a, F32, D)
                L1B, e_ = A.view(e_, F32, D)
                junkL, e_ = A.view(e_, F32, D)
                xb0, e_ = A.view(e_, BF16, D)
                xb1, e_ = A.view(e_, BF16, D)
                assert e_ <= off_a + 4 * S
                DMA(P, "pool", WO, w_o[l].rearrange("(kc p) c -> p kc c", p=128), "wo", tag, [], ["WO"])
                DMA(P, "sp", L1G, ln1_g[l].partition_broadcast(128), "ln1", tag, [], ["L1"])
                DMA(P, "sp", L1B, ln1_b[l].partition_broadcast(128), "ln1", tag, [], ["L1"])
                tlL = {"st": stv, "junk": junkL}
                for i in range(NT):
                    tsl = slice(i * 128, (i + 1) * 128)
                    for ch in range(2):
                        yb = 2 * (i % 2) + ch
                        for kc in range(KC):
                            MM(P, ps(yb), MT[:, kc, tsl], WO[:, kc, ch * 512:(ch + 1) * 512], kc == 0, kc == KC - 1,
                               [("MT", i, c) for c in range(4)] + ["WO"], [("ps", yb)])
                        STT(P, "dve", BIG[:, i, ch * 512:(ch + 1) * 512], BIG[:, i, ch * 512:(ch + 1) * 512], ALPHA, ps(yb),
                            ALU.mult, ALU.add, [("BIG", i), ("ps", yb)], [("BIG", i)])
                    layer_norm(i, L1G, L1B, "L1", tlL)
                    if "d_x1" in dbg_t and sq == 0 and l == 0:
                        DMA(P, "sp", dbg_t["d_x1"][i * 128:(i + 1) * 128, :], BIG[:, i, :], "dbg", ("dbg", len(P.ops)), [("BIG", i)], ["dbgout"])
                    to_XT(i, [xb0, xb1])
                P.barrier()
                A.release(m_layer)
                chk("merge2")

                m0 = A.mark()
                WR = A.alloc(F32, KC, 36)
                BR = A.alloc(F32, 36)
                X32T = A.alloc(F32, KC, 128)
                COMB = A.alloc(F32, NT, 32)
                CT = A.alloc(F32, S)
                LG = A.alloc(F32, 36)
                rr = A.alloc(F32, 16)
                e4 = A.alloc(F32, 4)
                gm = A.alloc(F32, 4)
                pen = A.alloc(F32, 4)
                subm = A.alloc(F32, 32)
                subm2 = A.alloc(F32, 32)
                oh1 = A.alloc(F32, 32)
                oh2 = A.alloc(F32, 32)
                W13 = [A.alloc(BF16, KC, 2, 2, 256) for _ in range(2)]
                W2s = [A.alloc(BF16, 2, 2, D) for _ in range(2)]
                HC = [A.alloc(BF16, 4, 512) for _ in range(2)]
                CB = [A.alloc(F32, 512) for _ in range(2)]
                SL = [A.alloc(F32, 512) for _ in range(2)]
                T1 = [A.alloc(F32, 512) for _ in range(2)]
                L2G = A.alloc(F32, D)
                L2B = A.alloc(F32, D)
                junkM = A.alloc(F32, D)
                stM = A.alloc(F32, 8)
                xbm = [A.alloc(BF16, D) for _ in range(2)]
                DMA(P, "sp", WR[:, :, 0:4], w_group[l].rearrange("(kc p) c -> p kc c", p=128), "wr", tag, [], ["WR"])
                DMA(P, "sp", WR[:, :, 4:36], w_sub[l].rearrange("(kc p) c -> p kc c", p=128), "wr", tag, [], ["WR"])
                DMA(P, "sp", BR[:, 0:4], b_group[l].partition_broadcast(128), "wr", tag, [], ["BR"])
                DMA(P, "sp", BR[:, 4:36], b_sub[l].partition_broadcast(128), "wr", tag, [], ["BR"])
                DMA(P, "sp", L2G, ln2_g[l].partition_broadcast(128), "ln2", tag, [], ["L2"])
                DMA(P, "sp", L2B, ln2_b[l].partition_broadcast(128), "ln2", tag, [], ["L2"])

                def load_pair(q):
                    s_ = q % 2
                    for e2 in range(2):
                        e_id = 2 * q + e2
                        DMA(P, "pool", W13[s_][:, :, e2, 0, :], w1[l, e_id].rearrange("(kc p) f -> p kc f", p=128),
                            "w13_%d" % s_, (tag, q), [], [("W13", s_)])
                        DMA(P, "pool", W13[s_][:, :, e2, 1, :], w3[l, e_id].rearrange("(kc p) f -> p kc f", p=128),
                            "w13_%d" % s_, (tag, q), [], [("W13", s_)])
                        DMA(P, "pool", W2s[s_][:, e2, :, :], w2[l, e_id].rearrange("(fc p) c -> p fc c", p=128),
                            "w2_%d" % s_, (tag, q), [], [("W2", s_)])

                load_pair(0)
                for i in range(NT):
                    kB = ("BIG", i)
                    for half in range(2):
                        tb = 6 + half
                        pvf = ps(tb).rearrange("p (k t) -> p k t", k=4)
                        for kk in range(4):
                            kc = 4 * half + kk
                            TR(P, pvf[:, kk, :], BIG[:, i, kc * 128:(kc + 1) * 128], identf, [kB, "identf"], [("ps", tb)])
                        CP(P, "act", X32T[:, 4 * half:4 * half + 4, :], pvf, [("ps", tb)], [("X32T", half)])
                    lb = 5
                    lg = ps(lb)[:, 0:36]
                    for kc in range(KC):
                        MM(P, lg, X32T[:, kc, :], WR[:, kc, :], kc == 0, kc == KC - 1,
                           [("X32T", 0), ("X32T", 1), "WR"], [("ps", lb)])
                    TT(P, "dve", LG, lg, BR, ALU.add, [("ps", lb), "BR"], ["LG"])
                    RED(P, rr[:, 0:1], LG[:, 0:4], ALU.max, ["LG"], ["rr0"])
                    TS(P, "dve", rr[:, 1:2], rr[:, 0:1], -1.0, None, ALU.mult, None, ["rr0"], ["rr1"])
                    ACTV(P, e4, LG[:, 0:4], AF.Exp, ["LG", "rr1"], ["e4", "rr2"], bias=rr[:, 1:2], accum=rr[:, 2:3])
                    RECIP(P, rr[:, 3:4], rr[:, 2:3], ["rr2"], ["rr3"])
                    TS(P, "dve", gm, LG[:, 0:4], rr[:, 0:1], None, ALU.is_ge, None, ["LG", "rr0"], ["gm"])
                    TS(P, "dve", pen, gm, 1.0e30, -1.0e30, ALU.mult, ALU.add, ["gm"], ["pen"])
                    TT(P, "dve", subm.rearrange("p (g e) -> p g e", g=4), LG[:, 4:36].rearrange("p (g e) -> p g e", g=4),
                       pen.unsqueeze(2).to_broadcast([128, 4, 8]), ALU.add, ["LG", "pen"], ["subm"])
                    RED(P, rr[:, 4:5], subm, ALU.max, ["subm"], ["rr4"])
                    TS(P, "dve", oh1, subm, rr[:, 4:5], None, ALU.is_ge, None, ["subm", "rr4"], ["oh1"])
                    STT(P, "dve", subm2, oh1, -1.0e30, subm, ALU.mult, ALU.add, ["oh1", "subm"], ["subm2"])
                    RED(P, rr[:, 5:6], subm2, ALU.max, ["subm2"], ["rr5"])
                    TS(P, "dve", oh2, subm2, rr[:, 5:6], None, ALU.is_ge, None, ["subm2", "rr5"], ["oh2"])
                    TT(P, "dve", rr[:, 6:7], rr[:, 5:6], rr[:, 4:5], ALU.subtract, ["rr4", "rr5"], ["rr6"])
                    ACTV(P, rr[:, 7:8], rr[:, 6:7], AF.Exp, ["rr6"], ["rr7"])
                    TS(P, "dve", rr[:, 8:9], rr[:, 7:8], 1.0, None, ALU.add, None, ["rr7"], ["rr8"])
                    RECIP(P, rr[:, 9:10], rr[:, 8:9], ["rr8"], ["rr9"])
                    TT(P, "dve", rr[:, 10:11], rr[:, 9:10], rr[:, 3:4], ALU.mult, ["rr9", "rr3"], ["rr10"])
                    TT(P, "dve", rr[:, 11:12], rr[:, 10:11], rr[:, 7:8], ALU.mult, ["rr10", "rr7"], ["rr11"])
                    TS(P, "dve", COMB[:, i, :], oh1, rr[:, 10:11], None, ALU.mult, None, ["oh1", "rr10"], [("COMB", i)])
                    STT(P, "dve", COMB[:, i, :], oh2, rr[:, 11:12], COMB[:, i, :], ALU.mult, ALU.add,
                        ["oh2", "rr11", ("COMB", i)], [("COMB", i)])
                    tb = 6
                    TR(P, ps(tb)[0:32, 0:128], COMB[:, i, :], identf, [("COMB", i), "identf"], [("ps", tb)])
                    CP(P, "act", CT[0:32, i * 128:(i + 1) * 128], ps(tb)[0:32, 0:128], [("ps", tb)], [("CT", i // 4)])
                    TS(P, "dve", BIG[:, i, :], BIG[:, i, :], ALPHA, None, ALU.mult, None, [kB], [kB])
                if "d_comb" in dbg_t and sq == 0 and l == 0:
                    DMA(P, "sp", dbg_t["d_comb"].rearrange("p (i e) -> p i e", i=NT), COMB, "dbg", ("dbg", len(P.ops)),
                        [("COMB", i) for i in range(NT)], ["dbgout"])
                def emit_y(tc, hs, s_):
                    for t_ in range(4):
                        ti = 4 * tc + t_
                        for ch in range(2):
                            yb = 5 + slot("Yb", 3)
                            for fcc in range(4):
                                MM(P, ps(yb), HC[hs][:, fcc, 128 * t_:128 * t_ + 128],
                                   W2s[s_][:, fcc // 2, fcc % 2, 512 * ch:512 * ch + 512], fcc == 0, fcc == 3,
                                   [("HC", hs), ("W2", s_)], [("ps", yb)])
                            TT(P, "dve", BIG[:, ti, 512 * ch:512 * ch + 512], BIG[:, ti, 512 * ch:512 * ch + 512], ps(yb),
                               ALU.add, [("BIG", ti), ("ps", yb)], [("BIG", ti)])

                pending = None
                for q in range(16):
                    s_ = q % 2
                    for tc in range(4):
                        csl = slice(512 * tc, 512 * tc + 512)
                        hs = slot("HC", 2)
                        for e2 in range(2):
                            e_id = 2 * q + e2
                            MM(P, ps(4), identf[0:32, e_id:e_id + 1].to_broadcast([32, 128]), CT[0:32, csl], True, True,
                               [("CT", tc), "identf"], [("ps", 4)])
                            CP(P, "act", CB[e2], ps(4), [("ps", 4)], [("CB", e2)])
                        for e2 in range(2):
                            for fc in range(2):
                                hb = 2 * slot("H", 2)
                                for wi in range(2):
                                    for kc in range(KC):
                                        MM(P, ps(hb + wi), W13[s_][:, kc, e2, wi, 128 * fc:128 * fc + 128], XT[:, kc, csl],
                                           kc == 0, kc == KC - 1,
                                           [("W13", s_)] + [("XT", 4 * tc + t_) for t_ in range(4)], [("ps", hb + wi)])
                                ks = slot("SL", 2)
                                ACTV(P, SL[ks], ps(hb), AF.Silu, [("ps", hb)], [("SL", ks)])
                                TT(P, "dve", T1[ks], SL[ks], ps(hb + 1), ALU.mult, [("SL", ks), ("ps", hb + 1)], [("T1", ks)])
                                TT(P, "dve", HC[hs][:, 2 * e2 + fc, :], T1[ks], CB[e2], ALU.mult,
                                   [("T1", ks), ("CB", e2)], [("HC", hs)])
                        if pending is not None:
                            emit_y(*pending)
                        pending = (tc, hs, s_)
                        if tc == 0 and q + 1 < 16:
                            load_pair(q + 1)
                emit_y(*pending)
                tlM = {"st": stM, "junk": junkM}
                for i in range(NT):
                    layer_norm(i, L2G, L2B, "L2", tlM)
                    if l == nlayers - 1:
                        DMA(P, "sp", out[sq, i * 128:(i + 1) * 128, :], BIG[:, i, :], "out", sq, [("BIG", i)], ["OUT"])
                    else:
                        to_XT(i, xbm)
                P.barrier()
                A.release(m0)
                chk("moe")

        try:
            for sq in range(nseq):
                for l in range(nlayers):
                    seq_layer(sq, l)
        except Stop:
            pass

        import os
        lim = int(os.environ.get("OPLIM", "0"))
        if lim:
            P.ops = P.ops[:lim]
            P.last_eng = {}
            P.last_dma = {}
            for ii, oo in enumerate(P.ops):
                if oo.dma is None:
                    P.last_eng[oo.eng] = ii
                else:
                    P.last_dma[oo.dma[0]] = ii
            P.gen = {}
        P.add("sp", lambda e: e.nop(), ["OUT", "dbgout"], [])
        P.barrier()
        P.add("sp", lambda e: e.nop(), [], [])
        print("arena peak (bf16 elems):", A.peak, "ops:", len(P.ops))
        P.emit(nc, stack)
    return nc


def host_consts():
    ident = np.eye(128, dtype=np.float32)
    s_ = np.arange(128)[:, None]
    t_ = np.arange(128)[None, :]
    caus = (s_ <= t_)
    prev = (s_ >= t_)
    r4 = ((t_ - s_) % 4 == 0)
    r16 = ((t_ - s_) % 16 == 0)
    masks = np.stack([caus, prev, r4 & caus, r4, r4 & prev, r16 & caus, r16], axis=1).astype(np.float32)
    negm = np.where(np.arange(128)[None, :] <= np.arange(128)[:, None], 0.0, NEG).astype(np.float32)
    pos = (np.arange(NT)[None, :] * 128 + np.arange(128)[:, None]).astype(np.float32)

    def tab(rot):
        inv = (500000.0 ** (-np.arange(0, rot, 2, dtype=np.float32) / rot)).astype(np.float32)
        ang = (pos[:, :, None] * inv[None, None, :]).astype(np.float32)
        c = np.cos(ang).astype(np.float32)
        s = np.sin(ang).astype(np.float32)
        return np.concatenate([c, c, -s, s], axis=-1).astype(np.float32)

    return {
        "c_ident": ident,
        "c_masks": np.ascontiguousarray(masks.reshape(128, 7 * 128)),
        "c_negm": negm,
        "c_ropep": np.ascontiguousarray(tab(16).reshape(128, NT * 32)),
        "c_ropem": np.ascontiguousarray(tab(32).reshape(128, NT * 64)),
    }


_NC_CACHE = {}

IMPLEMENTED = True


SEQ_PER_LAUNCH = 4


def kernel(**inputs):
    nseq_total = 32 // NCORES
    npl = SEQ_PER_LAUNCH
    if npl not in _NC_CACHE:
        _NC_CACHE[npl] = build(nseq=npl)
    nc = _NC_CACHE[npl]
    consts = host_consts()
    xs = np.ascontiguousarray(inputs["x"], dtype=np.float32)
    base = {k: np.ascontiguousarray(v, dtype=np.float32) for k, v in inputs.items() if k != "x"}
    base.update(consts)
    outs = [[None] * (nseq_total // npl) for _ in range(NCORES)]
    for r in range(nseq_total // npl):
        in_maps = []
        for c in range(NCORES):
            m = dict(base)
            lo = c * nseq_total + r * npl
            m["x"] = xs[lo:lo + npl]
            in_maps.append(m)
        res = run_bass_kernel_spmd(nc, in_maps, core_ids=list(range(NCORES)))
        for c in range(NCORES):
            outs[c][r] = np.asarray(res.results[c]["out"], dtype=np.float32)
    return np.concatenate([o for c in range(NCORES) for o in outs[c]], axis=0).astype(np.float32)
```
